# Optimizing a Trainium2 kernel written in Bass

```python
import math
import jax, jax.numpy as jnp
from jax import lax
import numpy as np

D_MODEL = 2048
BATCH = 4
SEQ = 4096
DEPTH = 1

MIX_WIDTH = D_MODEL
POOL_WIDTH = MIX_WIDTH // 2
POOL_WINDOWS = (2, 4, 8, 16)
N_POOL_GROUPS = len(POOL_WINDOWS)
POOL_GROUP = POOL_WIDTH // N_POOL_GROUPS
ATTN_WIDTH = MIX_WIDTH - POOL_WIDTH
HEAD_DIM = 128
N_HEADS = ATTN_WIDTH // HEAD_DIM
N_KV_HEADS = 2
Q_PER_KV = N_HEADS // N_KV_HEADS
WINDOW = 128
BLOCK = 128
N_BUCKETS = 32
MAX_DISTANCE = 128
N_EXPERTS = 16
CAPACITY_FACTOR = 2
D_FF = 2 * D_MODEL
IN_WIDTH = POOL_WIDTH + ATTN_WIDTH + 2 * N_KV_HEADS * HEAD_DIM
EPS = 1e-6

kernel_name = "hybrid_pool_swa_ecmoe_encoder"


def rmsnorm(x, g):
    xf = x.astype(jnp.float32)
    y = xf * lax.rsqrt(jnp.mean(xf * xf, axis=-1, keepdims=True) + EPS)
    return (y * g.astype(jnp.float32)).astype(x.dtype)


def multiscale_pool(u, pool_w, pool_scale):
    B, S, _ = u.shape
    ug = u.reshape(B, S, N_POOL_GROUPS, POOL_GROUP)
    ugf = ug.astype(jnp.float32)
    c = jnp.cumsum(ugf, axis=1)
    c = jnp.pad(c, ((0, 0), (1, 0), (0, 0), (0, 0)))
    t = jnp.arange(S)
    means = []
    for g, w in enumerate(POOL_WINDOWS):
        lo = jnp.clip(t - w // 2, 0, S)
        hi = jnp.clip(t + w // 2, 0, S)
        cg = c[:, :, g]
        cnt = (hi - lo).astype(jnp.float32)[None, :, None]
        means.append((cg[:, hi] - cg[:, lo]) / cnt)
    pooled = (jnp.stack(means, axis=2) - ugf).astype(u.dtype)
    mixed = jnp.einsum('bsgc,gcd->bsgd', pooled, pool_w)
    return mixed.reshape(B, S, POOL_WIDTH) * pool_scale


def t5_bucket(rel):
    half = N_BUCKETS // 2
    max_exact = half // 2
    ret = jnp.where(rel > 0, half, 0)
    n = jnp.abs(rel)
    nf = jnp.maximum(n, 1).astype(jnp.float32)
    large = max_exact + (jnp.log(nf / max_exact) / math.log(MAX_DISTANCE / max_exact)
                         * (half - max_exact)).astype(jnp.int32)
    large = jnp.minimum(large, half - 1)
    return ret + jnp.where(n < max_exact, n, large)


def windowed_gqa(q, k, v, sink, rel_bias):
    B, S, _ = q.shape
    nb = S // BLOCK
    qb = q.reshape(B, nb, BLOCK, N_KV_HEADS, Q_PER_KV, HEAD_DIM)

    def band(t):
        t = t.reshape(B, nb, BLOCK, N_KV_HEADS, HEAD_DIM)
        tp = jnp.pad(t, ((0, 0), (1, 1), (0, 0), (0, 0), (0, 0)))
        return jnp.concatenate([tp[:, :-2], tp[:, 1:-1], tp[:, 2:]], axis=2)

    kb, vb = band(k), band(v)
    scale = HEAD_DIM ** -0.5
    logits = jnp.einsum('bnqkgd,bnskd->bnkgqs', qb, kb).astype(jnp.float32) * scale

    qi = jnp.arange(BLOCK)[:, None]
    kj = jnp.arange(3 * BLOCK)[None, :] - BLOCK
    rel = kj - qi
    bias = rel_bias[t5_bucket(rel)].astype(jnp.float32)
    bias = jnp.transpose(bias, (2, 0, 1)).reshape(N_KV_HEADS, Q_PER_KV, BLOCK, 3 * BLOCK)

    kpos = jnp.arange(nb)[:, None] * BLOCK - BLOCK + jnp.arange(3 * BLOCK)[None, :]
    valid = (kpos >= 0) & (kpos < S)
    mask = (jnp.abs(rel) <= WINDOW)[None, :, :] & valid[:, None, :]
    logits = jnp.where(mask[None, :, None, None], logits + bias, -jnp.inf)

    sink_col = jnp.broadcast_to(
        sink.astype(jnp.float32).reshape(1, 1, N_KV_HEADS, Q_PER_KV, 1, 1),
        logits.shape[:-1] + (1,))
    p = jax.nn.softmax(jnp.concatenate([logits, sink_col], axis=-1), axis=-1)[..., :-1]
    out = jnp.einsum('bnkgqs,bnskd->bnqkgd', p.astype(v.dtype), vb)
    return out.reshape(B, S, ATTN_WIDTH)


def expert_choice_moe(h, w_router, w_gate, w_up, w_down):
    B, S, D = h.shape
    cap = CAPACITY_FACTOR * S // N_EXPERTS
    aff = jax.nn.softmax(jnp.einsum('bsd,de->bse', h, w_router).astype(jnp.float32), axis=-1)
    gate, idx = lax.top_k(jnp.swapaxes(aff, 1, 2), cap)
    xs = jax.vmap(lambda hb, ib: hb[ib])(h, idx)
    a = jnp.einsum('becd,edf->becf', xs, w_gate)
    u = jnp.einsum('becd,edf->becf', xs, w_up)
    y = jnp.einsum('becf,efd->becd', jax.nn.silu(a) * u, w_down)
    y = y * gate[..., None].astype(y.dtype)
    flat = (jnp.arange(B)[:, None, None] * S + idx).reshape(-1)
    out = jnp.zeros((B * S, D), h.dtype).at[flat].add(y.reshape(-1, D))
    return out.reshape(B, S, D)


def setup_inputs(seed: int = 0) -> dict:
    key = jax.random.key(seed)
    ks = jax.random.split(key, 20)
    f32 = jnp.float32
    nrm = lambda k, shape, s: jax.random.normal(k, shape, f32) * s
    L = DEPTH
    return {
        "x": nrm(ks[0], (BATCH, SEQ, D_MODEL), 1.0),
        "norm1_g": 1.0 + nrm(ks[1], (L, D_MODEL), 0.02),
        "w_in": nrm(ks[2], (L, D_MODEL, IN_WIDTH), D_MODEL ** -0.5),
        "pool_w": nrm(ks[3], (L, N_POOL_GROUPS, POOL_GROUP, POOL_GROUP), POOL_GROUP ** -0.5),
        "pool_scale": 1.0 + nrm(ks[4], (L, POOL_WIDTH), 0.02),
        "rel_bias": nrm(ks[5], (N_BUCKETS, N_HEADS), 0.5),
        "sink": nrm(ks[6], (L, N_HEADS), 0.5),
        "gn_pool": 1.0 + nrm(ks[7], (L, POOL_WIDTH), 0.02),
        "gn_attn": 1.0 + nrm(ks[8], (L, ATTN_WIDTH), 0.02),
        "w_out": nrm(ks[9], (L, MIX_WIDTH, D_MODEL), MIX_WIDTH ** -0.5),
        "norm2_g": 1.0 + nrm(ks[10], (L, D_MODEL), 0.02),
        "w_router": nrm(ks[11], (L, D_MODEL, N_EXPERTS), D_MODEL ** -0.5),
        "w_gate": nrm(ks[12], (L, N_EXPERTS, D_MODEL, D_FF), D_MODEL ** -0.5),
        "w_up": nrm(ks[13], (L, N_EXPERTS, D_MODEL, D_FF), D_MODEL ** -0.5),
        "w_down": nrm(ks[14], (L, N_EXPERTS, D_FF, D_MODEL), D_FF ** -0.5),
        "final_g": 1.0 + nrm(ks[15], (D_MODEL,), 0.02),
    }


def reference(x, norm1_g, w_in, pool_w, pool_scale, rel_bias, sink, gn_pool, gn_attn,
              w_out, norm2_g, w_router, w_gate, w_up, w_down, final_g):
    q_end = POOL_WIDTH + ATTN_WIDTH
    k_end = q_end + N_KV_HEADS * HEAD_DIM
    for l in range(DEPTH):
        h = rmsnorm(x, norm1_g[l])
        proj = jnp.einsum('bsd,dp->bsp', h, w_in[l])
        u_pool = proj[..., :POOL_WIDTH]
        q = proj[..., POOL_WIDTH:q_end]
        k = proj[..., q_end:k_end]
        v = proj[..., k_end:]
        y_pool = rmsnorm(multiscale_pool(u_pool, pool_w[l], pool_scale[l]), gn_pool[l])
        y_attn = rmsnorm(windowed_gqa(q, k, v, sink[l], rel_bias), gn_attn[l])
        mix = jnp.concatenate([y_pool, y_attn], axis=-1)
        x = x + jnp.einsum('bsm,md->bsd', mix, w_out[l])
        h2 = rmsnorm(x, norm2_g[l])
        x = x + expert_choice_moe(h2, w_router[l], w_gate[l], w_up[l], w_down[l])
    return rmsnorm(x, final_g)
```

```python
import os
import math
import numpy as np
import ml_dtypes
from contextlib import ExitStack
import concourse.bass as bass
import concourse.mybir as mybir
from concourse.bass_utils import run_bass_kernel_spmd

F32 = mybir.dt.float32
BF16 = mybir.dt.bfloat16
I32 = mybir.dt.int32
ALU = mybir.AluOpType
AF = mybir.ActivationFunctionType

S = 4096
D = 2048
NT = S // 128
NG = S // 512
INW = 2560
DFF = 4096
NE = 16
CAP = 512
EPS = 1e-6
NCORES = 4
NSLOT = 6
NBISECT = 31
SEM_EPOCH = 30000


class Buf:
    __slots__ = ("name", "w", "rs")

    def __init__(self, name=""):
        self.name = name
        self.w = None
        self.rs = {}


class SemC:
    __slots__ = ("sem", "count", "is_dma", "unit")

    def __init__(self, sem, is_dma, unit):
        self.sem = sem
        self.count = 0
        self.is_dma = is_dma
        self.unit = unit


class Ctx:
    def __init__(self, nc, es):
        self.nc = nc
        self.es = es
        self.engs = {"pe": nc.tensor, "act": nc.scalar, "dve": nc.vector, "pool": nc.gpsimd, "sp": nc.sync}
        self.esem = {}
        self.all_sems = []
        self.nsem = 0
        for k in self.engs:
            self._new_esem(k)
        self.waited = {k: {} for k in self.engs}
        self.nwaits = 0
        self.nops = 0

    def _new_esem(self, k):
        s = SemC(self.es.enter_context(self.nc.semaphore("e%s%d" % (k, self.nsem))), False, 1)
        self.nsem += 1
        self.esem[k] = s
        self.all_sems.append(s)

    def dsem(self, name):
        s = SemC(self.es.enter_context(self.nc.semaphore("d" + name)), True, 16)
        self.nsem += 1
        self.all_sems.append(s)
        return s

    def _wait(self, eng, semc, val):
        if semc.is_dma:
            val = semc.count * semc.unit
        if val <= 0:
            return
        w = self.waited[eng]
        if w.get(id(semc), 0) >= val:
            return
        self.engs[eng].wait_ge(semc.sem, val)
        w[id(semc)] = val
        self.nwaits += 1

    def op(self, eng, fn, reads=(), writes=(), dma=None):
        for b in reads:
            if b.w is not None:
                self._wait(eng, b.w[0], b.w[1])
        for b in writes:
            for t in b.rs.values():
                if dma is None and t[2] == eng:
                    continue
                self._wait(eng, t[0], t[1])
            t = b.w
            if t is not None and not (dma is None and t[2] == eng):
                self._wait(eng, t[0], t[1])
        inst = fn()
        self.nops += 1
        if dma is not None:
            dma.count += 1
            inst.then_inc(dma.sem, dma.unit)
            tok = (dma, dma.count * dma.unit, None)
        else:
            s = self.esem[eng]
            if s.count >= SEM_EPOCH:
                self._new_esem(eng)
                s = self.esem[eng]
            s.count += 1
            inst.then_inc(s.sem, 1)
            tok = (s, s.count, eng)
        for b in writes:
            b.w = tok
            b.rs = {}
        for b in reads:
            b.rs[id(tok[0])] = tok
        return tok

    def barrier(self):
        for e in self.engs:
            for s in self.all_sems:
                if s is self.esem.get(e):
                    continue
                self._wait(e, s, s.count * s.unit)


def _t5_bucket_np(rel):
    half, max_exact = 16, 8
    ret = np.where(rel > 0, half, 0)
    n = np.abs(rel)
    nf = np.maximum(n, 1).astype(np.float64)
    large = max_exact + np.floor(2.0 * np.log2(nf / max_exact) + 1e-6).astype(np.int64)
    large = np.minimum(large, half - 1)
    return ret + np.where(n < max_exact, n, large)


def _host_constants():
    c = {}
    c["ident_bf"] = np.eye(128, dtype=np.float32).astype(ml_dtypes.bfloat16)
    c["ident_f"] = np.eye(128, dtype=np.float32)
    c["iota512"] = np.broadcast_to(np.arange(512, dtype=np.float32), (128, 512)).copy()
    tok = (np.arange(128)[:, None] + 128 * np.arange(32)[None, :])
    tokab = np.stack([tok // 64, tok % 64], axis=-1).astype(np.float32)
    c["tokab"] = tokab.copy()
    kk = np.arange(128)[:, None]
    qq = np.arange(128)[None, :]
    mask = np.zeros((3, 128, 512), np.float32)
    bidx = np.zeros((3, 128, 128), np.int64)
    for kb in range(3):
        rel = (kb - 1) * 128 + kk - qq
        m = np.where(np.abs(rel) <= 128, 0.0, -1.0e5).astype(np.float32)
        mask[kb] = np.tile(m, (1, 4))
        bidx[kb] = _t5_bucket_np(rel)
    c["mask01"] = mask
    c["_bidx"] = bidx
    edge = np.zeros((4, 16), np.float32)
    for wi, w in enumerate((2, 4, 8, 16)):
        for i in range(16):
            t = i if i < 8 else S - 16 + i
            lo = max(t - w // 2, 0)
            hi = min(t + w // 2, S)
            edge[wi, i] = 1.0 / float(hi - lo)
    c["edgef"] = np.broadcast_to(edge[None], (128, 4, 16)).copy()
    return c


def build_nc(debug=0):
    nc = bass.Bass("TRN2", target_bir_lowering=False)
    dt_in = lambda name, shape, dt=F32: nc.dram_tensor(name, shape, dt, kind="ExternalInput")
    x_d = dt_in("x", [S, D])
    g1bc_d = dt_in("g1bc", [128, D])
    g2bc_d = dt_in("g2bc", [128, D])
    gfbc_d = dt_in("gfbc", [128, D])
    w_in_d = dt_in("w_in", [D, INW])
    pool_w_d = dt_in("pool_w", [4, 256, 256])
    w_out_d = dt_in("w_out", [D, D])
    w_router_d = dt_in("w_router", [D, NE])
    if debug == 0 or debug >= 5:
        w_gate_d = dt_in("w_gate", [NE, D, DFF])
        w_up_d = dt_in("w_up", [NE, D, DFF])
        w_down_d = dt_in("w_down", [NE, DFF, D])
    pscale_d = dt_in("pscale", [128, 8])
    gnfm_d = dt_in("gnfm", [128, 16])
    sinkbc_d = dt_in("sinkbc", [128, 1024])
    biastab_d = dt_in("biastab", [6, 128, 512])
    mask01_d = dt_in("mask01", [3, 128, 512])
    ident_bf_d = dt_in("ident_bf", [128, 128], BF16)
    ident_f_d = dt_in("ident_f", [128, 128])
    iota512_d = dt_in("iota512", [128, 512])
    tokab_d = dt_in("tokab", [128, 32, 2])
    edgef_d = dt_in("edgef", [128, 4, 16])
    out_d = nc.dram_tensor("out", [S, D], F32, kind="ExternalOutput")
    dk = lambda lv: "ExternalOutput" if debug == lv else "Internal"
    qu_scr = nc.dram_tensor("qu_scr", [16, 128, S], BF16, kind=dk(1))
    mix_scr = nc.dram_tensor("mix_scr", [16, 128, S], BF16, kind=dk(2))
    h2_scr = nc.dram_tensor("h2_scr", [S, D], BF16, kind=dk(3))
    acc_d = nc.dram_tensor("acc", [S, D], F32, kind=dk(3))
    if debug:
        dbg_aff = nc.dram_tensor("dbg_aff", [NE, S], F32, kind="ExternalOutput")
        dbg_kv = nc.dram_tensor("dbg_kv", [128, 2 * S + NT * 256], BF16, kind="ExternalOutput")
        dbg_rstd = nc.dram_tensor("dbg_rstd", [128, 64], F32, kind="ExternalOutput")
        dbg_idx = nc.dram_tensor("dbg_idx", [NE, 2, 128, 4], F32, kind="ExternalOutput")
        dbg_thr = nc.dram_tensor("dbg_thr", [2, NE, 1], F32, kind="ExternalOutput")

    qu_pm = qu_scr.ap().rearrange("c p t -> p c t")
    mix_pm = mix_scr.ap().rearrange("c p t -> p c t")

    with ExitStack() as es:
        cx = Ctx(nc, es)
        sbuf = lambda st, name, shape, dt: st.enter_context(nc.sbuf_tensor("s_" + name, shape, dt))
        psum = lambda st, name, shape, dt=F32: st.enter_context(nc.psum_tensor("p_" + name, shape, dt))
        V, A, P, PE, SP = "dve", "act", "pool", "pe", "sp"
        nv, na, npl, nt, nsp = nc.vector, nc.scalar, nc.gpsimd, nc.tensor, nc.sync

        B_qu, B_mix, B_h2, B_acc, B_out = Buf("qu"), Buf("mix"), Buf("h2"), Buf("acc"), Buf("out")
        d_out = cx.dsem("out")

        ident_bf = sbuf(es, "ident_bf", [128, 128], BF16)
        ident_f = sbuf(es, "ident_f", [128, 128], F32)
        rstd_pa = sbuf(es, "rstd_pa", [128, 64], F32)
        eps_t = sbuf(es, "eps_t", [128, 1], F32)
        B_const, B_rstd = Buf("const"), Buf("rstd")
        d_const = cx.dsem("const")
        cx.op(SP, lambda: nsp.dma_start(out=ident_bf[:], in_=ident_bf_d[:, :]), writes=[B_const], dma=d_const)
        cx.op(SP, lambda: nsp.dma_start(out=ident_f[:], in_=ident_f_d[:, :]), writes=[B_const], dma=d_const)
        cx.op(V, lambda: nv.memset(eps_t[:], EPS), writes=[B_const])

        def rstd_from_ssq(ssq_ap, out_ap, n, rb, wb):
            cx.op(A, lambda: na.activation(out=out_ap, in_=ssq_ap, func=AF.Sqrt, scale=1.0 / float(n), bias=eps_t[:, 0:1]),
                  reads=list(rb) + [B_const], writes=wb)
            cx.op(V, lambda: nv.reciprocal(out=out_ap, in_=out_ap), reads=wb, writes=wb)

        with ExitStack() as s1:
            kT_all = sbuf(s1, "kT_all", [128, 2, S], BF16)
            V_all = sbuf(s1, "V_all", [128, NT, 256], BF16)
            B_kT = [Buf("kT%d" % g) for g in range(NG)]
            B_V = [Buf("V%d" % g) for g in range(NG)]

            with ExitStack() as ph:
                w_in_sb = sbuf(ph, "w_in_sb", [128, 16, INW], BF16)
                g1bc = sbuf(ph, "g1bc", [128, D], F32)
                xbuf = [sbuf(ph, "xb%d" % i, [128, D], F32) for i in range(2)]
                hb = [sbuf(ph, "hb%d" % i, [128, D], BF16) for i in range(2)]
                hT = [sbuf(ph, "hT%d" % i, [128, 16, 512], BF16) for i in range(2)]
                qu_st = sbuf(ph, "qu_st", [128, 16, 512], BF16)
                ssq = sbuf(ph, "ssq", [128, NT], F32)
                rs1 = sbuf(ph, "rs1", [128, NT], F32)
                tp = [psum(ph, "tp%d" % i, [128, 8, 128], BF16) for i in range(3)]
                pp = [psum(ph, "pp%d" % i, [128, 512], F32) for i in range(5)]
                B_w, B_g1 = Buf("w_in"), Buf("g1")
                B_x = [Buf() for _ in range(2)]
                B_hb = [Buf() for _ in range(2)]
                B_hT = [Buf() for _ in range(2)]
                B_ssq = [Buf() for _ in range(2)]
                B_rs = [Buf() for _ in range(2)]
                B_tp = [Buf() for _ in range(3)]
                B_pp = [Buf() for _ in range(5)]
                B_qs = Buf("qu_st")
                d_w, d_x, d_qs = cx.dsem("w1"), [cx.dsem("x0"), cx.dsem("x1")], cx.dsem("qs")
                w_in_v = w_in_d.ap().rearrange("(kc p) n -> p kc n", p=128)
                for q4 in range(4):
                    cx.op(P, lambda: npl.dma_start(out=w_in_sb[:, 4 * q4:4 * q4 + 4, :], in_=w_in_v[:, 4 * q4:4 * q4 + 4, :]),
                          writes=[B_w], dma=d_w)
                cx.op(SP, lambda: nsp.dma_start(out=g1bc[:], in_=g1bc_d[:, :]), writes=[B_g1], dma=d_const)
                cx.op(V, lambda: nv.memset(ssq[:], 0.0), writes=B_ssq)
                tpi = 0
                ppi = 0
                evi = 0
                for g in range(NG):
                    gp = g % 2
                    for i in range(4):
                        T = 4 * g + i
                        tpar = T % 2
                        cx.op(SP, lambda: nsp.dma_start(out=xbuf[tpar][:], in_=x_d[T * 128:(T + 1) * 128, :]),
                              writes=[B_x[tpar]], dma=d_x[tpar])
                        cx.op(A, lambda: na.activation(out=hb[tpar][:], in_=xbuf[tpar][:], func=AF.Square,
                                                       accum_out=ssq[:, T:T + 1]),
                              reads=[B_x[tpar]], writes=[B_hb[tpar], B_ssq[tpar]])
                        rstd_from_ssq(ssq[:, T:T + 1], rs1[:, T:T + 1], D, [B_ssq[tpar]], [B_rs[tpar]])
                        cx.op(V, lambda: nv.scalar_tensor_tensor(out=hb[tpar][:], in0=xbuf[tpar][:], scalar=rs1[:, T:T + 1],
                                                                 in1=g1bc[:], op0=ALU.mult, op1=ALU.mult),
                              reads=[B_x[tpar], B_rs[tpar], B_g1], writes=[B_hb[tpar]])
                        for half in range(2):
                            tb = tpi % 3
                            tpi += 1
                            for k in range(8):
                                kc = half * 8 + k
                                cx.op(PE, lambda: nt.transpose(out=tp[tb][:, k, :], in_=hb[tpar][:, kc * 128:(kc + 1) * 128],
                                                               identity=ident_bf[:]),
                                      reads=[B_hb[tpar], B_const], writes=[B_tp[tb]])
                            dst = hT[gp][:, half * 8:half * 8 + 8, i * 128:(i + 1) * 128]
                            if half == 0:
                                cx.op(A, lambda: na.copy(out=dst, in_=tp[tb][:]), reads=[B_tp[tb]], writes=[B_hT[gp]])
                            else:
                                cx.op(V, lambda: nv.tensor_copy(out=dst, in_=tp[tb][:]), reads=[B_tp[tb]], writes=[B_hT[gp]])
                    for oc in range(18):
                        c0 = oc * 128
                        pb = ppi % 5
                        ppi += 1
                        for kc in range(16):
                            cx.op(PE, lambda: nt.matmul(pp[pb][:], lhsT=w_in_sb[:, kc, c0:c0 + 128], rhs=hT[gp][:, kc, :],
                                                        start=(kc == 0), stop=(kc == 15)),
                                  reads=[B_w, B_hT[gp]], writes=[B_pp[pb]])
                        if oc < 16:
                            dst, wb = qu_st[:, oc, :], B_qs
                        else:
                            dst, wb = kT_all[:, oc - 16, g * 512:(g + 1) * 512], B_kT[g]
                        evi += 1
                        if evi % 2 == 0:
                            cx.op(A, lambda: na.copy(out=dst, in_=pp[pb][:]), reads=[B_pp[pb]], writes=[wb])
                        else:
                            cx.op(V, lambda: nv.tensor_copy(out=dst, in_=pp[pb][:]), reads=[B_pp[pb]], writes=[wb])
                    cx.op(SP, lambda: nsp.dma_start(out=qu_pm[:, :, g * 512:(g + 1) * 512], in_=qu_st[:]),
                          reads=[B_qs], writes=[B_qu], dma=d_qs)
                    for i in range(4):
                        pb = ppi % 5
                        ppi += 1
                        for kc in range(16):
                            cx.op(PE, lambda: nt.matmul(pp[pb][:, 0:256], lhsT=hT[gp][:, kc, i * 128:(i + 1) * 128],
                                                        rhs=w_in_sb[:, kc, 2304:2560], start=(kc == 0), stop=(kc == 15)),
                                  reads=[B_w, B_hT[gp]], writes=[B_pp[pb]])
                        evi += 1
                        if evi % 2 == 0:
                            cx.op(A, lambda: na.copy(out=V_all[:, 4 * g + i, :], in_=pp[pb][:, 0:256]),
                                  reads=[B_pp[pb]], writes=[B_V[g]])
                        else:
                            cx.op(V, lambda: nv.tensor_copy(out=V_all[:, 4 * g + i, :], in_=pp[pb][:, 0:256]),
                                  reads=[B_pp[pb]], writes=[B_V[g]])
                if debug:
                    d_dbg = cx.dsem("dbg")
                    cx.op(SP, lambda: nsp.dma_start(out=dbg_kv[:, 0:2 * S], in_=kT_all[:]), reads=B_kT, writes=[B_out], dma=d_dbg)
                    cx.op(SP, lambda: nsp.dma_start(out=dbg_kv[:, 2 * S:], in_=V_all[:]), reads=B_V, writes=[B_out], dma=d_dbg)
                cx.barrier()
            if debug == 1:
                cx.barrier()
                return nc

            with ExitStack() as ph:
                eb = sbuf(ph, "eb", [128, 6, 512], F32)
                Bs = sbuf(ph, "Bs", [128, 6, 512], BF16)
                B_Bs = Buf()
                mk = sbuf(ph, "mk", [128, 3, 512], F32)
                sinkexp = sbuf(ph, "sinkexp", [128, 1024], F32)
                ones_bf = sbuf(ph, "ones_bf", [128, 128], BF16)
                pw_sb = sbuf(ph, "pw_sb", [128, 4, 2, 256], BF16)
                pscale = sbuf(ph, "pscale", [128, 8], F32)
                edgef = sbuf(ph, "edgef", [128, 4, 16], F32)
                qT = [sbuf(ph, "qT%d" % i, [128, 8, 512], BF16) for i in range(2)]
                uT = [sbuf(ph, "uT%d" % i, [128, 8, 528], BF16) for i in range(2)]
                tmp = [sbuf(ph, "ptmp%d" % i, [128, 2, 528], F32) for i in range(4)]
                etmp = sbuf(ph, "etmp", [128, 2, 8], F32)
                tmp4 = sbuf(ph, "ptmp4", [128, 2, 512], F32)
                pooledT = sbuf(ph, "pooledT", [128, 8, 512], BF16)
                sqp = sbuf(ph, "sqp", [128, 8, 512], BF16)
                et = [sbuf(ph, "et%d" % i, [128, 512], F32) for i in range(2)]
                pt = [sbuf(ph, "pt%d" % i, [128, 512], BF16) for i in range(9)]
                den = [sbuf(ph, "den%d" % i, [128, 512], F32) for i in range(2)]
                at = [sbuf(ph, "at%d" % i, [128, 4, 128], F32) for i in range(2)]
                sqa = [sbuf(ph, "sqa%d" % i, [128, 4, 128], BF16) for i in range(4)]
                mixst = [sbuf(ph, "mixst%d" % i, [128, 16, 512], BF16) for i in range(2)]
                sps = [psum(ph, "sps%d" % i, [128, 512], F32) for i in range(2)]
                o_ps = [psum(ph, "ops%d" % i, [128, 4, 128], F32) for i in range(2)]
                d_ps = [psum(ph, "dps%d" % i, [128, 512], F32) for i in range(2)]
                pw_ps = psum(ph, "pwps", [128, 512], F32)
                ss_ps = psum(ph, "ssps", [128, 512], F32)
                B_eb, B_mk, B_sk, B_pw, B_c2 = Buf(), Buf(), Buf(), Buf(), Buf()
                B_q = [Buf() for _ in range(2)]
                B_u = [Buf() for _ in range(2)]
                B_tmp, B_etmp, B_pooled, B_sqp = Buf(), Buf(), Buf(), Buf()
                B_et = [Buf() for _ in range(2)]
                B_pt = [Buf() for _ in range(9)]
                B_den = [Buf() for _ in range(2)]
                B_at = [Buf() for _ in range(2)]
                B_sqa = [Buf() for _ in range(4)]
                B_mixst = [Buf() for _ in range(2)]
                B_sps = [Buf() for _ in range(2)]
                B_ops = [Buf() for _ in range(2)]
                B_dps = [Buf() for _ in range(2)]
                B_pwps, B_ss = Buf(), Buf()
                d_c2, d_q, d_u, d_ms = cx.dsem("c2"), [cx.dsem("q0"), cx.dsem("q1")], [cx.dsem("u0"), cx.dsem("u1")], [cx.dsem("ms0"), cx.dsem("ms1")]
                d_pw = cx.dsem("pw")
                cx.op(SP, lambda: nsp.dma_start(out=eb[:], in_=biastab_d.ap().rearrange("j p c -> p j c")), writes=[B_eb], dma=d_c2)
                cx.op(SP, lambda: nsp.dma_start(out=mk[:], in_=mask01_d.ap().rearrange("j p c -> p j c")), writes=[B_mk], dma=d_c2)
                cx.op(SP, lambda: nsp.dma_start(out=sinkexp[:], in_=sinkbc_d[:, :]), writes=[B_sk], dma=d_c2)
                cx.op(SP, lambda: nsp.dma_start(out=pscale[:], in_=pscale_d[:, :]), writes=[B_c2], dma=d_c2)
                cx.op(SP, lambda: nsp.dma_start(out=edgef[:], in_=edgef_d[:, :, :]), writes=[B_c2], dma=d_c2)
                cx.op(P, lambda: npl.dma_start(out=pw_sb[:], in_=pool_w_d.ap().rearrange("g (kc p) d -> p g kc d", p=128)),
                      writes=[B_pw], dma=d_pw)
                cx.op(V, lambda: nv.memset(ones_bf[:], 1.0), writes=[B_c2])
                cx.op(A, lambda: na.activation(out=sinkexp[:], in_=sinkexp[:], func=AF.Exp), reads=[B_sk], writes=[B_sk])
                for jk in range(6):
                    cx.op(V, lambda: nv.scalar_tensor_tensor(out=Bs[:, jk, :], in0=eb[:, jk, :], scalar=1.0 / (128.0 ** -0.5), in1=mk[:, jk % 3, :],
                                                             op0=ALU.mult, op1=ALU.add),
                          reads=[B_eb, B_mk], writes=[B_Bs])
                cx.op(V, lambda: nv.memset(uT[0][:, :, 0:8], 0.0), writes=[B_u[0]])
                SCALE = 128.0 ** -0.5
                ctr = {"sp": 0, "pt": 0, "bk": 0, "sq": 0}
                NPT = len(pt)

                def emit_loads(g):
                    gp = g % 2
                    t0 = g * 512
                    cx.op(SP, lambda: nsp.dma_start(out=qT[gp][:], in_=qu_pm[:, 8:16, t0:t0 + 512]),
                          reads=[B_qu], writes=[B_q[gp]], dma=d_q[gp])
                    if g == 0:
                        cx.op(SP, lambda: nsp.dma_start(out=uT[gp][:, :, 8:528], in_=qu_pm[:, 0:8, 0:520]),
                              reads=[B_qu], writes=[B_u[gp]], dma=d_u[gp])
                    elif g == NG - 1:
                        cx.op(V, lambda: nv.memset(uT[gp][:, :, 520:528], 0.0), writes=[B_u[gp]])
                        cx.op(SP, lambda: nsp.dma_start(out=uT[gp][:, :, 0:520], in_=qu_pm[:, 0:8, t0 - 8:S]),
                              reads=[B_qu], writes=[B_u[gp]], dma=d_u[gp])
                    else:
                        cx.op(SP, lambda: nsp.dma_start(out=uT[gp][:], in_=qu_pm[:, 0:8, t0 - 8:t0 + 520]),
                              reads=[B_qu], writes=[B_u[gp]], dma=d_u[gp])

                def stage_A(g, i, j):
                    gp = g % 2
                    Bk = 4 * g + i
                    pts = []
                    for kb in range(3):
                        KB = Bk + kb - 1
                        if KB < 0 or KB >= NT:
                            continue
                        sb_ = ctr["sp"] % 2
                        ctr["sp"] += 1
                        cx.op(PE, lambda: nt.matmul(sps[sb_][:], lhsT=kT_all[:, j, KB * 128:(KB + 1) * 128],
                                                    rhs=qT[gp][:, 4 * j:4 * j + 4, i * 128:(i + 1) * 128], start=True, stop=False),
                              reads=[B_kT[KB // 4], B_q[gp]], writes=[B_sps[sb_]])
                        cx.op(PE, lambda: nt.matmul(sps[sb_][:], lhsT=ident_bf[:], rhs=Bs[:, j * 3 + kb, :], start=False, stop=True),
                              reads=[B_const, B_Bs], writes=[B_sps[sb_]])
                        pb_ = ctr["pt"] % NPT
                        ctr["pt"] += 1
                        cx.op(A, lambda: na.activation(out=pt[pb_][:], in_=sps[sb_][:], func=AF.Exp, scale=SCALE),
                              reads=[B_sps[sb_]], writes=[B_pt[pb_]])
                        pts.append((pb_, KB))
                    return pts

                def stage_B(g, i, j, pts):
                    gp = g % 2
                    ob = ctr["bk"] % 2
                    ctr["bk"] += 1
                    for n, (pb_, KB) in enumerate(pts):
                        cx.op(PE, lambda: nt.matmul(o_ps[ob][:], lhsT=V_all[:, KB, j * 128:(j + 1) * 128], rhs=pt[pb_][:],
                                                    start=(n == 0), stop=(n == len(pts) - 1)),
                              reads=[B_V[KB // 4], B_pt[pb_]], writes=[B_ops[ob]])
                    for n, (pb_, KB) in enumerate(pts):
                        cx.op(PE, lambda: nt.matmul(d_ps[ob][:], lhsT=ones_bf[:], rhs=pt[pb_][:],
                                                    start=(n == 0), stop=(n == len(pts) - 1)),
                              reads=[B_c2, B_pt[pb_]], writes=[B_dps[ob]])
                    cx.op(V, lambda: nv.tensor_tensor(out=den[ob][:], in0=d_ps[ob][:], in1=sinkexp[:, j * 512:(j + 1) * 512], op=ALU.add),
                          reads=[B_dps[ob], B_sk], writes=[B_den[ob]])
                    cx.op(V, lambda: nv.reciprocal(out=den[ob][:], in_=den[ob][:]), reads=[B_den[ob]], writes=[B_den[ob]])
                    cx.op(V, lambda: nv.tensor_tensor(out=at[ob][:], in0=o_ps[ob][:],
                                                      in1=den[ob][:].rearrange("p (a b) -> p a b", b=128), op=ALU.mult),
                          reads=[B_ops[ob], B_den[ob]], writes=[B_at[ob]])
                    sb2 = ctr["sq"] % 4
                    ctr["sq"] += 1
                    cx.op(A, lambda: na.activation(out=sqa[sb2][:], in_=at[ob][:], func=AF.Square),
                          reads=[B_at[ob]], writes=[B_sqa[sb2]])
                    cx.op(V, lambda: nv.tensor_copy(out=mixst[gp][:, 8 + 4 * j:8 + 4 * j + 4, i * 128:(i + 1) * 128], in_=at[ob][:]),
                          reads=[B_at[ob]], writes=[B_mixst[gp]])
                    return sb2

                def stage_C(i, sq_pair):
                    n = 0
                    for sb2 in sq_pair:
                        for gq in range(4):
                            cx.op(PE, lambda: nt.matmul(ss_ps[:, i:i + 1], lhsT=sqa[sb2][:, gq, :], rhs=ones_bf[:, 0:1],
                                                        start=(n == 0), stop=(n == 7)),
                                  reads=[B_sqa[sb2], B_c2], writes=[B_ss])
                            n += 1

                def pooling(g, grp):
                    gp = g % 2
                    w = (2, 4, 8, 16)[grp]
                    U = uT[gp][:, 2 * grp:2 * grp + 2, :]
                    cx.op(V, lambda: nv.tensor_tensor(out=tmp[0][:, :, 1:528], in0=U[:, :, 0:527], in1=U[:, :, 1:528], op=ALU.add),
                          reads=[B_u[gp]], writes=[B_tmp])
                    if grp >= 1:
                        cx.op(V, lambda: nv.tensor_tensor(out=tmp[1][:, :, 2:527], in0=tmp[0][:, :, 1:526], in1=tmp[0][:, :, 3:528], op=ALU.add),
                              reads=[B_tmp], writes=[B_tmp])
                    if grp >= 2:
                        cx.op(V, lambda: nv.tensor_tensor(out=tmp[2][:, :, 4:525], in0=tmp[1][:, :, 2:523], in1=tmp[1][:, :, 6:527], op=ALU.add),
                              reads=[B_tmp], writes=[B_tmp])
                    if grp >= 3:
                        cx.op(V, lambda: nv.tensor_tensor(out=tmp[3][:, :, 8:521], in0=tmp[2][:, :, 4:517], in1=tmp[2][:, :, 12:525], op=ALU.add),
                              reads=[B_tmp], writes=[B_tmp])
                    sw = tmp[grp]
                    cx.op(V, lambda: nv.scalar_tensor_tensor(out=pooledT[:, 2 * grp:2 * grp + 2, :], in0=sw[:, :, 8:520], scalar=1.0 / w,
                                                             in1=U[:, :, 8:520], op0=ALU.mult, op1=ALU.subtract),
                          reads=[B_tmp, B_u[gp]], writes=[B_pooled])
                    edges = []
                    if g == 0:
                        edges.append((8, 0, 0))
                    if g == NG - 1:
                        edges.append((512, 504, 8))
                    for (uc, pc, ec) in edges:
                        for ch in range(2):
                            cx.op(V, lambda: nv.tensor_tensor(out=etmp[:, ch, :], in0=sw[:, ch, uc:uc + 8], in1=edgef[:, grp, ec:ec + 8], op=ALU.mult),
                                  reads=[B_tmp, B_c2], writes=[B_etmp])
                            cx.op(V, lambda: nv.tensor_tensor(out=pooledT[:, 2 * grp + ch, pc:pc + 8], in0=etmp[:, ch, :],
                                                              in1=uT[gp][:, 2 * grp + ch, uc:uc + 8], op=ALU.subtract),
                                  reads=[B_etmp, B_u[gp]], writes=[B_pooled])

                def group_end(g):
                    gp = g % 2
                    t0 = g * 512
                    for oc in range(8):
                        grp, hf = oc // 2, oc % 2
                        for kc in range(2):
                            cx.op(PE, lambda: nt.matmul(pw_ps[:], lhsT=pw_sb[:, grp, kc, hf * 128:(hf + 1) * 128], rhs=pooledT[:, 2 * grp + kc, :],
                                                        start=(kc == 0), stop=(kc == 1)),
                                  reads=[B_pw, B_pooled], writes=[B_pwps])
                        cx.op(A, lambda: na.activation(out=mixst[gp][:, oc, :], in_=pw_ps[:], func=AF.Copy, scale=pscale[:, oc:oc + 1]),
                              reads=[B_pwps, B_c2], writes=[B_mixst[gp]])
                        cx.op(A, lambda: na.activation(out=sqp[:, oc, :], in_=pw_ps[:], func=AF.Square, scale=pscale[:, oc:oc + 1]),
                              reads=[B_pwps, B_c2], writes=[B_sqp])
                    for i in range(4):
                        for oc in range(8):
                            cx.op(PE, lambda: nt.matmul(ss_ps[:, 4 + i:5 + i], lhsT=sqp[:, oc, i * 128:(i + 1) * 128], rhs=ones_bf[:, 0:1],
                                                        start=(oc == 0), stop=(oc == 7)),
                                  reads=[B_sqp, B_c2], writes=[B_ss])
                    rstd_from_ssq(ss_ps[:, 0:4], rstd_pa[:, 32 + 4 * g:32 + 4 * g + 4], 1024, [B_ss], [B_rstd])
                    rstd_from_ssq(ss_ps[:, 4:8], rstd_pa[:, 4 * g:4 * g + 4], 1024, [B_ss], [B_rstd])
                    cx.op(SP, lambda: nsp.dma_start(out=mix_pm[:, :, t0:t0 + 512], in_=mixst[gp][:]),
                          reads=[B_mixst[gp]], writes=[B_mix], dma=d_ms[gp])

                units = [(g, i, j) for g in range(NG) for i in range(4) for j in range(2)]
                sq_of = {}
                pend = []

                def do_B(u, pts):
                    g, i, j = u
                    sb2 = stage_B(g, i, j, pts)
                    sq_of.setdefault((g, i), []).append(sb2)
                    for fn in pend:
                        fn()
                    del pend[:]
                    if j == 0:
                        pooling(g, i)
                    if j == 1:
                        pair = sq_of.pop((g, i))
                        pend.append(lambda: stage_C(i, pair))
                        if i == 3:
                            pend.append(lambda: group_end(g))

                prev = None
                for u in units:
                    g, i, j = u
                    if i == 0 and j == 0:
                        emit_loads(g)
                    pts = stage_A(g, i, j)
                    if prev is not None:
                        do_B(*prev)
                    prev = (u, pts)
                do_B(*prev)
                for fn in pend:
                    fn()
                del pend[:]
                if debug == 2:
                    d_dbg = cx.dsem("dbg2")
                    cx.op(SP, lambda: nsp.dma_start(out=dbg_rstd[:, :], in_=rstd_pa[:]), reads=[B_rstd], writes=[B_out], dma=d_dbg)
                cx.barrier()
        if debug == 2:
            cx.barrier()
            return nc

        s3 = es.enter_context(ExitStack())
        posT = sbuf(s3, "posT", [128, NT, NE], F32)
        gaT = sbuf(s3, "gaT", [128, NT, NE], F32)
        vals = sbuf(s3, "vals", [128, NT, NE, 5], BF16)
        iota512 = sbuf(s3, "iota512", [128, 512], F32)
        B_posT, B_gaT, B_vals, B_iota = Buf(), Buf(), Buf(), Buf()
        with ExitStack() as s2:
            affT = sbuf(s2, "affT", [NE, S], F32)
            B_aff = Buf("aff")
            with ExitStack() as ph:
                w_out_sb = sbuf(ph, "w_out_sb", [128, 16, D], BF16)
                wstage = sbuf(ph, "wstage", [128, D], F32)
                gnfm = sbuf(ph, "gnfm", [128, 16], F32)
                wr_sb = sbuf(ph, "wr_sb", [128, 16, NE], F32)
                wr_hi = sbuf(ph, "wr_hi", [128, 16, NE], BF16)
                wr_lo = sbuf(ph, "wr_lo", [128, 16, NE], BF16)
                h2lo = sbuf(ph, "h2lo", [128, D], BF16)
                hiT = sbuf(ph, "hiT", [128, 16, 128], BF16)
                loT = sbuf(ph, "loT", [128, 16, 128], BF16)
                B_h2lo, B_hiT, B_loT = Buf(), Buf(), Buf()
                g2bc = sbuf(ph, "g2bc", [128, D], F32)
                mixld = [sbuf(ph, "mixld%d" % i, [128, 16, 512], BF16) for i in range(2)]
                xt = [sbuf(ph, "xt%d" % i, [128, D], F32) for i in range(2)]
                x1t = [sbuf(ph, "x1t%d" % i, [128, D], F32) for i in range(2)]
                h2f = sbuf(ph, "h2f", [128, D], F32)
                h2b = [sbuf(ph, "h2b%d" % i, [128, D], BF16) for i in range(2)]
                ex = sbuf(ph, "ex", [NE, 512], F32)
                rc = sbuf(ph, "rc", [NE, 512], F32)
                ones16 = sbuf(ph, "ones16", [NE, NE], F32)
                ssq2 = sbuf(ph, "ssq2", [128, NT], F32)
                rs2 = sbuf(ph, "rs2", [128, NT], F32)
                P1 = [psum(ph, "P1_%d" % i, [128, 512], F32) for i in range(2)]
                P2 = [psum(ph, "P2_%d" % i, [128, 512], F32) for i in range(2)]
                tpf = [psum(ph, "tpf%d" % i, [128, 8, 128], BF16) for i in range(2)]
                lg_ps = psum(ph, "lgps", [NE, 512], F32)
                sm_ps = psum(ph, "smps", [NE, 512], F32)
                B_wo, B_ws, B_c3, B_g2 = Buf(), Buf(), Buf(), Buf()
                B_ml = [Buf() for _ in range(2)]
                B_xt = [Buf() for _ in range(2)]
                B_x1 = [Buf() for _ in range(2)]
                B_h2f = Buf()
                B_h2b = [Buf() for _ in range(2)]
                B_h2T, B_ex, B_rc, B_ssq2, B_rs2 = Buf(), Buf(), Buf(), Buf(), Buf()
                B_P1 = [Buf() for _ in range(2)]
                B_P2 = [Buf() for _ in range(2)]
                B_tpf = [Buf() for _ in range(2)]
                B_lg, B_sm = Buf(), Buf()
                d_c3, d_ws = cx.dsem("c3"), cx.dsem("ws")
                d_ml, d_xt = [cx.dsem("ml0"), cx.dsem("ml1")], [cx.dsem("xt0"), cx.dsem("xt1")]
                d_x1, d_h2 = [cx.dsem("x1s0"), cx.dsem("x1s1")], [cx.dsem("h2s0"), cx.dsem("h2s1")]
                cx.op(SP, lambda: nsp.dma_start(out=gnfm[:], in_=gnfm_d[:, :]), writes=[B_c3], dma=d_c3)
                cx.op(SP, lambda: nsp.dma_start(out=g2bc[:], in_=g2bc_d[:, :]), writes=[B_g2], dma=d_c3)
                cx.op(SP, lambda: nsp.dma_start(out=wr_sb[:], in_=w_router_d.ap().rearrange("(kc p) e -> p kc e", p=128)),
                      writes=[B_c3], dma=d_c3)
                cx.op(V, lambda: nv.memset(ones16[:], 1.0), writes=[B_c3])
                cx.op(V, lambda: nv.memset(ssq2[:], 0.0), writes=[B_ssq2])
                cx.op(V, lambda: nv.tensor_copy(out=wr_hi[:], in_=wr_sb[:]), reads=[B_c3], writes=[B_c3])
                cx.op(V, lambda: nv.tensor_tensor(out=wr_sb[:], in0=wr_sb[:], in1=wr_hi[:], op=ALU.subtract), reads=[B_c3], writes=[B_c3])
                cx.op(V, lambda: nv.tensor_copy(out=wr_lo[:], in_=wr_sb[:]), reads=[B_c3], writes=[B_c3])
                for kc in range(16):
                    cx.op(SP, lambda: nsp.dma_start(out=wstage[:], in_=w_out_d[kc * 128:(kc + 1) * 128, :]), writes=[B_ws], dma=d_ws)
                    cx.op(V, lambda: nv.tensor_scalar(out=w_out_sb[:, kc, :], in0=wstage[:], scalar1=gnfm[:, kc:kc + 1], scalar2=None, op0=ALU.mult),
                          reads=[B_ws, B_c3], writes=[B_wo])
                pbi = 0
                tfi = 0
                for g in range(NG):
                    gp = g % 2
                    t0 = g * 512
                    cx.op(SP, lambda: nsp.dma_start(out=mixld[gp][:], in_=mix_pm[:, :, t0:t0 + 512]),
                          reads=[B_mix], writes=[B_ml[gp]], dma=d_ml[gp])
                    for i in range(4):
                        T = 4 * g + i
                        tq = T % 2
                        cx.op(SP, lambda: nsp.dma_start(out=xt[tq][:], in_=x_d[T * 128:(T + 1) * 128, :]), writes=[B_xt[tq]], dma=d_xt[tq])
                        for ds in range(4):
                            pb = pbi % 2
                            pbi += 1
                            dsl = slice(ds * 512, (ds + 1) * 512)
                            for kc in range(8):
                                cx.op(PE, lambda: nt.matmul(P1[pb][:], lhsT=mixld[gp][:, kc, i * 128:(i + 1) * 128], rhs=w_out_sb[:, kc, dsl],
                                                            start=(kc == 0), stop=(kc == 7)),
                                      reads=[B_ml[gp], B_wo], writes=[B_P1[pb]])
                            for kc in range(8, 16):
                                cx.op(PE, lambda: nt.matmul(P2[pb][:], lhsT=mixld[gp][:, kc, i * 128:(i + 1) * 128], rhs=w_out_sb[:, kc, dsl],
                                                            start=(kc == 8), stop=(kc == 15)),
                                      reads=[B_ml[gp], B_wo], writes=[B_P2[pb]])
                            cx.op(V, lambda: nv.scalar_tensor_tensor(out=x1t[tq][:, dsl], in0=P1[pb][:], scalar=rstd_pa[:, T:T + 1],
                                                                     in1=xt[tq][:, dsl], op0=ALU.mult, op1=ALU.add),
                                  reads=[B_P1[pb], B_rstd, B_xt[tq]], writes=[B_x1[tq]])
                            cx.op(V, lambda: nv.scalar_tensor_tensor(out=x1t[tq][:, dsl], in0=P2[pb][:], scalar=rstd_pa[:, 32 + T:33 + T],
                                                                     in1=x1t[tq][:, dsl], op0=ALU.mult, op1=ALU.add),
                                  reads=[B_P2[pb], B_rstd, B_x1[tq]], writes=[B_x1[tq]])
                        cx.op(SP, lambda: nsp.dma_start(out=acc_d[T * 128:(T + 1) * 128, :], in_=x1t[tq][:]),
                              reads=[B_x1[tq]], writes=[B_acc], dma=d_x1[tq])
                        cx.op(A, lambda: na.activation(out=h2f[:], in_=x1t[tq][:], func=AF.Square, accum_out=ssq2[:, T:T + 1]),
                              reads=[B_x1[tq]], writes=[B_h2f, B_ssq2])
                        rstd_from_ssq(ssq2[:, T:T + 1], rs2[:, T:T + 1], D, [B_ssq2], [B_rs2])
                        cx.op(V, lambda: nv.scalar_tensor_tensor(out=h2f[:], in0=x1t[tq][:], scalar=rs2[:, T:T + 1], in1=g2bc[:],
                                                                 op0=ALU.mult, op1=ALU.mult),
                              reads=[B_x1[tq], B_rs2, B_g2], writes=[B_h2f])
                        cx.op(A, lambda: na.copy(out=h2b[tq][:], in_=h2f[:]), reads=[B_h2f], writes=[B_h2b[tq]])
                        cx.op(SP, lambda: nsp.dma_start(out=h2_scr[T * 128:(T + 1) * 128, :], in_=h2b[tq][:]),
                              reads=[B_h2b[tq]], writes=[B_h2], dma=d_h2[tq])
                        cx.op(V, lambda: nv.tensor_tensor(out=h2lo[:], in0=h2f[:], in1=h2b[tq][:], op=ALU.subtract),
                              reads=[B_h2f, B_h2b[tq]], writes=[B_h2lo])
                        for (src, srcB, dstT, dstB) in ((h2b[tq], B_h2b[tq], hiT, B_hiT), (h2lo, B_h2lo, loT, B_loT)):
                            for half in range(2):
                                tb = tfi % 2
                                tfi += 1
                                for k in range(8):
                                    kc = half * 8 + k
                                    cx.op(PE, lambda: nt.transpose(out=tpf[tb][:, k, :], in_=src[:, kc * 128:(kc + 1) * 128], identity=ident_bf[:]),
                                          reads=[srcB, B_const], writes=[B_tpf[tb]])
                                if half == 0:
                                    cx.op(A, lambda: na.copy(out=dstT[:, half * 8:half * 8 + 8, :], in_=tpf[tb][:]), reads=[B_tpf[tb]], writes=[dstB])
                                else:
                                    cx.op(V, lambda: nv.tensor_copy(out=dstT[:, half * 8:half * 8 + 8, :], in_=tpf[tb][:]), reads=[B_tpf[tb]], writes=[dstB])
                        n = 0
                        for (wsb, rT, rB) in ((wr_hi, hiT, B_hiT), (wr_hi, loT, B_loT), (wr_lo, hiT, B_hiT)):
                            for kc in range(16):
                                cx.op(PE, lambda: nt.matmul(lg_ps[:, i * 128:(i + 1) * 128], lhsT=wsb[:, kc, :], rhs=rT[:, kc, :],
                                                            start=(n == 0), stop=(n == 47)),
                                      reads=[B_c3, rB], writes=[B_lg])
                                n += 1
                    cx.op(A, lambda: na.activation(out=ex[:], in_=lg_ps[:], func=AF.Exp), reads=[B_lg], writes=[B_ex])
                    cx.op(PE, lambda: nt.matmul(sm_ps[:], lhsT=ones16[:], rhs=ex[:], start=True, stop=True),
                          reads=[B_c3, B_ex], writes=[B_sm])
                    cx.op(V, lambda: nv.reciprocal(out=rc[:], in_=sm_ps[:]), reads=[B_sm], writes=[B_rc])
                    cx.op(V, lambda: nv.tensor_tensor(out=affT[:, t0:t0 + 512], in0=ex[:], in1=rc[:], op=ALU.mult),
                          reads=[B_ex, B_rc], writes=[B_aff])
                if debug in (3, 4):
                    d_dbg = cx.dsem("dbg3")
                    cx.op(SP, lambda: nsp.dma_start(out=dbg_aff[:, :], in_=affT[:]), reads=[B_aff], writes=[B_out], dma=d_dbg)
                cx.barrier()
            if debug == 3:
                cx.barrier()
                return nc

            with ExitStack() as ph:
                lo = sbuf(ph, "lo", [NE, 1], F32)
                hi = sbuf(ph, "hi", [NE, 1], F32)
                mid = sbuf(ph, "mid", [NE, 1], F32)
                cnt = sbuf(ph, "cnt", [NE, 1], F32)
                sel = sbuf(ph, "sel", [NE, 1], F32)
                nsel = sbuf(ph, "nsel", [NE, 1], F32)
                ta = sbuf(ph, "ta", [NE, 1], F32)
                tb_ = sbuf(ph, "tb", [NE, 1], F32)
                junk = sbuf(ph, "junk", [NE, S], F32)
                msk = sbuf(ph, "msk", [NE, S], F32)
                cs = sbuf(ph, "cs", [NE, S], F32)
                ones_s = sbuf(ph, "ones_s", [NE, S], F32)
                tokab = sbuf(ph, "tokab", [128, NT, 2], F32)
                r1 = sbuf(ph, "r1", [128, NT, NE], F32)
                pT_ps = psum(ph, "pTps", [128, NT, NE], F32)
                gT_ps = psum(ph, "gTps", [128, NT, NE], F32)
                B_b, B_junk, B_msk, B_cs, B_ones, B_tok, B_r1, B_pTps, B_gTps = [Buf() for _ in range(9)]
                d_c4 = cx.dsem("c4")
                cx.op(SP, lambda: nsp.dma_start(out=iota512[:], in_=iota512_d[:, :]), writes=[B_iota], dma=d_c4)
                cx.op(SP, lambda: nsp.dma_start(out=tokab[:], in_=tokab_d[:, :, :]), writes=[B_tok], dma=d_c4)
                cx.op(V, lambda: nv.memset(lo[:], 0.0), writes=[B_b])
                cx.op(V, lambda: nv.memset(hi[:], 1.0), writes=[B_b])
                cx.op(P, lambda: npl.memset(ones_s[:], 1.0), writes=[B_ones])
                for it in range(NBISECT):
                    cx.op(V, lambda: nv.tensor_scalar(out=mid[:], in0=lo[:], scalar1=hi[:, 0:1], scalar2=0.5, op0=ALU.add, op1=ALU.mult),
                          reads=[B_b], writes=[B_b])
                    cx.op(V, lambda: nv.memset(cnt[:], 0.0), reads=[B_b], writes=[B_b])
                    cx.op(V, lambda: nv.tensor_scalar(out=junk[:], in0=affT[:], scalar1=mid[:, 0:1], scalar2=0.0, op0=ALU.is_ge, op1=ALU.add,
                                                      accum_out=cnt[:]),
                          reads=[B_aff, B_b], writes=[B_junk, B_b])
                    cx.op(V, lambda: nv.tensor_scalar(out=sel[:], in0=cnt[:], scalar1=float(CAP), scalar2=None, op0=ALU.is_ge),
                          reads=[B_b], writes=[B_b])
                    cx.op(V, lambda: nv.tensor_scalar(out=nsel[:], in0=sel[:], scalar1=-1.0, scalar2=1.0, op0=ALU.mult, op1=ALU.add),
                          reads=[B_b], writes=[B_b])
                    cx.op(V, lambda: nv.tensor_scalar(out=ta[:], in0=mid[:], scalar1=nsel[:, 0:1], scalar2=None, op0=ALU.mult),
                          reads=[B_b], writes=[B_b])
                    cx.op(V, lambda: nv.scalar_tensor_tensor(out=hi[:], in0=hi[:], scalar=sel[:, 0:1], in1=ta[:], op0=ALU.mult, op1=ALU.add),
                          reads=[B_b], writes=[B_b])
                    cx.op(V, lambda: nv.tensor_scalar(out=tb_[:], in0=lo[:], scalar1=nsel[:, 0:1], scalar2=None, op0=ALU.mult),
                          reads=[B_b], writes=[B_b])
                    cx.op(V, lambda: nv.scalar_tensor_tensor(out=lo[:], in0=mid[:], scalar=sel[:, 0:1], in1=tb_[:], op0=ALU.mult, op1=ALU.add),
                          reads=[B_b], writes=[B_b])
                cx.op(V, lambda: nv.tensor_scalar(out=msk[:], in0=affT[:], scalar1=lo[:, 0:1], scalar2=None, op0=ALU.is_ge),
                      reads=[B_aff, B_b], writes=[B_msk])
                cx.op(V, lambda: nv.tensor_tensor_scan(out=cs[:], data0=ones_s[:], data1=msk[:], initial=0.0, op0=ALU.mult, op1=ALU.add),
                      reads=[B_ones, B_msk], writes=[B_cs])
                cx.op(V, lambda: nv.tensor_tensor(out=cs[:], in0=cs[:], in1=msk[:], op=ALU.mult), reads=[B_cs, B_msk], writes=[B_cs])
                cx.op(V, lambda: nv.tensor_scalar(out=cs[:], in0=cs[:], scalar1=-1.0, scalar2=None, op0=ALU.add), reads=[B_cs], writes=[B_cs])
                for jt in range(NT):
                    cx.op(PE, lambda: nt.transpose(out=pT_ps[:, jt, :], in_=cs[:, jt * 128:(jt + 1) * 128], identity=ident_f[0:NE, 0:NE]),
                          reads=[B_cs, B_const], writes=[B_pTps])
                for jt in range(NT):
                    cx.op(PE, lambda: nt.transpose(out=gT_ps[:, jt, :], in_=affT[:, jt * 128:(jt + 1) * 128], identity=ident_f[0:NE, 0:NE]),
                          reads=[B_aff, B_const], writes=[B_gTps])
                cx.op(V, lambda: nv.tensor_copy(out=posT[:], in_=pT_ps[:]), reads=[B_pTps], writes=[B_posT])
                cx.op(A, lambda: na.copy(out=gaT[:], in_=gT_ps[:]), reads=[B_gTps], writes=[B_gaT])
                cx.op(V, lambda: nv.tensor_copy(out=vals[:, :, :, 2], in_=gaT[:]), reads=[B_gaT], writes=[B_vals])
                cx.op(V, lambda: nv.tensor_tensor(out=r1[:], in0=gaT[:], in1=vals[:, :, :, 2], op=ALU.subtract), reads=[B_gaT, B_vals], writes=[B_r1])
                cx.op(V, lambda: nv.tensor_copy(out=vals[:, :, :, 3], in_=r1[:]), reads=[B_r1], writes=[B_vals])
                cx.op(V, lambda: nv.tensor_tensor(out=r1[:], in0=r1[:], in1=vals[:, :, :, 3], op=ALU.subtract), reads=[B_r1, B_vals], writes=[B_r1])
                cx.op(V, lambda: nv.tensor_copy(out=vals[:, :, :, 4], in_=r1[:]), reads=[B_r1], writes=[B_vals])
                for e in range(NE):
                    cx.op(P, lambda: npl.tensor_copy(out=vals[:, :, e, 0:2], in_=tokab[:]), reads=[B_tok], writes=[B_vals])
                if debug == 4:
                    d_dbg = cx.dsem("dbg4")
                    cx.op(SP, lambda: nsp.dma_start(out=dbg_thr[0, :, :], in_=lo[:]), reads=[B_b], writes=[B_out], dma=d_dbg)
                    cx.op(SP, lambda: nsp.dma_start(out=dbg_thr[1, :, :], in_=hi[:]), reads=[B_b], writes=[B_out], dma=d_dbg)
                cx.barrier()
        s4 = es.enter_context(ExitStack())
        oh = [sbuf(s4, "oh%d" % i, [128, 512], BF16) for i in range(2)]
        cmp_sb = sbuf(s4, "cmp_sb", [128, 4, 5], F32)
        idxf = [sbuf(s4, "idxf%d" % i, [128, 4], F32) for i in range(3)]
        idxi = [sbuf(s4, "idxi%d" % i, [128, 4], I32) for i in range(3)]
        gts = [sbuf(s4, "gts%d" % i, [128, 4], F32) for i in range(3)]
        idxqf = [sbuf(s4, "idxqf%d" % i, [128, 4, 4], F32) for i in range(3)]
        idxqi = [sbuf(s4, "idxqi%d" % i, [128, 4, 4], I32) for i in range(3)]
        cmp_ps = psum(s4, "cmpps", [128, 4, 5], F32)
        B_oh = [Buf() for _ in range(2)]
        B_cmps, B_cmpp = Buf(), Buf()
        B_idx = [Buf() for _ in range(3)]
        ohc = [0]

        def compact(e):
            ep = e % 3
            for jt in range(NT):
                ob = ohc[0] % 2
                ohc[0] += 1
                cx.op(V, lambda: nv.tensor_scalar(out=oh[ob][:], in0=iota512[:], scalar1=posT[:, jt, e:e + 1], scalar2=None, op0=ALU.is_equal),
                      reads=[B_iota, B_posT], writes=[B_oh[ob]])
                for cc in range(4):
                    cx.op(PE, lambda: nt.matmul(cmp_ps[:, cc, :], lhsT=oh[ob][:, cc * 128:(cc + 1) * 128], rhs=vals[:, jt, e, :],
                                                start=(jt == 0 and cc == 0), stop=(jt == NT - 1)),
                          reads=[B_oh[ob], B_vals], writes=[B_cmpp])
            cx.op(A, lambda: na.copy(out=cmp_sb[:], in_=cmp_ps[:]), reads=[B_cmpp], writes=[B_cmps])
            cx.op(V, lambda: nv.scalar_tensor_tensor(out=idxf[ep][:], in0=cmp_sb[:, :, 0], scalar=64.0, in1=cmp_sb[:, :, 1],
                                                     op0=ALU.mult, op1=ALU.add), reads=[B_cmps], writes=[B_idx[ep]])
            cx.op(V, lambda: nv.tensor_copy(out=idxi[ep][:], in_=idxf[ep][:]), reads=[B_idx[ep]], writes=[B_idx[ep]])
            for dsq in range(4):
                cx.op(V, lambda: nv.tensor_scalar(out=idxqf[ep][:, dsq, :], in0=idxf[ep][:], scalar1=4.0, scalar2=float(dsq), op0=ALU.mult, op1=ALU.add),
                      reads=[B_idx[ep]], writes=[B_idx[ep]])
            cx.op(V, lambda: nv.tensor_copy(out=idxqi[ep][:], in_=idxqf[ep][:]), reads=[B_idx[ep]], writes=[B_idx[ep]])
            cx.op(V, lambda: nv.tensor_tensor(out=gts[ep][:], in0=cmp_sb[:, :, 2], in1=cmp_sb[:, :, 3], op=ALU.add),
                  reads=[B_cmps], writes=[B_idx[ep]])
            cx.op(V, lambda: nv.tensor_tensor(out=gts[ep][:], in0=gts[ep][:], in1=cmp_sb[:, :, 4], op=ALU.add),
                  reads=[B_cmps, B_idx[ep]], writes=[B_idx[ep]])

        if debug == 4:
            d_dbg5 = cx.dsem("dbg5")
            for e in range(NE):
                compact(e)
                cx.op(SP, lambda: nsp.dma_start(out=dbg_idx[e, 0, :, :], in_=idxf[e % 3][:]), reads=[B_idx[e % 3]], writes=[B_out], dma=d_dbg5)
                cx.op(SP, lambda: nsp.dma_start(out=dbg_idx[e, 1, :, :], in_=gts[e % 3][:]), reads=[B_idx[e % 3]], writes=[B_out], dma=d_dbg5)
            cx.barrier()
            return nc

        with ExitStack() as ph:
            ring = [sbuf(ph, "ring%d" % i, [128, 16, 512], BF16) for i in range(NSLOT)]
            xs = sbuf(ph, "xs", [128, 4, D], BF16)
            xsT = sbuf(ph, "xsT", [128, 16, 512], BF16)
            gT = sbuf(ph, "gT", [128, 32, 512], BF16)
            sa = [sbuf(ph, "sa%d" % i, [128, 512], F32) for i in range(2)]
            ystage = [sbuf(ph, "ystage%d" % i, [128, 4, 512], F32) for i in range(2)]
            a_ps = [psum(ph, "aps%d" % i, [128, 512], F32) for i in range(2)]
            u_ps = [psum(ph, "ups%d" % i, [128, 512], F32) for i in range(2)]
            y_ps = [psum(ph, "yps%d" % i, [128, 512], F32) for i in range(2)]
            tp3 = psum(ph, "tp3", [128, 8, 128], BF16)
            B_ring = [Buf() for _ in range(NSLOT)]
            d_ring = [cx.dsem("rg%d" % i) for i in range(NSLOT)]
            B_xs, B_xsT, B_gT, B_tp3 = Buf(), Buf(), Buf(), Buf()
            B_ys = [Buf() for _ in range(2)]
            B_sa = [Buf() for _ in range(2)]
            B_aps = [Buf() for _ in range(2)]
            B_ups = [Buf() for _ in range(2)]
            B_yps = [Buf() for _ in range(2)]
            B_sc = [[Buf() for _ in range(16)] for _ in range(2)]
            d_xs = cx.dsem("xs")
            d_sc = [cx.dsem("sc%d" % i) for i in range(4)]
            wg_v = [w_gate_d[e].rearrange("(kc p) f -> p kc f", p=128) for e in range(NE)]
            wu_v = [w_up_d[e].rearrange("(kc p) f -> p kc f", p=128) for e in range(NE)]
            wd_v = [w_down_d[e].rearrange("(fc p) d -> p fc d", p=128) for e in range(NE)]
            acc_q = acc_d.ap().rearrange("s (a c) -> (s a) c", c=512)
            loads = []
            for e in range(NE):
                for fg in range(8):
                    loads.append(wg_v[e][:, :, fg * 512:(fg + 1) * 512])
                    loads.append(wu_v[e][:, :, fg * 512:(fg + 1) * 512])
                for ds in range(4):
                    for hf in range(2):
                        loads.append(wd_v[e][:, hf * 16:(hf + 1) * 16, ds * 512:(ds + 1) * 512])
            nxt = [0]

            def ensure_loads(upto):
                while nxt[0] <= upto and nxt[0] < len(loads):
                    k = nxt[0]
                    sl = k % NSLOT
                    src = loads[k]
                    cx.op(P, lambda: npl.dma_start(out=ring[sl][:], in_=src), writes=[B_ring[sl]], dma=d_ring[sl])
                    nxt[0] += 1

            def gather(e):
                ep = e % 3
                for cc in range(4):
                    cx.op(P, lambda: npl.indirect_dma_start(out=xs[:, cc, :], out_offset=None, in_=h2_scr[:, :],
                                                            in_offset=bass.IndirectOffsetOnAxis(ap=idxi[ep][:, cc:cc + 1], axis=0)),
                          reads=[B_idx[ep], B_h2], writes=[B_xs], dma=d_xs)

            def transposes(e):
                for cc in range(4):
                    for half in range(2):
                        for k in range(8):
                            kc = half * 8 + k
                            cx.op(PE, lambda: nt.transpose(out=tp3[:, k, :], in_=xs[:, cc, kc * 128:(kc + 1) * 128], identity=ident_bf[:]),
                                  reads=[B_xs, B_const], writes=[B_tp3])
                        dst = xsT[:, half * 8:half * 8 + 8, cc * 128:(cc + 1) * 128]
                        if half == 0:
                            cx.op(A, lambda: na.copy(out=dst, in_=tp3[:]), reads=[B_tp3], writes=[B_xsT])
                        else:
                            cx.op(V, lambda: nv.tensor_copy(out=dst, in_=tp3[:]), reads=[B_tp3], writes=[B_xsT])

            pend_sc = []

            def flush_sc():
                for (e_, ds_) in pend_sc:
                    ep_ = e_ % 3
                    for tt in range(4):
                        cx.op(P, lambda: npl.indirect_dma_start(out=acc_q,
                                                                out_offset=bass.IndirectOffsetOnAxis(ap=idxqi[ep_][:, ds_, tt:tt + 1], axis=0),
                                                                in_=ystage[ds_ % 2][:, tt, :], in_offset=None, compute_op=ALU.add),
                              reads=[B_ys[ds_ % 2], B_idx[ep_], B_acc] + B_sc[(e_ + 1) % 2], writes=[B_sc[e_ % 2][ds_ * 4 + tt]], dma=d_sc[tt])
                del pend_sc[:]

            ensure_loads(NSLOT - 1)
            compact(0)
            gather(0)
            transposes(0)
            compact(1)
            gather(1)
            abi = 0
            ybi = 0
            for e in range(NE):
                ep = e % 3
                sp_ = e % 2
                base = e * 24
                for fg in range(8):
                    kg = base + 2 * fg
                    ku = kg + 1
                    ensure_loads(ku + NSLOT - 2)
                    sg, su = kg % NSLOT, ku % NSLOT
                    if fg == 1:
                        flush_sc()
                    for fl in range(4):
                        fc = fg * 4 + fl
                        ab = abi % 2
                        abi += 1
                        for kc in range(16):
                            cx.op(PE, lambda: nt.matmul(a_ps[ab][:], lhsT=ring[sg][:, kc, fl * 128:(fl + 1) * 128], rhs=xsT[:, kc, :],
                                                        start=(kc == 0), stop=(kc == 15)),
                                  reads=[B_ring[sg], B_xsT], writes=[B_aps[ab]])
                        for kc in range(16):
                            cx.op(PE, lambda: nt.matmul(u_ps[ab][:], lhsT=ring[su][:, kc, fl * 128:(fl + 1) * 128], rhs=xsT[:, kc, :],
                                                        start=(kc == 0), stop=(kc == 15)),
                                  reads=[B_ring[su], B_xsT], writes=[B_ups[ab]])
                        cx.op(A, lambda: na.activation(out=sa[ab][:], in_=a_ps[ab][:], func=AF.Silu), reads=[B_aps[ab]], writes=[B_sa[ab]])
                        cx.op(V, lambda: nv.tensor_tensor(out=gT[:, fc, :], in0=sa[ab][:], in1=u_ps[ab][:], op=ALU.mult),
                              reads=[B_sa[ab], B_ups[ab]], writes=[B_gT])
                for ds in range(4):
                    k0 = base + 16 + 2 * ds
                    k1 = k0 + 1
                    ensure_loads(k1 + NSLOT - 2)
                    s0, s1_ = k0 % NSLOT, k1 % NSLOT
                    for tt in range(4):
                        yb = ybi % 2
                        ybi += 1
                        for fc in range(32):
                            sl = s0 if fc < 16 else s1_
                            cx.op(PE, lambda: nt.matmul(y_ps[yb][:], lhsT=gT[:, fc, tt * 128:(tt + 1) * 128], rhs=ring[sl][:, fc % 16, :],
                                                        start=(fc == 0), stop=(fc == 31)),
                                  reads=[B_gT, B_ring[sl]], writes=[B_yps[yb]])
                        cx.op(A, lambda: na.activation(out=ystage[ds % 2][:, tt, :], in_=y_ps[yb][:], func=AF.Copy,
                                                       scale=gts[ep][:, tt:tt + 1]),
                              reads=[B_yps[yb], B_idx[ep]], writes=[B_ys[ds % 2]])
                    flush_sc()
                    pend_sc.append((e, ds))
                    if ds == 0 and e + 1 < NE:
                        transposes(e + 1)
                        if e + 2 < NE:
                            compact(e + 2)
                if e + 2 < NE:
                    gather(e + 2)
            flush_sc()
            cx.barrier()

        with ExitStack() as ph:
            gfbc = sbuf(ph, "gfbc", [128, D], F32)
            a4 = [sbuf(ph, "a4_%d" % i, [128, D], F32) for i in range(2)]
            o4 = [sbuf(ph, "o4_%d" % i, [128, D], F32) for i in range(2)]
            ssq4 = sbuf(ph, "ssq4", [128, NT], F32)
            rs4 = sbuf(ph, "rs4", [128, NT], F32)
            B_gf, B_ssq4, B_rs4 = Buf(), Buf(), Buf()
            B_a4 = [Buf() for _ in range(2)]
            B_o4 = [Buf() for _ in range(2)]
            d_a4 = [cx.dsem("a40"), cx.dsem("a41")]
            cx.op(SP, lambda: nsp.dma_start(out=gfbc[:], in_=gfbc_d[:, :]), writes=[B_gf], dma=d_const)
            cx.op(V, lambda: nv.memset(ssq4[:], 0.0), writes=[B_ssq4])
            for T in range(NT):
                tq = T % 2
                cx.op(SP, lambda: nsp.dma_start(out=a4[tq][:], in_=acc_d[T * 128:(T + 1) * 128, :]),
                      reads=[B_acc] + B_sc[0] + B_sc[1], writes=[B_a4[tq]], dma=d_a4[tq])
                cx.op(A, lambda: na.activation(out=o4[tq][:], in_=a4[tq][:], func=AF.Square, accum_out=ssq4[:, T:T + 1]),
                      reads=[B_a4[tq]], writes=[B_o4[tq], B_ssq4])
                rstd_from_ssq(ssq4[:, T:T + 1], rs4[:, T:T + 1], D, [B_ssq4], [B_rs4])
                cx.op(V, lambda: nv.scalar_tensor_tensor(out=o4[tq][:], in0=a4[tq][:], scalar=rs4[:, T:T + 1], in1=gfbc[:],
                                                         op0=ALU.mult, op1=ALU.mult),
                      reads=[B_a4[tq], B_rs4, B_gf], writes=[B_o4[tq]])
                cx.op(SP, lambda: nsp.dma_start(out=out_d[T * 128:(T + 1) * 128, :], in_=o4[tq][:]),
                      reads=[B_o4[tq]], writes=[B_out], dma=d_out)
        cx.barrier()

    return nc


_CONST = None
_NC_CACHE = {}


def _shared_maps(inp):
    global _CONST
    if _CONST is None:
        _CONST = _host_constants()
    c = _CONST
    f32 = lambda a: np.ascontiguousarray(np.asarray(a, dtype=np.float32))
    bc = lambda v: np.ascontiguousarray(np.broadcast_to(f32(v).reshape(1, -1), (128, f32(v).size)))
    m = {}
    m["g1bc"] = bc(inp["norm1_g"][0])
    m["g2bc"] = bc(inp["norm2_g"][0])
    m["gfbc"] = bc(inp["final_g"])
    m["w_in"] = f32(inp["w_in"][0])
    m["pool_w"] = f32(inp["pool_w"][0])
    m["w_out"] = f32(inp["w_out"][0])
    m["w_router"] = f32(inp["w_router"][0])
    m["w_gate"] = f32(inp["w_gate"][0])
    m["w_up"] = f32(inp["w_up"][0])
    m["w_down"] = f32(inp["w_down"][0])
    m["pscale"] = np.ascontiguousarray(f32(inp["pool_scale"][0]).reshape(8, 128).T)
    gn = np.concatenate([f32(inp["gn_pool"][0]), f32(inp["gn_attn"][0])])
    m["gnfm"] = np.ascontiguousarray(gn.reshape(16, 128).T)
    sink = f32(inp["sink"][0])
    m["sinkbc"] = np.ascontiguousarray(np.broadcast_to(np.repeat(sink, 128)[None, :], (128, 1024)))
    rb = f32(inp["rel_bias"])
    bidx = c["_bidx"]
    bt = np.zeros((2, 3, 128, 4, 128), np.float32)
    for j in range(2):
        for gq in range(4):
            bt[j, :, :, gq, :] = rb[:, 4 * j + gq][bidx]
    m["biastab"] = np.ascontiguousarray(bt.reshape(6, 128, 512))
    for k in ("mask01", "ident_bf", "ident_f", "iota512", "tokab", "edgef"):
        m[k] = c[k]
    return m


def _get_nc(debug=0):
    if debug not in _NC_CACHE:
        _NC_CACHE[debug] = build_nc(debug)
    return _NC_CACHE[debug]


def kernel(**inputs):
    inp = {k: np.asarray(v) for k, v in inputs.items()}
    shared = _shared_maps(inp)
    x = np.asarray(inp["x"], dtype=np.float32)
    in_maps = []
    for c in range(NCORES):
        m = dict(shared)
        m["x"] = np.ascontiguousarray(x[c])
        in_maps.append(m)
    nc = _get_nc(0)
    res = run_bass_kernel_spmd(nc, in_maps, core_ids=list(range(NCORES)))
    out = np.stack([np.asarray(res.results[b]["out"], dtype=np.float32) for b in range(4)], axis=0)
    return out
```

```python
import os
import math
import numpy as np
import ml_dtypes
from contextlib import ExitStack
import concourse.bass as bass
import concourse.mybir as mybir
from concourse.bass_utils import run_bass_kernel_spmd

F32 = mybir.dt.float32
BF16 = mybir.dt.bfloat16
I32 = mybir.dt.int32
ALU = mybir.AluOpType
AF = mybir.ActivationFunctionType

S = 4096
D = 2048
NT = S // 128
NG = S // 512
INW = 2560
DFF = 4096
NE = 16
CAP = 512
EPS = 1e-6
NCORES = 4
NSLOT = 6
NBISECT = 31
SEM_EPOCH = 30000


class Buf:
    __slots__ = ("name", "w", "rs")

    def __init__(self, name=""):
        self.name = name
        self.w = None
        self.rs = {}


class SemC:
    __slots__ = ("sem", "count", "is_dma", "unit")

    def __init__(self, sem, is_dma, unit):
        self.sem = sem
        self.count = 0
        self.is_dma = is_dma
        self.unit = unit


class Ctx:
    def __init__(self, nc, es):
        self.nc = nc
        self.es = es
        self.engs = {"pe": nc.tensor, "act": nc.scalar, "dve": nc.vector, "pool": nc.gpsimd, "sp": nc.sync}
        self.esem = {}
        self.all_sems = []
        self.nsem = 0
        for k in self.engs:
            self._new_esem(k)
        self.waited = {k: {} for k in self.engs}
        self.nwaits = 0
        self.nops = 0

    def _new_esem(self, k):
        s = SemC(self.es.enter_context(self.nc.semaphore("e%s%d" % (k, self.nsem))), False, 1)
        self.nsem += 1
        self.esem[k] = s
        self.all_sems.append(s)

    def dsem(self, name):
        s = SemC(self.es.enter_context(self.nc.semaphore("d" + name)), True, 16)
        self.nsem += 1
        self.all_sems.append(s)
        return s

    def _wait(self, eng, semc, val):
        if semc.is_dma:
            val = semc.count * semc.unit
        if val <= 0:
            return
        w = self.waited[eng]
        if w.get(id(semc), 0) >= val:
            return
        self.engs[eng].wait_ge(semc.sem, val)
        w[id(semc)] = val
        self.nwaits += 1

    def op(self, eng, fn, reads=(), writes=(), dma=None):
        for b in reads:
            if b.w is not None:
                self._wait(eng, b.w[0], b.w[1])
        for b in writes:
            for t in b.rs.values():
                if dma is None and t[2] == eng:
                    continue
                self._wait(eng, t[0], t[1])
            t = b.w
            if t is not None and not (dma is None and t[2] == eng):
                self._wait(eng, t[0], t[1])
        inst = fn()
        self.nops += 1
        if dma is not None:
            dma.count += 1
            inst.then_inc(dma.sem, dma.unit)
            tok = (dma, dma.count * dma.unit, None)
        else:
            s = self.esem[eng]
            if s.count >= SEM_EPOCH:
                self._new_esem(eng)
                s = self.esem[eng]
            s.count += 1
            inst.then_inc(s.sem, 1)
            tok = (s, s.count, eng)
        for b in writes:
            b.w = tok
            b.rs = {}
        for b in reads:
            b.rs[id(tok[0])] = tok
        return tok

    def barrier(self):
        for e in self.engs:
            for s in self.all_sems:
                if s is self.esem.get(e):
                    continue
                self._wait(e, s, s.count * s.unit)


def _t5_bucket_np(rel):
    half, max_exact = 16, 8
    ret = np.where(rel > 0, half, 0)
    n = np.abs(rel)
    nf = np.maximum(n, 1).astype(np.float64)
    large = max_exact + np.floor(2.0 * np.log2(nf / max_exact) + 1e-6).astype(np.int64)
    large = np.minimum(large, half - 1)
    return ret + np.where(n < max_exact, n, large)


def _host_constants():
    c = {}
    c["ident_bf"] = np.eye(128, dtype=np.float32).astype(ml_dtypes.bfloat16)
    c["ident_f"] = np.eye(128, dtype=np.float32)
    c["iota512"] = np.broadcast_to(np.arange(512, dtype=np.float32), (128, 512)).copy()
    tok = (np.arange(128)[:, None] + 128 * np.arange(32)[None, :])
    tokab = np.stack([tok // 64, tok % 64], axis=-1).astype(np.float32)
    c["tokab"] = tokab.copy()
    kk = np.arange(128)[:, None]
    qq = np.arange(128)[None, :]
    mask = np.zeros((3, 128, 512), np.float32)
    bidx = np.zeros((3, 128, 128), np.int64)
    for kb in range(3):
        rel = (kb - 1) * 128 + kk - qq
        m = np.where(np.abs(rel) <= 128, 0.0, -1.0e5).astype(np.float32)
        mask[kb] = np.tile(m, (1, 4))
        bidx[kb] = _t5_bucket_np(rel)
    c["mask01"] = mask
    c["_bidx"] = bidx
    edge = np.zeros((4, 16), np.float32)
    for wi, w in enumerate((2, 4, 8, 16)):
        for i in range(16):
            t = i if i < 8 else S - 16 + i
            lo = max(t - w // 2, 0)
            hi = min(t + w // 2, S)
            edge[wi, i] = 1.0 / float(hi - lo)
    c["edgef"] = np.broadcast_to(edge[None], (128, 4, 16)).copy()
    return c


def build_nc(debug=0):
    nc = bass.Bass("TRN2", target_bir_lowering=False)
    dt_in = lambda name, shape, dt=F32: nc.dram_tensor(name, shape, dt, kind="ExternalInput")
    x_d = dt_in("x", [S, D])
    g1bc_d = dt_in("g1bc", [128, D])
    g2bc_d = dt_in("g2bc", [128, D])
    gfbc_d = dt_in("gfbc", [128, D])
    w_in_d = dt_in("w_in", [D, INW])
    pool_w_d = dt_in("pool_w", [4, 256, 256])
    w_out_d = dt_in("w_out", [D, D])
    w_router_d = dt_in("w_router", [D, NE])
    if debug == 0 or debug >= 5:
        w_gate_d = dt_in("w_gate", [NE, D, DFF])
        w_up_d = dt_in("w_up", [NE, D, DFF])
        w_down_d = dt_in("w_down", [NE, DFF, D])
    pscale_d = dt_in("pscale", [128, 8])
    gnfm_d = dt_in("gnfm", [128, 16])
    sinkbc_d = dt_in("sinkbc", [128, 1024])
    biastab_d = dt_in("biastab", [6, 128, 512])
    mask01_d = dt_in("mask01", [3, 128, 512])
    ident_bf_d = dt_in("ident_bf", [128, 128], BF16)
    ident_f_d = dt_in("ident_f", [128, 128])
    iota512_d = dt_in("iota512", [128, 512])
    tokab_d = dt_in("tokab", [128, 32, 2])
    edgef_d = dt_in("edgef", [128, 4, 16])
    out_d = nc.dram_tensor("out", [S, D], F32, kind="ExternalOutput")
    dk = lambda lv: "ExternalOutput" if debug == lv else "Internal"
    qu_scr = nc.dram_tensor("qu_scr", [16, 128, S], BF16, kind=dk(1))
    mix_scr = nc.dram_tensor("mix_scr", [16, 128, S], BF16, kind=dk(2))
    h2_scr = nc.dram_tensor("h2_scr", [S, D], BF16, kind=dk(3))
    acc_d = nc.dram_tensor("acc", [S, D], F32, kind=dk(3))
    if debug:
        dbg_aff = nc.dram_tensor("dbg_aff", [NE, S], F32, kind="ExternalOutput")
        dbg_kv = nc.dram_tensor("dbg_kv", [128, 2 * S + NT * 256], BF16, kind="ExternalOutput")
        dbg_rstd = nc.dram_tensor("dbg_rstd", [128, 64], F32, kind="ExternalOutput")
        dbg_idx = nc.dram_tensor("dbg_idx", [NE, 2, 128, 4], F32, kind="ExternalOutput")
        dbg_thr = nc.dram_tensor("dbg_thr", [2, NE, 1], F32, kind="ExternalOutput")

    qu_pm = qu_scr.ap().rearrange("c p t -> p c t")
    mix_pm = mix_scr.ap().rearrange("c p t -> p c t")

    with ExitStack() as es:
        cx = Ctx(nc, es)
        sbuf = lambda st, name, shape, dt: st.enter_context(nc.sbuf_tensor("s_" + name, shape, dt))
        psum = lambda st, name, shape, dt=F32: st.enter_context(nc.psum_tensor("p_" + name, shape, dt))
        V, A, P, PE, SP = "dve", "act", "pool", "pe", "sp"
        nv, na, npl, nt, nsp = nc.vector, nc.scalar, nc.gpsimd, nc.tensor, nc.sync

        B_qu, B_mix, B_h2, B_acc, B_out = Buf("qu"), Buf("mix"), Buf("h2"), Buf("acc"), Buf("out")
        d_out = cx.dsem("out")

        ident_bf = sbuf(es, "ident_bf", [128, 128], BF16)
        ident_f = sbuf(es, "ident_f", [128, 128], F32)
        rstd_pa = sbuf(es, "rstd_pa", [128, 64], F32)
        eps_t = sbuf(es, "eps_t", [128, 1], F32)
        B_const, B_rstd = Buf("const"), Buf("rstd")
        d_const = cx.dsem("const")
        cx.op(SP, lambda: nsp.dma_start(out=ident_bf[:], in_=ident_bf_d[:, :]), writes=[B_const], dma=d_const)
        cx.op(SP, lambda: nsp.dma_start(out=ident_f[:], in_=ident_f_d[:, :]), writes=[B_const], dma=d_const)
        cx.op(V, lambda: nv.memset(eps_t[:], EPS), writes=[B_const])

        def rstd_from_ssq(ssq_ap, out_ap, n, rb, wb):
            cx.op(A, lambda: na.activation(out=out_ap, in_=ssq_ap, func=AF.Sqrt, scale=1.0 / float(n), bias=eps_t[:, 0:1]),
                  reads=list(rb) + [B_const], writes=wb)
            cx.op(V, lambda: nv.reciprocal(out=out_ap, in_=out_ap), reads=wb, writes=wb)

        with ExitStack() as s1:
            kT_all = sbuf(s1, "kT_all", [128, 2, S], BF16)
            V_all = sbuf(s1, "V_all", [128, NT, 256], BF16)
            B_kT = [Buf("kT%d" % g) for g in range(NG)]
            B_V = [Buf("V%d" % g) for g in range(NG)]

            with ExitStack() as ph:
                w_in_sb = sbuf(ph, "w_in_sb", [128, 16, INW], BF16)
                g1bc = sbuf(ph, "g1bc", [128, D], F32)
                xbuf = [sbuf(ph, "xb%d" % i, [128, D], F32) for i in range(2)]
                hb = [sbuf(ph, "hb%d" % i, [128, D], BF16) for i in range(2)]
                hT = [sbuf(ph, "hT%d" % i, [128, 16, 512], BF16) for i in range(2)]
                qu_st = sbuf(ph, "qu_st", [128, 16, 512], BF16)
                ssq = sbuf(ph, "ssq", [128, NT], F32)
                rs1 = sbuf(ph, "rs1", [128, NT], F32)
                tp = [psum(ph, "tp%d" % i, [128, 8, 128], BF16) for i in range(3)]
                pp = [psum(ph, "pp%d" % i, [128, 512], F32) for i in range(5)]
                B_w, B_g1 = Buf("w_in"), Buf("g1")
                B_x = [Buf() for _ in range(2)]
                B_hb = [Buf() for _ in range(2)]
                B_hT = [Buf() for _ in range(2)]
                B_ssq = [Buf() for _ in range(2)]
                B_rs = [Buf() for _ in range(2)]
                B_tp = [Buf() for _ in range(3)]
                B_pp = [Buf() for _ in range(5)]
                B_qs = Buf("qu_st")
                d_w, d_x, d_qs = cx.dsem("w1"), [cx.dsem("x0"), cx.dsem("x1")], cx.dsem("qs")
                w_in_v = w_in_d.ap().rearrange("(kc p) n -> p kc n", p=128)
                for q4 in range(4):
                    cx.op(P, lambda: npl.dma_start(out=w_in_sb[:, 4 * q4:4 * q4 + 4, :], in_=w_in_v[:, 4 * q4:4 * q4 + 4, :]),
                          writes=[B_w], dma=d_w)
                cx.op(SP, lambda: nsp.dma_start(out=g1bc[:], in_=g1bc_d[:, :]), writes=[B_g1], dma=d_const)
                cx.op(V, lambda: nv.memset(ssq[:], 0.0), writes=B_ssq)
                tpi = 0
                ppi = 0
                evi = 0
                for g in range(NG):
                    gp = g % 2
                    for i in range(4):
                        T = 4 * g + i
                        tpar = T % 2
                        cx.op(SP, lambda: nsp.dma_start(out=xbuf[tpar][:], in_=x_d[T * 128:(T + 1) * 128, :]),
                              writes=[B_x[tpar]], dma=d_x[tpar])
                        cx.op(A, lambda: na.activation(out=hb[tpar][:], in_=xbuf[tpar][:], func=AF.Square,
                                                       accum_out=ssq[:, T:T + 1]),
                              reads=[B_x[tpar]], writes=[B_hb[tpar], B_ssq[tpar]])
                        rstd_from_ssq(ssq[:, T:T + 1], rs1[:, T:T + 1], D, [B_ssq[tpar]], [B_rs[tpar]])
                        cx.op(V, lambda: nv.scalar_tensor_tensor(out=hb[tpar][:], in0=xbuf[tpar][:], scalar=rs1[:, T:T + 1],
                                                                 in1=g1bc[:], op0=ALU.mult, op1=ALU.mult),
                              reads=[B_x[tpar], B_rs[tpar], B_g1], writes=[B_hb[tpar]])
                        for half in range(2):
                            tb = tpi % 3
                            tpi += 1
                            for k in range(8):
                                kc = half * 8 + k
                                cx.op(PE, lambda: nt.transpose(out=tp[tb][:, k, :], in_=hb[tpar][:, kc * 128:(kc + 1) * 128],
                                                               identity=ident_bf[:]),
                                      reads=[B_hb[tpar], B_const], writes=[B_tp[tb]])
                            dst = hT[gp][:, half * 8:half * 8 + 8, i * 128:(i + 1) * 128]
                            if half == 0:
                                cx.op(A, lambda: na.copy(out=dst, in_=tp[tb][:]), reads=[B_tp[tb]], writes=[B_hT[gp]])
                            else:
                                cx.op(V, lambda: nv.tensor_copy(out=dst, in_=tp[tb][:]), reads=[B_tp[tb]], writes=[B_hT[gp]])
                    for oc in range(18):
                        c0 = oc * 128
                        pb = ppi % 5
                        ppi += 1
                        for kc in range(16):
                            cx.op(PE, lambda: nt.matmul(pp[pb][:], lhsT=w_in_sb[:, kc, c0:c0 + 128], rhs=hT[gp][:, kc, :],
                                                        start=(kc == 0), stop=(kc == 15)),
                                  reads=[B_w, B_hT[gp]], writes=[B_pp[pb]])
                        if oc < 16:
                            dst, wb = qu_st[:, oc, :], B_qs
                        else:
                            dst, wb = kT_all[:, oc - 16, g * 512:(g + 1) * 512], B_kT[g]
                        evi += 1
                        if evi % 2 == 0:
                            cx.op(A, lambda: na.copy(out=dst, in_=pp[pb][:]), reads=[B_pp[pb]], writes=[wb])
                        else:
                            cx.op(V, lambda: nv.tensor_copy(out=dst, in_=pp[pb][:]), reads=[B_pp[pb]], writes=[wb])
                    cx.op(SP, lambda: nsp.dma_start(out=qu_pm[:, :, g * 512:(g + 1) * 512], in_=qu_st[:]),
                          reads=[B_qs], writes=[B_qu], dma=d_qs)
                    for i in range(4):
                        pb = ppi % 5
                        ppi += 1
                        for kc in range(16):
                            cx.op(PE, lambda: nt.matmul(pp[pb][:, 0:256], lhsT=hT[gp][:, kc, i * 128:(i + 1) * 128],
                                                        rhs=w_in_sb[:, kc, 2304:2560], start=(kc == 0), stop=(kc == 15)),
                                  reads=[B_w, B_hT[gp]], writes=[B_pp[pb]])
                        evi += 1
                        if evi % 2 == 0:
                            cx.op(A, lambda: na.copy(out=V_all[:, 4 * g + i, :], in_=pp[pb][:, 0:256]),
                                  reads=[B_pp[pb]], writes=[B_V[g]])
                        else:
                            cx.op(V, lambda: nv.tensor_copy(out=V_all[:, 4 * g + i, :], in_=pp[pb][:, 0:256]),
                                  reads=[B_pp[pb]], writes=[B_V[g]])
                if debug:
                    d_dbg = cx.dsem("dbg")
                    cx.op(SP, lambda: nsp.dma_start(out=dbg_kv[:, 0:2 * S], in_=kT_all[:]), reads=B_kT, writes=[B_out], dma=d_dbg)
                    cx.op(SP, lambda: nsp.dma_start(out=dbg_kv[:, 2 * S:], in_=V_all[:]), reads=B_V, writes=[B_out], dma=d_dbg)
                cx.barrier()
            if debug == 1:
                cx.barrier()
                return nc

            with ExitStack() as ph:
                eb = sbuf(ph, "eb", [128, 6, 512], F32)
                Bs = sbuf(ph, "Bs", [128, 6, 512], BF16)
                B_Bs = Buf()
                mk = sbuf(ph, "mk", [128, 3, 512], F32)
                sinkexp = sbuf(ph, "sinkexp", [128, 1024], F32)
                ones_bf = sbuf(ph, "ones_bf", [128, 128], BF16)
                pw_sb = sbuf(ph, "pw_sb", [128, 4, 2, 256], BF16)
                pscale = sbuf(ph, "pscale", [128, 8], F32)
                edgef = sbuf(ph, "edgef", [128, 4, 16], F32)
                qT = [sbuf(ph, "qT%d" % i, [128, 8, 512], BF16) for i in range(2)]
                uT = [sbuf(ph, "uT%d" % i, [128, 8, 528], BF16) for i in range(2)]
                tmp = [sbuf(ph, "ptmp%d" % i, [128, 2, 528], F32) for i in range(4)]
                etmp = sbuf(ph, "etmp", [128, 2, 8], F32)
                tmp4 = sbuf(ph, "ptmp4", [128, 2, 512], F32)
                pooledT = sbuf(ph, "pooledT", [128, 8, 512], BF16)
                sqp = sbuf(ph, "sqp", [128, 8, 512], BF16)
                et = [sbuf(ph, "et%d" % i, [128, 512], F32) for i in range(2)]
                pt = [sbuf(ph, "pt%d" % i, [128, 512], BF16) for i in range(9)]
                den = [sbuf(ph, "den%d" % i, [128, 512], F32) for i in range(2)]
                at = [sbuf(ph, "at%d" % i, [128, 4, 128], F32) for i in range(2)]
                sqa = [sbuf(ph, "sqa%d" % i, [128, 4, 128], BF16) for i in range(4)]
                mixst = [sbuf(ph, "mixst%d" % i, [128, 16, 512], BF16) for i in range(2)]
                sps = [psum(ph, "sps%d" % i, [128, 512], F32) for i in range(2)]
                o_ps = [psum(ph, "ops%d" % i, [128, 4, 128], F32) for i in range(2)]
                d_ps = [psum(ph, "dps%d" % i, [128, 512], F32) for i in range(2)]
                pw_ps = psum(ph, "pwps", [128, 512], F32)
                ss_ps = psum(ph, "ssps", [128, 512], F32)
                B_eb, B_mk, B_sk, B_pw, B_c2 = Buf(), Buf(), Buf(), Buf(), Buf()
                B_q = [Buf() for _ in range(2)]
                B_u = [Buf() for _ in range(2)]
                B_tmp, B_etmp, B_pooled, B_sqp = Buf(), Buf(), Buf(), Buf()
                B_et = [Buf() for _ in range(2)]
                B_pt = [Buf() for _ in range(9)]
                B_den = [Buf() for _ in range(2)]
                B_at = [Buf() for _ in range(2)]
                B_sqa = [Buf() for _ in range(4)]
                B_mixst = [Buf() for _ in range(2)]
                B_sps = [Buf() for _ in range(2)]
                B_ops = [Buf() for _ in range(2)]
                B_dps = [Buf() for _ in range(2)]
                B_pwps, B_ss = Buf(), Buf()
                d_c2, d_q, d_u, d_ms = cx.dsem("c2"), [cx.dsem("q0"), cx.dsem("q1")], [cx.dsem("u0"), cx.dsem("u1")], [cx.dsem("ms0"), cx.dsem("ms1")]
                d_pw = cx.dsem("pw")
                cx.op(SP, lambda: nsp.dma_start(out=eb[:], in_=biastab_d.ap().rearrange("j p c -> p j c")), writes=[B_eb], dma=d_c2)
                cx.op(SP, lambda: nsp.dma_start(out=mk[:], in_=mask01_d.ap().rearrange("j p c -> p j c")), writes=[B_mk], dma=d_c2)
                cx.op(SP, lambda: nsp.dma_start(out=sinkexp[:], in_=sinkbc_d[:, :]), writes=[B_sk], dma=d_c2)
                cx.op(SP, lambda: nsp.dma_start(out=pscale[:], in_=pscale_d[:, :]), writes=[B_c2], dma=d_c2)
                cx.op(SP, lambda: nsp.dma_start(out=edgef[:], in_=edgef_d[:, :, :]), writes=[B_c2], dma=d_c2)
                cx.op(P, lambda: npl.dma_start(out=pw_sb[:], in_=pool_w_d.ap().rearrange("g (kc p) d -> p g kc d", p=128)),
                      writes=[B_pw], dma=d_pw)
                cx.op(V, lambda: nv.memset(ones_bf[:], 1.0), writes=[B_c2])
                cx.op(A, lambda: na.activation(out=sinkexp[:], in_=sinkexp[:], func=AF.Exp), reads=[B_sk], writes=[B_sk])
                for jk in range(6):
                    cx.op(V, lambda: nv.scalar_tensor_tensor(out=Bs[:, jk, :], in0=eb[:, jk, :], scalar=1.0 / (128.0 ** -0.5), in1=mk[:, jk % 3, :],
                                                             op0=ALU.mult, op1=ALU.add),
                          reads=[B_eb, B_mk], writes=[B_Bs])
                cx.op(V, lambda: nv.memset(uT[0][:, :, 0:8], 0.0), writes=[B_u[0]])
                SCALE = 128.0 ** -0.5
                ctr = {"sp": 0, "pt": 0, "bk": 0, "sq": 0}
                NPT = len(pt)

                def emit_loads(g):
                    gp = g % 2
                    t0 = g * 512
                    cx.op(SP, lambda: nsp.dma_start(out=qT[gp][:], in_=qu_pm[:, 8:16, t0:t0 + 512]),
                          reads=[B_qu], writes=[B_q[gp]], dma=d_q[gp])
                    if g == 0:
                        cx.op(SP, lambda: nsp.dma_start(out=uT[gp][:, :, 8:528], in_=qu_pm[:, 0:8, 0:520]),
                              reads=[B_qu], writes=[B_u[gp]], dma=d_u[gp])
                    elif g == NG - 1:
                        cx.op(V, lambda: nv.memset(uT[gp][:, :, 520:528], 0.0), writes=[B_u[gp]])
                        cx.op(SP, lambda: nsp.dma_start(out=uT[gp][:, :, 0:520], in_=qu_pm[:, 0:8, t0 - 8:S]),
                              reads=[B_qu], writes=[B_u[gp]], dma=d_u[gp])
                    else:
                        cx.op(SP, lambda: nsp.dma_start(out=uT[gp][:], in_=qu_pm[:, 0:8, t0 - 8:t0 + 520]),
                              reads=[B_qu], writes=[B_u[gp]], dma=d_u[gp])

                def stage_A(g, i, j):
                    gp = g % 2
                    Bk = 4 * g + i
                    pts = []
                    for kb in range(3):
                        KB = Bk + kb - 1
                        if KB < 0 or KB >= NT:
                            continue
                        sb_ = ctr["sp"] % 2
                        ctr["sp"] += 1
                        cx.op(PE, lambda: nt.matmul(sps[sb_][:], lhsT=kT_all[:, j, KB * 128:(KB + 1) * 128],
                                                    rhs=qT[gp][:, 4 * j:4 * j + 4, i * 128:(i + 1) * 128], start=True, stop=False),
                              reads=[B_kT[KB // 4], B_q[gp]], writes=[B_sps[sb_]])
                        cx.op(PE, lambda: nt.matmul(sps[sb_][:], lhsT=ident_bf[:], rhs=Bs[:, j * 3 + kb, :], start=False, stop=True),
                              reads=[B_const, B_Bs], writes=[B_sps[sb_]])
                        pb_ = ctr["pt"] % NPT
                        ctr["pt"] += 1
                        cx.op(A, lambda: na.activation(out=pt[pb_][:], in_=sps[sb_][:], func=AF.Exp, scale=SCALE),
                              reads=[B_sps[sb_]], writes=[B_pt[pb_]])
                        pts.append((pb_, KB))
                    return pts

                def stage_B(g, i, j, pts):
                    gp = g % 2
                    ob = ctr["bk"] % 2
                    ctr["bk"] += 1
                    for n, (pb_, KB) in enumerate(pts):
                        cx.op(PE, lambda: nt.matmul(o_ps[ob][:], lhsT=V_all[:, KB, j * 128:(j + 1) * 128], rhs=pt[pb_][:],
                                                    start=(n == 0), stop=(n == len(pts) - 1)),
                              reads=[B_V[KB // 4], B_pt[pb_]], writes=[B_ops[ob]])
                    for n, (pb_, KB) in enumerate(pts):
                        cx.op(PE, lambda: nt.matmul(d_ps[ob][:], lhsT=ones_bf[:], rhs=pt[pb_][:],
                                                    start=(n == 0), stop=(n == len(pts) - 1)),
                              reads=[B_c2, B_pt[pb_]], writes=[B_dps[ob]])
                    cx.op(V, lambda: nv.tensor_tensor(out=den[ob][:], in0=d_ps[ob][:], in1=sinkexp[:, j * 512:(j + 1) * 512], op=ALU.add),
                          reads=[B_dps[ob], B_sk], writes=[B_den[ob]])
                    cx.op(V, lambda: nv.reciprocal(out=den[ob][:], in_=den[ob][:]), reads=[B_den[ob]], writes=[B_den[ob]])
                    cx.op(V, lambda: nv.tensor_tensor(out=at[ob][:], in0=o_ps[ob][:],
                                                      in1=den[ob][:].rearrange("p (a b) -> p a b", b=128), op=ALU.mult),
                          reads=[B_ops[ob], B_den[ob]], writes=[B_at[ob]])
                    sb2 = ctr["sq"] % 4
                    ctr["sq"] += 1
                    cx.op(A, lambda: na.activation(out=sqa[sb2][:], in_=at[ob][:], func=AF.Square),
                          reads=[B_at[ob]], writes=[B_sqa[sb2]])
                    cx.op(V, lambda: nv.tensor_copy(out=mixst[gp][:, 8 + 4 * j:8 + 4 * j + 4, i * 128:(i + 1) * 128], in_=at[ob][:]),
                          reads=[B_at[ob]], writes=[B_mixst[gp]])
                    return sb2

                def stage_C(i, sq_pair):
                    n = 0
                    for sb2 in sq_pair:
                        for gq in range(4):
                            cx.op(PE, lambda: nt.matmul(ss_ps[:, i:i + 1], lhsT=sqa[sb2][:, gq, :], rhs=ones_bf[:, 0:1],
                                                        start=(n == 0), stop=(n == 7)),
                                  reads=[B_sqa[sb2], B_c2], writes=[B_ss])
                            n += 1

                def pooling(g, grp):
                    gp = g % 2
                    w = (2, 4, 8, 16)[grp]
                    U = uT[gp][:, 2 * grp:2 * grp + 2, :]
                    cx.op(V, lambda: nv.tensor_tensor(out=tmp[0][:, :, 1:528], in0=U[:, :, 0:527], in1=U[:, :, 1:528], op=ALU.add),
                          reads=[B_u[gp]], writes=[B_tmp])
                    if grp >= 1:
                        cx.op(V, lambda: nv.tensor_tensor(out=tmp[1][:, :, 2:527], in0=tmp[0][:, :, 1:526], in1=tmp[0][:, :, 3:528], op=ALU.add),
                              reads=[B_tmp], writes=[B_tmp])
                    if grp >= 2:
                        cx.op(V, lambda: nv.tensor_tensor(out=tmp[2][:, :, 4:525], in0=tmp[1][:, :, 2:523], in1=tmp[1][:, :, 6:527], op=ALU.add),
                              reads=[B_tmp], writes=[B_tmp])
                    if grp >= 3:
                        cx.op(V, lambda: nv.tensor_tensor(out=tmp[3][:, :, 8:521], in0=tmp[2][:, :, 4:517], in1=tmp[2][:, :, 12:525], op=ALU.add),
                              reads=[B_tmp], writes=[B_tmp])
                    sw = tmp[grp]
                    cx.op(V, lambda: nv.scalar_tensor_tensor(out=pooledT[:, 2 * grp:2 * grp + 2, :], in0=sw[:, :, 8:520], scalar=1.0 / w,
                                                             in1=U[:, :, 8:520], op0=ALU.mult, op1=ALU.subtract),
                          reads=[B_tmp, B_u[gp]], writes=[B_pooled])
                    edges = []
                    if g == 0:
                        edges.append((8, 0, 0))
                    if g == NG - 1:
                        edges.append((512, 504, 8))
                    for (uc, pc, ec) in edges:
                        for ch in range(2):
                            cx.op(V, lambda: nv.tensor_tensor(out=etmp[:, ch, :], in0=sw[:, ch, uc:uc + 8], in1=edgef[:, grp, ec:ec + 8], op=ALU.mult),
                                  reads=[B_tmp, B_c2], writes=[B_etmp])
                            cx.op(V, lambda: nv.tensor_tensor(out=pooledT[:, 2 * grp + ch, pc:pc + 8], in0=etmp[:, ch, :],
                                                              in1=uT[gp][:, 2 * grp + ch, uc:uc + 8], op=ALU.subtract),
                                  reads=[B_etmp, B_u[gp]], writes=[B_pooled])

                def group_end(g):
                    gp = g % 2
                    t0 = g * 512
                    for oc in range(8):
                        grp, hf = oc // 2, oc % 2
                        for kc in range(2):
                            cx.op(PE, lambda: nt.matmul(pw_ps[:], lhsT=pw_sb[:, grp, kc, hf * 128:(hf + 1) * 128], rhs=pooledT[:, 2 * grp + kc, :],
                                                        start=(kc == 0), stop=(kc == 1)),
                                  reads=[B_pw, B_pooled], writes=[B_pwps])
                        cx.op(A, lambda: na.activation(out=mixst[gp][:, oc, :], in_=pw_ps[:], func=AF.Copy, scale=pscale[:, oc:oc + 1]),
                              reads=[B_pwps, B_c2], writes=[B_mixst[gp]])
                        cx.op(A, lambda: na.activation(out=sqp[:, oc, :], in_=pw_ps[:], func=AF.Square, scale=pscale[:, oc:oc + 1]),
                              reads=[B_pwps, B_c2], writes=[B_sqp])
                    for i in range(4):
                        for oc in range(8):
                            cx.op(PE, lambda: nt.matmul(ss_ps[:, 4 + i:5 + i], lhsT=sqp[:, oc, i * 128:(i + 1) * 128], rhs=ones_bf[:, 0:1],
                                                        start=(oc == 0), stop=(oc == 7)),
                                  reads=[B_sqp, B_c2], writes=[B_ss])
                    rstd_from_ssq(ss_ps[:, 0:4], rstd_pa[:, 32 + 4 * g:32 + 4 * g + 4], 1024, [B_ss], [B_rstd])
                    rstd_from_ssq(ss_ps[:, 4:8], rstd_pa[:, 4 * g:4 * g + 4], 1024, [B_ss], [B_rstd])
                    cx.op(SP, lambda: nsp.dma_start(out=mix_pm[:, :, t0:t0 + 512], in_=mixst[gp][:]),
                          reads=[B_mixst[gp]], writes=[B_mix], dma=d_ms[gp])

                units = [(g, i, j) for g in range(NG) for i in range(4) for j in range(2)]
                sq_of = {}
                pend = []

                def do_B(u, pts):
                    g, i, j = u
                    sb2 = stage_B(g, i, j, pts)
                    sq_of.setdefault((g, i), []).append(sb2)
                    for fn in pend:
                        fn()
                    del pend[:]
                    if j == 0:
                        pooling(g, i)
                    if j == 1:
                        pair = sq_of.pop((g, i))
                        pend.append(lambda: stage_C(i, pair))
                        if i == 3:
                            pend.append(lambda: group_end(g))

                prev = None
                for u in units:
                    g, i, j = u
                    if i == 0 and j == 0:
                        emit_loads(g)
                    pts = stage_A(g, i, j)
                    if prev is not None:
                        do_B(*prev)
                    prev = (u, pts)
                do_B(*prev)
                for fn in pend:
                    fn()
                del pend[:]
                if debug == 2:
                    d_dbg = cx.dsem("dbg2")
                    cx.op(SP, lambda: nsp.dma_start(out=dbg_rstd[:, :], in_=rstd_pa[:]), reads=[B_rstd], writes=[B_out], dma=d_dbg)
                cx.barrier()
        if debug == 2:
            cx.barrier()
            return nc

        s3 = es.enter_context(ExitStack())
        posT = sbuf(s3, "posT", [128, NT, NE], F32)
        gaT = sbuf(s3, "gaT", [128, NT, NE], F32)
        vals = sbuf(s3, "vals", [128, NT, NE, 5], BF16)
        iota512 = sbuf(s3, "iota512", [128, 512], F32)
        B_posT, B_gaT, B_vals, B_iota = Buf(), Buf(), Buf(), Buf()
        with ExitStack() as s2:
            affT = sbuf(s2, "affT", [NE, S], F32)
            B_aff = Buf("aff")
            with ExitStack() as ph:
                w_out_sb = sbuf(ph, "w_out_sb", [128, 16, D], BF16)
                gnfm = sbuf(ph, "gnfm", [128, 16], F32)
                wr_sb = sbuf(ph, "wr_sb", [128, 16, NE], F32)
                wr_hi = sbuf(ph, "wr_hi", [128, 16, NE], BF16)
                wr_lo = sbuf(ph, "wr_lo", [128, 16, NE], BF16)
                h2lo = [sbuf(ph, "h2lo%d" % i, [128, D], BF16) for i in range(2)]
                hiT = sbuf(ph, "hiT", [128, 16, 128], BF16)
                loT = sbuf(ph, "loT", [128, 16, 128], BF16)
                B_hiT, B_loT = Buf(), Buf()
                B_h2lo = [Buf() for _ in range(2)]
                g2bc = sbuf(ph, "g2bc", [128, D], F32)
                mixld = [sbuf(ph, "mixld%d" % i, [128, 16, 512], BF16) for i in range(2)]
                xt = [sbuf(ph, "xt%d" % i, [128, D], F32) for i in range(2)]
                x1t = [sbuf(ph, "x1t%d" % i, [128, D], F32) for i in range(2)]
                h2f = sbuf(ph, "h2f", [128, D], F32)
                h2b = [sbuf(ph, "h2b%d" % i, [128, D], BF16) for i in range(2)]
                ex = sbuf(ph, "ex", [NE, 512], F32)
                rc = sbuf(ph, "rc", [NE, 512], F32)
                ones16 = sbuf(ph, "ones16", [NE, NE], F32)
                ssq2 = sbuf(ph, "ssq2", [128, NT], F32)
                rs2 = sbuf(ph, "rs2", [128, NT], F32)
                P1 = [psum(ph, "P1_%d" % i, [128, 512], F32) for i in range(2)]
                P2 = [psum(ph, "P2_%d" % i, [128, 512], F32) for i in range(2)]
                tpf = [psum(ph, "tpf%d" % i, [128, 8, 128], BF16) for i in range(2)]
                lg_ps = [psum(ph, "lgps%d" % i, [NE, 512], F32) for i in range(2)]
                B_wo, B_ws, B_c3, B_g2 = Buf(), Buf(), Buf(), Buf()
                B_ml = [Buf() for _ in range(2)]
                B_xt = [Buf() for _ in range(2)]
                B_x1 = [Buf() for _ in range(2)]
                B_h2f = Buf()
                B_h2b = [Buf() for _ in range(2)]
                B_h2T, B_ex, B_rc, B_ssq2, B_rs2 = Buf(), Buf(), Buf(), Buf(), Buf()
                B_P1 = [Buf() for _ in range(2)]
                B_P2 = [Buf() for _ in range(2)]
                B_tpf = [Buf() for _ in range(2)]
                B_lg = [Buf() for _ in range(2)]
                B_sm = Buf()
                d_c3, d_ws = cx.dsem("c3"), cx.dsem("ws")
                d_ml, d_xt = [cx.dsem("ml0"), cx.dsem("ml1")], [cx.dsem("xt0"), cx.dsem("xt1")]
                d_x1, d_h2 = [cx.dsem("x1s0"), cx.dsem("x1s1")], [cx.dsem("h2s0"), cx.dsem("h2s1")]
                cx.op(SP, lambda: nsp.dma_start(out=gnfm[:], in_=gnfm_d[:, :]), writes=[B_c3], dma=d_c3)
                cx.op(SP, lambda: nsp.dma_start(out=g2bc[:], in_=g2bc_d[:, :]), writes=[B_g2], dma=d_c3)
                cx.op(SP, lambda: nsp.dma_start(out=wr_sb[:], in_=w_router_d.ap().rearrange("(kc p) e -> p kc e", p=128)),
                      writes=[B_c3], dma=d_c3)
                cx.op(V, lambda: nv.memset(ones16[:], 1.0), writes=[B_c3])
                cx.op(V, lambda: nv.memset(ssq2[:], 0.0), writes=[B_ssq2])
                cx.op(V, lambda: nv.tensor_copy(out=wr_hi[:], in_=wr_sb[:]), reads=[B_c3], writes=[B_c3])
                cx.op(V, lambda: nv.tensor_tensor(out=wr_sb[:], in0=wr_sb[:], in1=wr_hi[:], op=ALU.subtract), reads=[B_c3], writes=[B_c3])
                cx.op(V, lambda: nv.tensor_copy(out=wr_lo[:], in_=wr_sb[:]), reads=[B_c3], writes=[B_c3])
                for kc in range(16):
                    wst = h2f if kc % 2 == 0 else xt[0]
                    wB = B_h2f if kc % 2 == 0 else B_xt[0]
                    cx.op(SP, lambda: nsp.dma_start(out=wst[:], in_=w_out_d[kc * 128:(kc + 1) * 128, :]), writes=[wB], dma=d_ws)
                    cx.op(V, lambda: nv.tensor_scalar(out=w_out_sb[:, kc, :], in0=wst[:], scalar1=gnfm[:, kc:kc + 1], scalar2=None, op0=ALU.mult),
                          reads=[wB, B_c3], writes=[B_wo])
                pend_rt = []
                rt_done = {}
                tfc = [0]

                def flush_router():
                    for (g_, i_, tq_) in pend_rt:
                        for (src, srcB, dstT, dstB) in ((h2b[tq_], B_h2b[tq_], hiT, B_hiT), (h2lo[tq_], B_h2lo[tq_], loT, B_loT)):
                            for half in range(2):
                                tb = tfc[0] % 2
                                tfc[0] += 1
                                for k in range(8):
                                    kc = half * 8 + k
                                    cx.op(PE, lambda: nt.transpose(out=tpf[tb][:, k, :], in_=src[:, kc * 128:(kc + 1) * 128], identity=ident_bf[:]),
                                          reads=[srcB, B_const], writes=[B_tpf[tb]])
                                if half == 0:
                                    cx.op(A, lambda: na.copy(out=dstT[:, half * 8:half * 8 + 8, :], in_=tpf[tb][:]), reads=[B_tpf[tb]], writes=[dstB])
                                else:
                                    cx.op(V, lambda: nv.tensor_copy(out=dstT[:, half * 8:half * 8 + 8, :], in_=tpf[tb][:]), reads=[B_tpf[tb]], writes=[dstB])
                        n = 0
                        for (wsb, rT, rB) in ((wr_hi, hiT, B_hiT), (wr_hi, loT, B_loT), (wr_lo, hiT, B_hiT)):
                            for kc in range(16):
                                cx.op(PE, lambda: nt.matmul(lg_ps[g_ % 2][:, i_ * 128:(i_ + 1) * 128], lhsT=wsb[:, kc, :], rhs=rT[:, kc, :],
                                                            start=(n == 0), stop=(n == 47)),
                                      reads=[B_c3, rB], writes=[B_lg[g_ % 2]])
                                n += 1
                        rt_done[g_] = rt_done.get(g_, 0) + 1
                    del pend_rt[:]

                sm_done = set()

                def softmax_group(g_, final):
                    for gg in range(NG):
                        if gg in sm_done or rt_done.get(gg, 0) < 4:
                            continue
                        sm_done.add(gg)
                        tg = gg * 512
                        cx.op(A, lambda: na.activation(out=ex[:], in_=lg_ps[gg % 2][:], func=AF.Exp), reads=[B_lg[gg % 2]], writes=[B_ex])
                        cx.op(PE, lambda: nt.matmul(P1[0][0:NE, :], lhsT=ones16[:], rhs=ex[:], start=True, stop=True),
                              reads=[B_c3, B_ex], writes=[B_P1[0]])
                        cx.op(V, lambda: nv.reciprocal(out=rc[:], in_=P1[0][0:NE, :]), reads=[B_P1[0]], writes=[B_rc])
                        cx.op(V, lambda: nv.tensor_tensor(out=affT[:, tg:tg + 512], in0=ex[:], in1=rc[:], op=ALU.mult),
                              reads=[B_ex, B_rc], writes=[B_aff])

                pbi = 0
                tfi = 0
                for g in range(NG):
                    gp = g % 2
                    t0 = g * 512
                    cx.op(SP, lambda: nsp.dma_start(out=mixld[gp][:], in_=mix_pm[:, :, t0:t0 + 512]),
                          reads=[B_mix], writes=[B_ml[gp]], dma=d_ml[gp])
                    for i in range(4):
                        T = 4 * g + i
                        tq = T % 2
                        cx.op(SP, lambda: nsp.dma_start(out=xt[tq][:], in_=x_d[T * 128:(T + 1) * 128, :]), writes=[B_xt[tq]], dma=d_xt[tq])
                        for ds in range(4):
                            pb = pbi % 2
                            pbi += 1
                            dsl = slice(ds * 512, (ds + 1) * 512)
                            for kc in range(8):
                                cx.op(PE, lambda: nt.matmul(P1[pb][:], lhsT=mixld[gp][:, kc, i * 128:(i + 1) * 128], rhs=w_out_sb[:, kc, dsl],
                                                            start=(kc == 0), stop=(kc == 7)),
                                      reads=[B_ml[gp], B_wo], writes=[B_P1[pb]])
                            for kc in range(8, 16):
                                cx.op(PE, lambda: nt.matmul(P2[pb][:], lhsT=mixld[gp][:, kc, i * 128:(i + 1) * 128], rhs=w_out_sb[:, kc, dsl],
                                                            start=(kc == 8), stop=(kc == 15)),
                                      reads=[B_ml[gp], B_wo], writes=[B_P2[pb]])
                            cx.op(V, lambda: nv.scalar_tensor_tensor(out=x1t[tq][:, dsl], in0=P1[pb][:], scalar=rstd_pa[:, T:T + 1],
                                                                     in1=xt[tq][:, dsl], op0=ALU.mult, op1=ALU.add),
                                  reads=[B_P1[pb], B_rstd, B_xt[tq]], writes=[B_x1[tq]])
                            cx.op(V, lambda: nv.scalar_tensor_tensor(out=x1t[tq][:, dsl], in0=P2[pb][:], scalar=rstd_pa[:, 32 + T:33 + T],
                                                                     in1=x1t[tq][:, dsl], op0=ALU.mult, op1=ALU.add),
                                  reads=[B_P2[pb], B_rstd, B_x1[tq]], writes=[B_x1[tq]])
                        cx.op(SP, lambda: nsp.dma_start(out=acc_d[T * 128:(T + 1) * 128, :], in_=x1t[tq][:]),
                              reads=[B_x1[tq]], writes=[B_acc], dma=d_x1[tq])
                        cx.op(A, lambda: na.activation(out=h2f[:], in_=x1t[tq][:], func=AF.Square, accum_out=ssq2[:, T:T + 1]),
                              reads=[B_x1[tq]], writes=[B_h2f, B_ssq2])
                        rstd_from_ssq(ssq2[:, T:T + 1], rs2[:, T:T + 1], D, [B_ssq2], [B_rs2])
                        cx.op(V, lambda: nv.scalar_tensor_tensor(out=h2f[:], in0=x1t[tq][:], scalar=rs2[:, T:T + 1], in1=g2bc[:],
                                                                 op0=ALU.mult, op1=ALU.mult),
                              reads=[B_x1[tq], B_rs2, B_g2], writes=[B_h2f])
                        cx.op(A, lambda: na.copy(out=h2b[tq][:], in_=h2f[:]), reads=[B_h2f], writes=[B_h2b[tq]])
                        cx.op(SP, lambda: nsp.dma_start(out=h2_scr[T * 128:(T + 1) * 128, :], in_=h2b[tq][:]),
                              reads=[B_h2b[tq]], writes=[B_h2], dma=d_h2[tq])
                        cx.op(V, lambda: nv.tensor_tensor(out=h2lo[tq][:], in0=h2f[:], in1=h2b[tq][:], op=ALU.subtract),
                              reads=[B_h2f, B_h2b[tq]], writes=[B_h2lo[tq]])
                        flush_router()
                        pend_rt.append((g, i, tq))
                    softmax_group(g - 1 if g > 0 else None, final=False)
                flush_router()
                softmax_group(NG - 1, final=True)
                if debug in (3, 4):
                    d_dbg = cx.dsem("dbg3")
                    cx.op(SP, lambda: nsp.dma_start(out=dbg_aff[:, :], in_=affT[:]), reads=[B_aff], writes=[B_out], dma=d_dbg)
                cx.barrier()
            if debug == 3:
                cx.barrier()
                return nc

            with ExitStack() as ph:
                lo = sbuf(ph, "lo", [NE, 1], F32)
                hi = sbuf(ph, "hi", [NE, 1], F32)
                mid = sbuf(ph, "mid", [NE, 1], F32)
                cnt = sbuf(ph, "cnt", [NE, 1], F32)
                sel = sbuf(ph, "sel", [NE, 1], F32)
                nsel = sbuf(ph, "nsel", [NE, 1], F32)
                ta = sbuf(ph, "ta", [NE, 1], F32)
                tb_ = sbuf(ph, "tb", [NE, 1], F32)
                junk = sbuf(ph, "junk", [NE, S], F32)
                msk = sbuf(ph, "msk", [NE, S], F32)
                cs = sbuf(ph, "cs", [NE, S], F32)
                ones_s = sbuf(ph, "ones_s", [NE, S], F32)
                tokab = sbuf(ph, "tokab", [128, NT, 2], F32)
                r1 = sbuf(ph, "r1", [128, NT, NE], F32)
                pT_ps = psum(ph, "pTps", [128, NT, NE], F32)
                gT_ps = psum(ph, "gTps", [128, NT, NE], F32)
                B_b, B_junk, B_msk, B_cs, B_ones, B_tok, B_r1, B_pTps, B_gTps = [Buf() for _ in range(9)]
                d_c4 = cx.dsem("c4")
                cx.op(SP, lambda: nsp.dma_start(out=iota512[:], in_=iota512_d[:, :]), writes=[B_iota], dma=d_c4)
                cx.op(SP, lambda: nsp.dma_start(out=tokab[:], in_=tokab_d[:, :, :]), writes=[B_tok], dma=d_c4)
                cx.op(V, lambda: nv.memset(lo[:], 0.0), writes=[B_b])
                cx.op(V, lambda: nv.memset(hi[:], 1.0), writes=[B_b])
                cx.op(P, lambda: npl.memset(ones_s[:], 1.0), writes=[B_ones])
                for it in range(NBISECT):
                    cx.op(V, lambda: nv.tensor_scalar(out=mid[:], in0=lo[:], scalar1=hi[:, 0:1], scalar2=0.5, op0=ALU.add, op1=ALU.mult),
                          reads=[B_b], writes=[B_b])
                    cx.op(V, lambda: nv.memset(cnt[:], 0.0), reads=[B_b], writes=[B_b])
                    cx.op(V, lambda: nv.tensor_scalar(out=junk[:], in0=affT[:], scalar1=mid[:, 0:1], scalar2=0.0, op0=ALU.is_ge, op1=ALU.add,
                                                      accum_out=cnt[:]),
                          reads=[B_aff, B_b], writes=[B_junk, B_b])
                    cx.op(V, lambda: nv.tensor_scalar(out=sel[:], in0=cnt[:], scalar1=float(CAP), scalar2=None, op0=ALU.is_ge),
                          reads=[B_b], writes=[B_b])
                    cx.op(V, lambda: nv.tensor_scalar(out=nsel[:], in0=sel[:], scalar1=-1.0, scalar2=1.0, op0=ALU.mult, op1=ALU.add),
                          reads=[B_b], writes=[B_b])
                    cx.op(V, lambda: nv.tensor_scalar(out=ta[:], in0=mid[:], scalar1=nsel[:, 0:1], scalar2=None, op0=ALU.mult),
                          reads=[B_b], writes=[B_b])
                    cx.op(V, lambda: nv.scalar_tensor_tensor(out=hi[:], in0=hi[:], scalar=sel[:, 0:1], in1=ta[:], op0=ALU.mult, op1=ALU.add),
                          reads=[B_b], writes=[B_b])
                    cx.op(V, lambda: nv.tensor_scalar(out=tb_[:], in0=lo[:], scalar1=nsel[:, 0:1], scalar2=None, op0=ALU.mult),
                          reads=[B_b], writes=[B_b])
                    cx.op(V, lambda: nv.scalar_tensor_tensor(out=lo[:], in0=mid[:], scalar=sel[:, 0:1], in1=tb_[:], op0=ALU.mult, op1=ALU.add),
                          reads=[B_b], writes=[B_b])
                cx.op(V, lambda: nv.tensor_scalar(out=msk[:], in0=affT[:], scalar1=lo[:, 0:1], scalar2=None, op0=ALU.is_ge),
                      reads=[B_aff, B_b], writes=[B_msk])
                cx.op(V, lambda: nv.tensor_tensor_scan(out=cs[:], data0=ones_s[:], data1=msk[:], initial=0.0, op0=ALU.mult, op1=ALU.add),
                      reads=[B_ones, B_msk], writes=[B_cs])
                cx.op(V, lambda: nv.tensor_tensor(out=cs[:], in0=cs[:], in1=msk[:], op=ALU.mult), reads=[B_cs, B_msk], writes=[B_cs])
                cx.op(V, lambda: nv.tensor_scalar(out=cs[:], in0=cs[:], scalar1=-1.0, scalar2=None, op0=ALU.add), reads=[B_cs], writes=[B_cs])
                for jt in range(NT):
                    cx.op(PE, lambda: nt.transpose(out=pT_ps[:, jt, :], in_=cs[:, jt * 128:(jt + 1) * 128], identity=ident_f[0:NE, 0:NE]),
                          reads=[B_cs, B_const], writes=[B_pTps])
                for jt in range(NT):
                    cx.op(PE, lambda: nt.transpose(out=gT_ps[:, jt, :], in_=affT[:, jt * 128:(jt + 1) * 128], identity=ident_f[0:NE, 0:NE]),
                          reads=[B_aff, B_const], writes=[B_gTps])
                cx.op(V, lambda: nv.tensor_copy(out=posT[:], in_=pT_ps[:]), reads=[B_pTps], writes=[B_posT])
                cx.op(A, lambda: na.copy(out=gaT[:], in_=gT_ps[:]), reads=[B_gTps], writes=[B_gaT])
                cx.op(V, lambda: nv.tensor_copy(out=vals[:, :, :, 2], in_=gaT[:]), reads=[B_gaT], writes=[B_vals])
                cx.op(V, lambda: nv.tensor_tensor(out=r1[:], in0=gaT[:], in1=vals[:, :, :, 2], op=ALU.subtract), reads=[B_gaT, B_vals], writes=[B_r1])
                cx.op(V, lambda: nv.tensor_copy(out=vals[:, :, :, 3], in_=r1[:]), reads=[B_r1], writes=[B_vals])
                cx.op(V, lambda: nv.tensor_tensor(out=r1[:], in0=r1[:], in1=vals[:, :, :, 3], op=ALU.subtract), reads=[B_r1, B_vals], writes=[B_r1])
                cx.op(V, lambda: nv.tensor_copy(out=vals[:, :, :, 4], in_=r1[:]), reads=[B_r1], writes=[B_vals])
                for e in range(NE):
                    cx.op(P, lambda: npl.tensor_copy(out=vals[:, :, e, 0:2], in_=tokab[:]), reads=[B_tok], writes=[B_vals])
                if debug == 4:
                    d_dbg = cx.dsem("dbg4")
                    cx.op(SP, lambda: nsp.dma_start(out=dbg_thr[0, :, :], in_=lo[:]), reads=[B_b], writes=[B_out], dma=d_dbg)
                    cx.op(SP, lambda: nsp.dma_start(out=dbg_thr[1, :, :], in_=hi[:]), reads=[B_b], writes=[B_out], dma=d_dbg)
                cx.barrier()
        s4 = es.enter_context(ExitStack())
        oh = [sbuf(s4, "oh%d" % i, [128, 512], BF16) for i in range(2)]
        cmp_sb = sbuf(s4, "cmp_sb", [128, 4, 5], F32)
        idxf = [sbuf(s4, "idxf%d" % i, [128, 4], F32) for i in range(3)]
        idxi = [sbuf(s4, "idxi%d" % i, [128, 4], I32) for i in range(3)]
        gts = [sbuf(s4, "gts%d" % i, [128, 4], F32) for i in range(3)]
        idxqf = [sbuf(s4, "idxqf%d" % i, [128, 4, 4], F32) for i in range(3)]
        idxqi = [sbuf(s4, "idxqi%d" % i, [128, 4, 4], I32) for i in range(3)]
        cmp_ps = psum(s4, "cmpps", [128, 4, 5], F32)
        B_oh = [Buf() for _ in range(2)]
        B_cmps, B_cmpp = Buf(), Buf()
        B_idx = [Buf() for _ in range(3)]
        ohc = [0]

        def compact(e):
            ep = e % 3
            for jt in range(NT):
                ob = ohc[0] % 2
                ohc[0] += 1
                cx.op(V, lambda: nv.tensor_scalar(out=oh[ob][:], in0=iota512[:], scalar1=posT[:, jt, e:e + 1], scalar2=None, op0=ALU.is_equal),
                      reads=[B_iota, B_posT], writes=[B_oh[ob]])
                for cc in range(4):
                    cx.op(PE, lambda: nt.matmul(cmp_ps[:, cc, :], lhsT=oh[ob][:, cc * 128:(cc + 1) * 128], rhs=vals[:, jt, e, :],
                                                start=(jt == 0 and cc == 0), stop=(jt == NT - 1)),
                          reads=[B_oh[ob], B_vals], writes=[B_cmpp])
            cx.op(A, lambda: na.copy(out=cmp_sb[:], in_=cmp_ps[:]), reads=[B_cmpp], writes=[B_cmps])
            cx.op(V, lambda: nv.scalar_tensor_tensor(out=idxf[ep][:], in0=cmp_sb[:, :, 0], scalar=64.0, in1=cmp_sb[:, :, 1],
                                                     op0=ALU.mult, op1=ALU.add), reads=[B_cmps], writes=[B_idx[ep]])
            cx.op(V, lambda: nv.tensor_copy(out=idxi[ep][:], in_=idxf[ep][:]), reads=[B_idx[ep]], writes=[B_idx[ep]])
            for dsq in range(4):
                cx.op(V, lambda: nv.tensor_scalar(out=idxqf[ep][:, dsq, :], in0=idxf[ep][:], scalar1=4.0, scalar2=float(dsq), op0=ALU.mult, op1=ALU.add),
                      reads=[B_idx[ep]], writes=[B_idx[ep]])
            cx.op(V, lambda: nv.tensor_copy(out=idxqi[ep][:], in_=idxqf[ep][:]), reads=[B_idx[ep]], writes=[B_idx[ep]])
            cx.op(V, lambda: nv.tensor_tensor(out=gts[ep][:], in0=cmp_sb[:, :, 2], in1=cmp_sb[:, :, 3], op=ALU.add),
                  reads=[B_cmps], writes=[B_idx[ep]])
            cx.op(V, lambda: nv.tensor_tensor(out=gts[ep][:], in0=gts[ep][:], in1=cmp_sb[:, :, 4], op=ALU.add),
                  reads=[B_cmps, B_idx[ep]], writes=[B_idx[ep]])

        if debug == 4:
            d_dbg5 = cx.dsem("dbg5")
            for e in range(NE):
                compact(e)
                cx.op(SP, lambda: nsp.dma_start(out=dbg_idx[e, 0, :, :], in_=idxf[e % 3][:]), reads=[B_idx[e % 3]], writes=[B_out], dma=d_dbg5)
                cx.op(SP, lambda: nsp.dma_start(out=dbg_idx[e, 1, :, :], in_=gts[e % 3][:]), reads=[B_idx[e % 3]], writes=[B_out], dma=d_dbg5)
            cx.barrier()
            return nc

        with ExitStack() as ph:
            ring = [sbuf(ph, "ring%d" % i, [128, 16, 512], BF16) for i in range(NSLOT)]
            xs = sbuf(ph, "xs", [128, 4, D], BF16)
            xsT = sbuf(ph, "xsT", [128, 16, 512], BF16)
            gT = sbuf(ph, "gT", [128, 32, 512], BF16)
            sa = [sbuf(ph, "sa%d" % i, [128, 512], F32) for i in range(2)]
            ystage = [sbuf(ph, "ystage%d" % i, [128, 4, 512], F32) for i in range(2)]
            a_ps = [psum(ph, "aps%d" % i, [128, 512], F32) for i in range(2)]
            u_ps = [psum(ph, "ups%d" % i, [128, 512], F32) for i in range(2)]
            y_ps = [psum(ph, "yps%d" % i, [128, 512], F32) for i in range(2)]
            tp3 = psum(ph, "tp3", [128, 8, 128], BF16)
            B_ring = [Buf() for _ in range(NSLOT)]
            d_ring = [cx.dsem("rg%d" % i) for i in range(NSLOT)]
            B_xs, B_xsT, B_gT, B_tp3 = Buf(), Buf(), Buf(), Buf()
            B_ys = [Buf() for _ in range(2)]
            B_sa = [Buf() for _ in range(2)]
            B_aps = [Buf() for _ in range(2)]
            B_ups = [Buf() for _ in range(2)]
            B_yps = [Buf() for _ in range(2)]
            B_sc = [[Buf() for _ in range(16)] for _ in range(2)]
            d_xs = cx.dsem("xs")
            d_sc = [cx.dsem("sc%d" % i) for i in range(4)]
            wg_v = [w_gate_d[e].rearrange("(kc p) f -> p kc f", p=128) for e in range(NE)]
            wu_v = [w_up_d[e].rearrange("(kc p) f -> p kc f", p=128) for e in range(NE)]
            wd_v = [w_down_d[e].rearrange("(fc p) d -> p fc d", p=128) for e in range(NE)]
            acc_q = acc_d.ap().rearrange("s (a c) -> (s a) c", c=512)
            loads = []
            for e in range(NE):
                for fg in range(8):
                    loads.append(wg_v[e][:, :, fg * 512:(fg + 1) * 512])
                    loads.append(wu_v[e][:, :, fg * 512:(fg + 1) * 512])
                for ds in range(4):
                    for hf in range(2):
                        loads.append(wd_v[e][:, hf * 16:(hf + 1) * 16, ds * 512:(ds + 1) * 512])
            nxt = [0]

            def ensure_loads(upto):
                while nxt[0] <= upto and nxt[0] < len(loads):
                    k = nxt[0]
                    sl = k % NSLOT
                    src = loads[k]
                    cx.op(P, lambda: npl.dma_start(out=ring[sl][:], in_=src), writes=[B_ring[sl]], dma=d_ring[sl])
                    nxt[0] += 1

            def gather(e):
                ep = e % 3
                for cc in range(4):
                    cx.op(P, lambda: npl.indirect_dma_start(out=xs[:, cc, :], out_offset=None, in_=h2_scr[:, :],
                                                            in_offset=bass.IndirectOffsetOnAxis(ap=idxi[ep][:, cc:cc + 1], axis=0)),
                          reads=[B_idx[ep], B_h2], writes=[B_xs], dma=d_xs)

            def transposes(e):
                for cc in range(4):
                    for half in range(2):
                        for k in range(8):
                            kc = half * 8 + k
                            cx.op(PE, lambda: nt.transpose(out=tp3[:, k, :], in_=xs[:, cc, kc * 128:(kc + 1) * 128], identity=ident_bf[:]),
                                  reads=[B_xs, B_const], writes=[B_tp3])
                        dst = xsT[:, half * 8:half * 8 + 8, cc * 128:(cc + 1) * 128]
                        if half == 0:
                            cx.op(A, lambda: na.copy(out=dst, in_=tp3[:]), reads=[B_tp3], writes=[B_xsT])
                        else:
                            cx.op(V, lambda: nv.tensor_copy(out=dst, in_=tp3[:]), reads=[B_tp3], writes=[B_xsT])

            pend_sc = []

            def flush_sc():
                for (e_, ds_) in pend_sc:
                    ep_ = e_ % 3
                    for tt in range(4):
                        cx.op(P, lambda: npl.indirect_dma_start(out=acc_q,
                                                                out_offset=bass.IndirectOffsetOnAxis(ap=idxqi[ep_][:, ds_, tt:tt + 1], axis=0),
                                                                in_=ystage[ds_ % 2][:, tt, :], in_offset=None, compute_op=ALU.add),
                              reads=[B_ys[ds_ % 2], B_idx[ep_], B_acc] + B_sc[(e_ + 1) % 2], writes=[B_sc[e_ % 2][ds_ * 4 + tt]], dma=d_sc[tt])
                del pend_sc[:]

            ensure_loads(NSLOT - 1)
            compact(0)
            gather(0)
            transposes(0)
            compact(1)
            gather(1)
            abi = 0
            ybi = 0
            for e in range(NE):
                ep = e % 3
                sp_ = e % 2
                base = e * 24
                for fg in range(8):
                    kg = base + 2 * fg
                    ku = kg + 1
                    ensure_loads(ku + NSLOT - 2)
                    sg, su = kg % NSLOT, ku % NSLOT
                    if fg == 1:
                        flush_sc()
                    for fl in range(4):
                        fc = fg * 4 + fl
                        ab = abi % 2
                        abi += 1
                        for kc in range(16):
                            cx.op(PE, lambda: nt.matmul(a_ps[ab][:], lhsT=ring[sg][:, kc, fl * 128:(fl + 1) * 128], rhs=xsT[:, kc, :],
                                                        start=(kc == 0), stop=(kc == 15)),
                                  reads=[B_ring[sg], B_xsT], writes=[B_aps[ab]])
                        for kc in range(16):
                            cx.op(PE, lambda: nt.matmul(u_ps[ab][:], lhsT=ring[su][:, kc, fl * 128:(fl + 1) * 128], rhs=xsT[:, kc, :],
                                                        start=(kc == 0), stop=(kc == 15)),
                                  reads=[B_ring[su], B_xsT], writes=[B_ups[ab]])
                        cx.op(A, lambda: na.activation(out=sa[ab][:], in_=a_ps[ab][:], func=AF.Silu), reads=[B_aps[ab]], writes=[B_sa[ab]])
                        cx.op(V, lambda: nv.tensor_tensor(out=gT[:, fc, :], in0=sa[ab][:], in1=u_ps[ab][:], op=ALU.mult),
                              reads=[B_sa[ab], B_ups[ab]], writes=[B_gT])
                for ds in range(4):
                    k0 = base + 16 + 2 * ds
                    k1 = k0 + 1
                    ensure_loads(k1 + NSLOT - 2)
                    s0, s1_ = k0 % NSLOT, k1 % NSLOT
                    for tt in range(4):
                        yb = ybi % 2
                        ybi += 1
                        for fc in range(32):
                            sl = s0 if fc < 16 else s1_
                            cx.op(PE, lambda: nt.matmul(y_ps[yb][:], lhsT=gT[:, fc, tt * 128:(tt + 1) * 128], rhs=ring[sl][:, fc % 16, :],
                                                        start=(fc == 0), stop=(fc == 31)),
                                  reads=[B_gT, B_ring[sl]], writes=[B_yps[yb]])
                        cx.op(A, lambda: na.activation(out=ystage[ds % 2][:, tt, :], in_=y_ps[yb][:], func=AF.Copy,
                                                       scale=gts[ep][:, tt:tt + 1]),
                              reads=[B_yps[yb], B_idx[ep]], writes=[B_ys[ds % 2]])
                    flush_sc()
                    pend_sc.append((e, ds))
                    if ds == 0 and e + 1 < NE:
                        transposes(e + 1)
                        if e + 2 < NE:
                            compact(e + 2)
                if e + 2 < NE:
                    gather(e + 2)
            flush_sc()
            cx.barrier()

        with ExitStack() as ph:
            gfbc = sbuf(ph, "gfbc", [128, D], F32)
            a4 = [sbuf(ph, "a4_%d" % i, [128, D], F32) for i in range(2)]
            o4 = [sbuf(ph, "o4_%d" % i, [128, D], F32) for i in range(2)]
            ssq4 = sbuf(ph, "ssq4", [128, NT], F32)
            rs4 = sbuf(ph, "rs4", [128, NT], F32)
            B_gf, B_ssq4, B_rs4 = Buf(), Buf(), Buf()
            B_a4 = [Buf() for _ in range(2)]
            B_o4 = [Buf() for _ in range(2)]
            d_a4 = [cx.dsem("a40"), cx.dsem("a41")]
            cx.op(SP, lambda: nsp.dma_start(out=gfbc[:], in_=gfbc_d[:, :]), writes=[B_gf], dma=d_const)
            cx.op(V, lambda: nv.memset(ssq4[:], 0.0), writes=[B_ssq4])
            for T in range(NT):
                tq = T % 2
                cx.op(SP, lambda: nsp.dma_start(out=a4[tq][:], in_=acc_d[T * 128:(T + 1) * 128, :]),
                      reads=[B_acc] + B_sc[0] + B_sc[1], writes=[B_a4[tq]], dma=d_a4[tq])
                cx.op(A, lambda: na.activation(out=o4[tq][:], in_=a4[tq][:], func=AF.Square, accum_out=ssq4[:, T:T + 1]),
                      reads=[B_a4[tq]], writes=[B_o4[tq], B_ssq4])
                rstd_from_ssq(ssq4[:, T:T + 1], rs4[:, T:T + 1], D, [B_ssq4], [B_rs4])
                cx.op(V, lambda: nv.scalar_tensor_tensor(out=o4[tq][:], in0=a4[tq][:], scalar=rs4[:, T:T + 1], in1=gfbc[:],
                                                         op0=ALU.mult, op1=ALU.mult),
                      reads=[B_a4[tq], B_rs4, B_gf], writes=[B_o4[tq]])
                cx.op(SP, lambda: nsp.dma_start(out=out_d[T * 128:(T + 1) * 128, :], in_=o4[tq][:]),
                      reads=[B_o4[tq]], writes=[B_out], dma=d_out)
        cx.barrier()

    return nc


_CONST = None
_NC_CACHE = {}


def _shared_maps(inp):
    global _CONST
    if _CONST is None:
        _CONST = _host_constants()
    c = _CONST
    f32 = lambda a: np.ascontiguousarray(np.asarray(a, dtype=np.float32))
    bc = lambda v: np.ascontiguousarray(np.broadcast_to(f32(v).reshape(1, -1), (128, f32(v).size)))
    m = {}
    m["g1bc"] = bc(inp["norm1_g"][0])
    m["g2bc"] = bc(inp["norm2_g"][0])
    m["gfbc"] = bc(inp["final_g"])
    m["w_in"] = f32(inp["w_in"][0])
    m["pool_w"] = f32(inp["pool_w"][0])
    m["w_out"] = f32(inp["w_out"][0])
    m["w_router"] = f32(inp["w_router"][0])
    m["w_gate"] = f32(inp["w_gate"][0])
    m["w_up"] = f32(inp["w_up"][0])
    m["w_down"] = f32(inp["w_down"][0])
    m["pscale"] = np.ascontiguousarray(f32(inp["pool_scale"][0]).reshape(8, 128).T)
    gn = np.concatenate([f32(inp["gn_pool"][0]), f32(inp["gn_attn"][0])])
    m["gnfm"] = np.ascontiguousarray(gn.reshape(16, 128).T)
    sink = f32(inp["sink"][0])
    m["sinkbc"] = np.ascontiguousarray(np.broadcast_to(np.repeat(sink, 128)[None, :], (128, 1024)))
    rb = f32(inp["rel_bias"])
    bidx = c["_bidx"]
    bt = np.zeros((2, 3, 128, 4, 128), np.float32)
    for j in range(2):
        for gq in range(4):
            bt[j, :, :, gq, :] = rb[:, 4 * j + gq][bidx]
    m["biastab"] = np.ascontiguousarray(bt.reshape(6, 128, 512))
    for k in ("mask01", "ident_bf", "ident_f", "iota512", "tokab", "edgef"):
        m[k] = c[k]
    return m


def _get_nc(debug=0):
    if debug not in _NC_CACHE:
        _NC_CACHE[debug] = build_nc(debug)
    return _NC_CACHE[debug]


def kernel(**inputs):
    inp = {k: np.asarray(v) for k, v in inputs.items()}
    shared = _shared_maps(inp)
    x = np.asarray(inp["x"], dtype=np.float32)
    in_maps = []
    for c in range(NCORES):
        m = dict(shared)
        m["x"] = np.ascontiguousarray(x[c])
        in_maps.append(m)
    nc = _get_nc(0)
    res = run_bass_kernel_spmd(nc, in_maps, core_ids=list(range(NCORES)))
    out = np.stack([np.asarray(res.results[b]["out"], dtype=np.float32) for b in range(4)], axis=0)
    return out
```

```python
import os
import math
import numpy as np
import ml_dtypes
from contextlib import ExitStack
import concourse.bass as bass
import concourse.mybir as mybir
from concourse.bass_utils import run_bass_kernel_spmd

F32 = mybir.dt.float32
BF16 = mybir.dt.bfloat16
I32 = mybir.dt.int32
ALU = mybir.AluOpType
AF = mybir.ActivationFunctionType

S = 4096
D = 2048
NT = S // 128
NG = S // 512
INW = 2560
DFF = 4096
NE = 16
CAP = 512
EPS = 1e-6
NCORES = 4
NSLOT = 6
NBISECT = 31
SEM_EPOCH = 30000


class Buf:
    __slots__ = ("name", "w", "rs")

    def __init__(self, name=""):
        self.name = name
        self.w = None
        self.rs = {}


class SemC:
    __slots__ = ("sem", "count", "is_dma", "unit")

    def __init__(self, sem, is_dma, unit):
        self.sem = sem
        self.count = 0
        self.is_dma = is_dma
        self.unit = unit


class Ctx:
    def __init__(self, nc, es):
        self.nc = nc
        self.es = es
        self.engs = {"pe": nc.tensor, "act": nc.scalar, "dve": nc.vector, "pool": nc.gpsimd, "sp": nc.sync}
        self.esem = {}
        self.all_sems = []
        self.nsem = 0
        for k in self.engs:
            self._new_esem(k)
        self.waited = {k: {} for k in self.engs}
        self.nwaits = 0
        self.nops = 0

    def _new_esem(self, k):
        s = SemC(self.es.enter_context(self.nc.semaphore("e%s%d" % (k, self.nsem))), False, 1)
        self.nsem += 1
        self.esem[k] = s
        self.all_sems.append(s)

    def dsem(self, name):
        s = SemC(self.es.enter_context(self.nc.semaphore("d" + name)), True, 16)
        self.nsem += 1
        self.all_sems.append(s)
        return s

    def _wait(self, eng, semc, val):
        if semc.is_dma:
            val = semc.count * semc.unit
        if val <= 0:
            return
        w = self.waited[eng]
        if w.get(id(semc), 0) >= val:
            return
        self.engs[eng].wait_ge(semc.sem, val)
        w[id(semc)] = val
        self.nwaits += 1

    def op(self, eng, fn, reads=(), writes=(), dma=None):
        for b in reads:
            if b.w is not None:
                self._wait(eng, b.w[0], b.w[1])
        for b in writes:
            for t in b.rs.values():
                if dma is None and t[2] == eng:
                    continue
                self._wait(eng, t[0], t[1])
            t = b.w
            if t is not None and not (dma is None and t[2] == eng):
                self._wait(eng, t[0], t[1])
        inst = fn()
        self.nops += 1
        if dma is not None:
            dma.count += 1
            inst.then_inc(dma.sem, dma.unit)
            tok = (dma, dma.count * dma.unit, None)
        else:
            s = self.esem[eng]
            if s.count >= SEM_EPOCH:
                self._new_esem(eng)
                s = self.esem[eng]
            s.count += 1
            inst.then_inc(s.sem, 1)
            tok = (s, s.count, eng)
        for b in writes:
            b.w = tok
            b.rs = {}
        for b in reads:
            b.rs[id(tok[0])] = tok
        return tok

    def barrier(self):
        for e in self.engs:
            for s in self.all_sems:
                if s is self.esem.get(e):
                    continue
                self._wait(e, s, s.count * s.unit)


def _t5_bucket_np(rel):
    half, max_exact = 16, 8
    ret = np.where(rel > 0, half, 0)
    n = np.abs(rel)
    nf = np.maximum(n, 1).astype(np.float64)
    large = max_exact + np.floor(2.0 * np.log2(nf / max_exact) + 1e-6).astype(np.int64)
    large = np.minimum(large, half - 1)
    return ret + np.where(n < max_exact, n, large)


def _host_constants():
    c = {}
    c["ident_bf"] = np.eye(128, dtype=np.float32).astype(ml_dtypes.bfloat16)
    c["ident_f"] = np.eye(128, dtype=np.float32)
    c["iota512"] = np.broadcast_to(np.arange(512, dtype=np.float32), (128, 512)).copy()
    tok = (np.arange(128)[:, None] + 128 * np.arange(32)[None, :])
    tokab = np.stack([tok // 64, tok % 64], axis=-1).astype(np.float32)
    c["tokab"] = tokab.copy()
    kk = np.arange(128)[:, None]
    qq = np.arange(128)[None, :]
    mask = np.zeros((3, 128, 512), np.float32)
    bidx = np.zeros((3, 128, 128), np.int64)
    for kb in range(3):
        rel = (kb - 1) * 128 + kk - qq
        m = np.where(np.abs(rel) <= 128, 0.0, -1.0e5).astype(np.float32)
        mask[kb] = np.tile(m, (1, 4))
        bidx[kb] = _t5_bucket_np(rel)
    c["mask01"] = mask
    c["_bidx"] = bidx
    edge = np.zeros((4, 16), np.float32)
    for wi, w in enumerate((2, 4, 8, 16)):
        for i in range(16):
            t = i if i < 8 else S - 16 + i
            lo = max(t - w // 2, 0)
            hi = min(t + w // 2, S)
            edge[wi, i] = 1.0 / float(hi - lo)
    c["edgef"] = np.broadcast_to(edge[None], (128, 4, 16)).copy()
    return c


def build_nc(debug=0):
    nc = bass.Bass("TRN2", target_bir_lowering=False)
    dt_in = lambda name, shape, dt=F32: nc.dram_tensor(name, shape, dt, kind="ExternalInput")
    x_d = dt_in("x", [S, D])
    g1bc_d = dt_in("g1bc", [128, D])
    g2bc_d = dt_in("g2bc", [128, D])
    gfbc_d = dt_in("gfbc", [128, D])
    w_in_d = dt_in("w_in", [D, INW])
    pool_w_d = dt_in("pool_w", [4, 256, 256])
    w_out_d = dt_in("w_out", [D, D])
    w_router_d = dt_in("w_router", [D, NE])
    if debug == 0 or debug >= 5:
        w_gate_d = dt_in("w_gate", [NE, D, DFF])
        w_up_d = dt_in("w_up", [NE, D, DFF])
        w_down_d = dt_in("w_down", [NE, DFF, D])
    pscale_d = dt_in("pscale", [128, 8])
    gnfm_d = dt_in("gnfm", [128, 16])
    sinkbc_d = dt_in("sinkbc", [128, 1024])
    biastab_d = dt_in("biastab", [6, 128, 512])
    mask01_d = dt_in("mask01", [3, 128, 512])
    ident_bf_d = dt_in("ident_bf", [128, 128], BF16)
    ident_f_d = dt_in("ident_f", [128, 128])
    iota512_d = dt_in("iota512", [128, 512])
    tokab_d = dt_in("tokab", [128, 32, 2])
    edgef_d = dt_in("edgef", [128, 4, 16])
    out_d = nc.dram_tensor("out", [S, D], F32, kind="ExternalOutput")
    dk = lambda lv: "ExternalOutput" if debug == lv else "Internal"
    qu_scr = nc.dram_tensor("qu_scr", [16, 128, S], BF16, kind=dk(1))
    mix_scr = nc.dram_tensor("mix_scr", [16, 128, S], BF16, kind=dk(2))
    h2_scr = nc.dram_tensor("h2_scr", [S, D], BF16, kind=dk(3))
    acc_d = nc.dram_tensor("acc", [S, D], F32, kind=dk(3))
    if debug:
        dbg_aff = nc.dram_tensor("dbg_aff", [NE, S], F32, kind="ExternalOutput")
        dbg_kv = nc.dram_tensor("dbg_kv", [128, 2 * S + NT * 256], BF16, kind="ExternalOutput")
        dbg_rstd = nc.dram_tensor("dbg_rstd", [128, 64], F32, kind="ExternalOutput")
        dbg_idx = nc.dram_tensor("dbg_idx", [NE, 2, 128, 4], F32, kind="ExternalOutput")
        dbg_thr = nc.dram_tensor("dbg_thr", [2, NE, 1], F32, kind="ExternalOutput")

    qu_pm = qu_scr.ap().rearrange("c p t -> p c t")
    mix_pm = mix_scr.ap().rearrange("c p t -> p c t")

    with ExitStack() as es:
        cx = Ctx(nc, es)
        sbuf = lambda st, name, shape, dt: st.enter_context(nc.sbuf_tensor("s_" + name, shape, dt))
        psum = lambda st, name, shape, dt=F32: st.enter_context(nc.psum_tensor("p_" + name, shape, dt))
        V, A, P, PE, SP = "dve", "act", "pool", "pe", "sp"
        nv, na, npl, nt, nsp = nc.vector, nc.scalar, nc.gpsimd, nc.tensor, nc.sync

        B_qu, B_mix, B_h2, B_acc, B_out = Buf("qu"), Buf("mix"), Buf("h2"), Buf("acc"), Buf("out")
        d_out = cx.dsem("out")

        ident_bf = sbuf(es, "ident_bf", [128, 128], BF16)
        ident_f = sbuf(es, "ident_f", [128, 128], F32)
        rstd_pa = sbuf(es, "rstd_pa", [128, 64], F32)
        eps_t = sbuf(es, "eps_t", [128, 1], F32)
        B_const, B_rstd = Buf("const"), Buf("rstd")
        d_const = cx.dsem("const")
        cx.op(SP, lambda: nsp.dma_start(out=ident_bf[:], in_=ident_bf_d[:, :]), writes=[B_const], dma=d_const)
        cx.op(SP, lambda: nsp.dma_start(out=ident_f[:], in_=ident_f_d[:, :]), writes=[B_const], dma=d_const)
        cx.op(V, lambda: nv.memset(eps_t[:], EPS), writes=[B_const])

        def rstd_from_ssq(ssq_ap, out_ap, n, rb, wb):
            cx.op(A, lambda: na.activation(out=out_ap, in_=ssq_ap, func=AF.Sqrt, scale=1.0 / float(n), bias=eps_t[:, 0:1]),
                  reads=list(rb) + [B_const], writes=wb)
            cx.op(V, lambda: nv.reciprocal(out=out_ap, in_=out_ap), reads=wb, writes=wb)

        with ExitStack() as s1:
            kT_all = sbuf(s1, "kT_all", [128, 2, S], BF16)
            V_all = sbuf(s1, "V_all", [128, NT, 256], BF16)
            B_kT = [Buf("kT%d" % g) for g in range(NG)]
            B_V = [Buf("V%d" % g) for g in range(NG)]

            with ExitStack() as ph:
                w_in_sb = sbuf(ph, "w_in_sb", [128, 16, INW], BF16)
                g1bc = sbuf(ph, "g1bc", [128, D], F32)
                xbuf = [sbuf(ph, "xb%d" % i, [128, D], F32) for i in range(2)]
                hb = [sbuf(ph, "hb%d" % i, [128, D], BF16) for i in range(2)]
                hT = [sbuf(ph, "hT%d" % i, [128, 16, 512], BF16) for i in range(2)]
                qu_st = sbuf(ph, "qu_st", [128, 16, 512], BF16)
                ssq = sbuf(ph, "ssq", [128, NT], F32)
                rs1 = sbuf(ph, "rs1", [128, NT], F32)
                tp = [psum(ph, "tp%d" % i, [128, 8, 128], BF16) for i in range(3)]
                pp = [psum(ph, "pp%d" % i, [128, 512], F32) for i in range(5)]
                B_w, B_g1 = Buf("w_in"), Buf("g1")
                B_x = [Buf() for _ in range(2)]
                B_hb = [Buf() for _ in range(2)]
                B_hT = [Buf() for _ in range(2)]
                B_ssq = [Buf() for _ in range(2)]
                B_rs = [Buf() for _ in range(2)]
                B_tp = [Buf() for _ in range(3)]
                B_pp = [Buf() for _ in range(5)]
                B_qs = Buf("qu_st")
                d_w, d_x, d_qs = cx.dsem("w1"), [cx.dsem("x0"), cx.dsem("x1")], cx.dsem("qs")
                w_in_v = w_in_d.ap().rearrange("(kc p) n -> p kc n", p=128)
                for q4 in range(4):
                    cx.op(P, lambda: npl.dma_start(out=w_in_sb[:, 4 * q4:4 * q4 + 4, :], in_=w_in_v[:, 4 * q4:4 * q4 + 4, :]),
                          writes=[B_w], dma=d_w)
                cx.op(SP, lambda: nsp.dma_start(out=g1bc[:], in_=g1bc_d[:, :]), writes=[B_g1], dma=d_const)
                cx.op(V, lambda: nv.memset(ssq[:], 0.0), writes=B_ssq)
                c1 = {"tp": 0, "pp": 0, "ev": 0}

                def norm_tile(g, i):
                    T = 4 * g + i
                    tpar = T % 2
                    cx.op(SP, lambda: nsp.dma_start(out=xbuf[tpar][:], in_=x_d[T * 128:(T + 1) * 128, :]),
                          writes=[B_x[tpar]], dma=d_x[tpar])
                    cx.op(A, lambda: na.activation(out=hb[tpar][:], in_=xbuf[tpar][:], func=AF.Square,
                                                   accum_out=ssq[:, T:T + 1]),
                          reads=[B_x[tpar]], writes=[B_hb[tpar], B_ssq[tpar]])
                    rstd_from_ssq(ssq[:, T:T + 1], rs1[:, T:T + 1], D, [B_ssq[tpar]], [B_rs[tpar]])
                    cx.op(V, lambda: nv.scalar_tensor_tensor(out=hb[tpar][:], in0=xbuf[tpar][:], scalar=rs1[:, T:T + 1],
                                                             in1=g1bc[:], op0=ALU.mult, op1=ALU.mult),
                          reads=[B_x[tpar], B_rs[tpar], B_g1], writes=[B_hb[tpar]])

                def tr_tile(g, i):
                    T = 4 * g + i
                    tpar = T % 2
                    gp = g % 2
                    for half in range(2):
                        tb = c1["tp"] % 3
                        c1["tp"] += 1
                        for k in range(8):
                            kc = half * 8 + k
                            cx.op(PE, lambda: nt.transpose(out=tp[tb][:, k, :], in_=hb[tpar][:, kc * 128:(kc + 1) * 128],
                                                           identity=ident_bf[:]),
                                  reads=[B_hb[tpar], B_const], writes=[B_tp[tb]])
                        dst = hT[gp][:, half * 8:half * 8 + 8, i * 128:(i + 1) * 128]
                        if half == 0:
                            cx.op(A, lambda: na.copy(out=dst, in_=tp[tb][:]), reads=[B_tp[tb]], writes=[B_hT[gp]])
                        else:
                            cx.op(V, lambda: nv.tensor_copy(out=dst, in_=tp[tb][:]), reads=[B_tp[tb]], writes=[B_hT[gp]])

                def proj_chain(g, oc):
                    gp = g % 2
                    pb = c1["pp"] % 5
                    c1["pp"] += 1
                    c1["ev"] += 1
                    if oc < 18:
                        c0 = oc * 128
                        for kc in range(16):
                            cx.op(PE, lambda: nt.matmul(pp[pb][:], lhsT=w_in_sb[:, kc, c0:c0 + 128], rhs=hT[gp][:, kc, :],
                                                        start=(kc == 0), stop=(kc == 15)),
                                  reads=[B_w, B_hT[gp]], writes=[B_pp[pb]])
                        if oc < 16:
                            dst, wb = qu_st[:, oc, :], B_qs
                        else:
                            dst, wb = kT_all[:, oc - 16, g * 512:(g + 1) * 512], B_kT[g]
                        src = pp[pb][:]
                    else:
                        i = oc - 18
                        for kc in range(16):
                            cx.op(PE, lambda: nt.matmul(pp[pb][:, 0:256], lhsT=hT[gp][:, kc, i * 128:(i + 1) * 128],
                                                        rhs=w_in_sb[:, kc, 2304:2560], start=(kc == 0), stop=(kc == 15)),
                                  reads=[B_w, B_hT[gp]], writes=[B_pp[pb]])
                        dst, wb, src = V_all[:, 4 * g + i, :], B_V[g], pp[pb][:, 0:256]
                    if c1["ev"] % 2 == 0:
                        cx.op(A, lambda: na.copy(out=dst, in_=src), reads=[B_pp[pb]], writes=[wb])
                    else:
                        cx.op(V, lambda: nv.tensor_copy(out=dst, in_=src), reads=[B_pp[pb]], writes=[wb])
                    if oc == 15:
                        cx.op(SP, lambda: nsp.dma_start(out=qu_pm[:, :, g * 512:(g + 1) * 512], in_=qu_st[:]),
                              reads=[B_qs], writes=[B_qu], dma=d_qs)

                for i in range(4):
                    norm_tile(0, i)
                    tr_tile(0, i)
                chunks = [list(range(0, 6)), list(range(6, 12)), list(range(12, 17)), list(range(17, 22))]
                for g in range(NG):
                    for c in range(4):
                        if g + 1 < NG:
                            norm_tile(g + 1, c)
                        for oc in chunks[c]:
                            proj_chain(g, oc)
                        if g + 1 < NG:
                            tr_tile(g + 1, c)
                if debug:
                    d_dbg = cx.dsem("dbg")
                    cx.op(SP, lambda: nsp.dma_start(out=dbg_kv[:, 0:2 * S], in_=kT_all[:]), reads=B_kT, writes=[B_out], dma=d_dbg)
                    cx.op(SP, lambda: nsp.dma_start(out=dbg_kv[:, 2 * S:], in_=V_all[:]), reads=B_V, writes=[B_out], dma=d_dbg)
                cx.barrier()
            if debug == 1:
                cx.barrier()
                return nc

            with ExitStack() as ph:
                eb = sbuf(ph, "eb", [128, 6, 512], F32)
                Bs = sbuf(ph, "Bs", [128, 6, 512], BF16)
                B_Bs = Buf()
                mk = sbuf(ph, "mk", [128, 3, 512], F32)
                sinkexp = sbuf(ph, "sinkexp", [128, 1024], F32)
                ones_bf = sbuf(ph, "ones_bf", [128, 128], BF16)
                pw_sb = sbuf(ph, "pw_sb", [128, 4, 2, 256], BF16)
                pscale = sbuf(ph, "pscale", [128, 8], F32)
                edgef = sbuf(ph, "edgef", [128, 4, 16], F32)
                qT = [sbuf(ph, "qT%d" % i, [128, 8, 512], BF16) for i in range(2)]
                uT = [sbuf(ph, "uT%d" % i, [128, 8, 528], BF16) for i in range(2)]
                tmp = [sbuf(ph, "ptmp%d" % i, [128, 2, 528], F32) for i in range(4)]
                etmp = sbuf(ph, "etmp", [128, 2, 8], F32)
                tmp4 = sbuf(ph, "ptmp4", [128, 2, 512], F32)
                pooledT = sbuf(ph, "pooledT", [128, 8, 512], BF16)
                sqp = sbuf(ph, "sqp", [128, 8, 512], BF16)
                et = [sbuf(ph, "et%d" % i, [128, 512], F32) for i in range(2)]
                pt = [sbuf(ph, "pt%d" % i, [128, 512], BF16) for i in range(9)]
                den = [sbuf(ph, "den%d" % i, [128, 512], F32) for i in range(2)]
                at = [sbuf(ph, "at%d" % i, [128, 4, 128], F32) for i in range(2)]
                sqa = [sbuf(ph, "sqa%d" % i, [128, 4, 128], BF16) for i in range(4)]
                mixst = [sbuf(ph, "mixst%d" % i, [128, 16, 512], BF16) for i in range(2)]
                sps = [psum(ph, "sps%d" % i, [128, 512], F32) for i in range(2)]
                o_ps = [psum(ph, "ops%d" % i, [128, 4, 128], F32) for i in range(2)]
                d_ps = [psum(ph, "dps%d" % i, [128, 512], F32) for i in range(2)]
                pw_ps = psum(ph, "pwps", [128, 512], F32)
                ss_ps = psum(ph, "ssps", [128, 512], F32)
                B_eb, B_mk, B_sk, B_pw, B_c2 = Buf(), Buf(), Buf(), Buf(), Buf()
                B_q = [Buf() for _ in range(2)]
                B_u = [Buf() for _ in range(2)]
                B_tmp, B_etmp, B_pooled, B_sqp = Buf(), Buf(), Buf(), Buf()
                B_et = [Buf() for _ in range(2)]
                B_pt = [Buf() for _ in range(9)]
                B_den = [Buf() for _ in range(2)]
                B_at = [Buf() for _ in range(2)]
                B_sqa = [Buf() for _ in range(4)]
                B_mixst = [Buf() for _ in range(2)]
                B_sps = [Buf() for _ in range(2)]
                B_ops = [Buf() for _ in range(2)]
                B_dps = [Buf() for _ in range(2)]
                B_pwps, B_ss = Buf(), Buf()
                d_c2, d_q, d_u, d_ms = cx.dsem("c2"), [cx.dsem("q0"), cx.dsem("q1")], [cx.dsem("u0"), cx.dsem("u1")], [cx.dsem("ms0"), cx.dsem("ms1")]
                d_pw = cx.dsem("pw")
                cx.op(SP, lambda: nsp.dma_start(out=eb[:], in_=biastab_d.ap().rearrange("j p c -> p j c")), writes=[B_eb], dma=d_c2)
                cx.op(SP, lambda: nsp.dma_start(out=mk[:], in_=mask01_d.ap().rearrange("j p c -> p j c")), writes=[B_mk], dma=d_c2)
                cx.op(SP, lambda: nsp.dma_start(out=sinkexp[:], in_=sinkbc_d[:, :]), writes=[B_sk], dma=d_c2)
                cx.op(SP, lambda: nsp.dma_start(out=pscale[:], in_=pscale_d[:, :]), writes=[B_c2], dma=d_c2)
                cx.op(SP, lambda: nsp.dma_start(out=edgef[:], in_=edgef_d[:, :, :]), writes=[B_c2], dma=d_c2)
                cx.op(P, lambda: npl.dma_start(out=pw_sb[:], in_=pool_w_d.ap().rearrange("g (kc p) d -> p g kc d", p=128)),
                      writes=[B_pw], dma=d_pw)
                cx.op(V, lambda: nv.memset(ones_bf[:], 1.0), writes=[B_c2])
                cx.op(A, lambda: na.activation(out=sinkexp[:], in_=sinkexp[:], func=AF.Exp), reads=[B_sk], writes=[B_sk])
                for jk in range(6):
                    cx.op(V, lambda: nv.scalar_tensor_tensor(out=Bs[:, jk, :], in0=eb[:, jk, :], scalar=1.0 / (128.0 ** -0.5), in1=mk[:, jk % 3, :],
                                                             op0=ALU.mult, op1=ALU.add),
                          reads=[B_eb, B_mk], writes=[B_Bs])
                cx.op(V, lambda: nv.memset(uT[0][:, :, 0:8], 0.0), writes=[B_u[0]])
                SCALE = 128.0 ** -0.5
                ctr = {"sp": 0, "pt": 0, "bk": 0, "sq": 0}
                NPT = len(pt)

                def emit_loads(g):
                    gp = g % 2
                    t0 = g * 512
                    cx.op(SP, lambda: nsp.dma_start(out=qT[gp][:], in_=qu_pm[:, 8:16, t0:t0 + 512]),
                          reads=[B_qu], writes=[B_q[gp]], dma=d_q[gp])
                    if g == 0:
                        cx.op(SP, lambda: nsp.dma_start(out=uT[gp][:, :, 8:528], in_=qu_pm[:, 0:8, 0:520]),
                              reads=[B_qu], writes=[B_u[gp]], dma=d_u[gp])
                    elif g == NG - 1:
                        cx.op(V, lambda: nv.memset(uT[gp][:, :, 520:528], 0.0), writes=[B_u[gp]])
                        cx.op(SP, lambda: nsp.dma_start(out=uT[gp][:, :, 0:520], in_=qu_pm[:, 0:8, t0 - 8:S]),
                              reads=[B_qu], writes=[B_u[gp]], dma=d_u[gp])
                    else:
                        cx.op(SP, lambda: nsp.dma_start(out=uT[gp][:], in_=qu_pm[:, 0:8, t0 - 8:t0 + 520]),
                              reads=[B_qu], writes=[B_u[gp]], dma=d_u[gp])

                def stage_A(g, i, j):
                    gp = g % 2
                    Bk = 4 * g + i
                    pts = []
                    for kb in range(3):
                        KB = Bk + kb - 1
                        if KB < 0 or KB >= NT:
                            continue
                        sb_ = ctr["sp"] % 2
                        ctr["sp"] += 1
                        cx.op(PE, lambda: nt.matmul(sps[sb_][:], lhsT=kT_all[:, j, KB * 128:(KB + 1) * 128],
                                                    rhs=qT[gp][:, 4 * j:4 * j + 4, i * 128:(i + 1) * 128], start=True, stop=False),
                              reads=[B_kT[KB // 4], B_q[gp]], writes=[B_sps[sb_]])
                        cx.op(PE, lambda: nt.matmul(sps[sb_][:], lhsT=ident_bf[:], rhs=Bs[:, j * 3 + kb, :], start=False, stop=True),
                              reads=[B_const, B_Bs], writes=[B_sps[sb_]])
                        pb_ = ctr["pt"] % NPT
                        ctr["pt"] += 1
                        cx.op(A, lambda: na.activation(out=pt[pb_][:], in_=sps[sb_][:], func=AF.Exp, scale=SCALE),
                              reads=[B_sps[sb_]], writes=[B_pt[pb_]])
                        pts.append((pb_, KB))
                    return pts

                def stage_B(g, i, j, pts):
                    gp = g % 2
                    ob = ctr["bk"] % 2
                    ctr["bk"] += 1
                    for n, (pb_, KB) in enumerate(pts):
                        cx.op(PE, lambda: nt.matmul(o_ps[ob][:], lhsT=V_all[:, KB, j * 128:(j + 1) * 128], rhs=pt[pb_][:],
                                                    start=(n == 0), stop=(n == len(pts) - 1)),
                              reads=[B_V[KB // 4], B_pt[pb_]], writes=[B_ops[ob]])
                    for n, (pb_, KB) in enumerate(pts):
                        cx.op(PE, lambda: nt.matmul(d_ps[ob][:], lhsT=ones_bf[:], rhs=pt[pb_][:],
                                                    start=(n == 0), stop=(n == len(pts) - 1)),
                              reads=[B_c2, B_pt[pb_]], writes=[B_dps[ob]])
                    cx.op(V, lambda: nv.tensor_tensor(out=den[ob][:], in0=d_ps[ob][:], in1=sinkexp[:, j * 512:(j + 1) * 512], op=ALU.add),
                          reads=[B_dps[ob], B_sk], writes=[B_den[ob]])
                    cx.op(V, lambda: nv.reciprocal(out=den[ob][:], in_=den[ob][:]), reads=[B_den[ob]], writes=[B_den[ob]])
                    cx.op(V, lambda: nv.tensor_tensor(out=at[ob][:], in0=o_ps[ob][:],
                                                      in1=den[ob][:].rearrange("p (a b) -> p a b", b=128), op=ALU.mult),
                          reads=[B_ops[ob], B_den[ob]], writes=[B_at[ob]])
                    sb2 = ctr["sq"] % 4
                    ctr["sq"] += 1
                    cx.op(A, lambda: na.activation(out=sqa[sb2][:], in_=at[ob][:], func=AF.Square),
                          reads=[B_at[ob]], writes=[B_sqa[sb2]])
                    cx.op(V, lambda: nv.tensor_copy(out=mixst[gp][:, 8 + 4 * j:8 + 4 * j + 4, i * 128:(i + 1) * 128], in_=at[ob][:]),
                          reads=[B_at[ob]], writes=[B_mixst[gp]])
                    return sb2

                def stage_C(i, sq_pair):
                    n = 0
                    for sb2 in sq_pair:
                        for gq in range(4):
                            cx.op(PE, lambda: nt.matmul(ss_ps[:, i:i + 1], lhsT=sqa[sb2][:, gq, :], rhs=ones_bf[:, 0:1],
                                                        start=(n == 0), stop=(n == 7)),
                                  reads=[B_sqa[sb2], B_c2], writes=[B_ss])
                            n += 1

                def pooling(g, grp):
                    gp = g % 2
                    w = (2, 4, 8, 16)[grp]
                    U = uT[gp][:, 2 * grp:2 * grp + 2, :]
                    cx.op(V, lambda: nv.tensor_tensor(out=tmp[0][:, :, 1:528], in0=U[:, :, 0:527], in1=U[:, :, 1:528], op=ALU.add),
                          reads=[B_u[gp]], writes=[B_tmp])
                    if grp >= 1:
                        cx.op(V, lambda: nv.tensor_tensor(out=tmp[1][:, :, 2:527], in0=tmp[0][:, :, 1:526], in1=tmp[0][:, :, 3:528], op=ALU.add),
                              reads=[B_tmp], writes=[B_tmp])
                    if grp >= 2:
                        cx.op(V, lambda: nv.tensor_tensor(out=tmp[2][:, :, 4:525], in0=tmp[1][:, :, 2:523], in1=tmp[1][:, :, 6:527], op=ALU.add),
                              reads=[B_tmp], writes=[B_tmp])
                    if grp >= 3:
                        cx.op(V, lambda: nv.tensor_tensor(out=tmp[3][:, :, 8:521], in0=tmp[2][:, :, 4:517], in1=tmp[2][:, :, 12:525], op=ALU.add),
                              reads=[B_tmp], writes=[B_tmp])
                    sw = tmp[grp]
                    cx.op(V, lambda: nv.scalar_tensor_tensor(out=pooledT[:, 2 * grp:2 * grp + 2, :], in0=sw[:, :, 8:520], scalar=1.0 / w,
                                                             in1=U[:, :, 8:520], op0=ALU.mult, op1=ALU.subtract),
                          reads=[B_tmp, B_u[gp]], writes=[B_pooled])
                    edges = []
                    if g == 0:
                        edges.append((8, 0, 0))
                    if g == NG - 1:
                        edges.append((512, 504, 8))
                    for (uc, pc, ec) in edges:
                        for ch in range(2):
                            cx.op(V, lambda: nv.tensor_tensor(out=etmp[:, ch, :], in0=sw[:, ch, uc:uc + 8], in1=edgef[:, grp, ec:ec + 8], op=ALU.mult),
                                  reads=[B_tmp, B_c2], writes=[B_etmp])
                            cx.op(V, lambda: nv.tensor_tensor(out=pooledT[:, 2 * grp + ch, pc:pc + 8], in0=etmp[:, ch, :],
                                                              in1=uT[gp][:, 2 * grp + ch, uc:uc + 8], op=ALU.subtract),
                                  reads=[B_etmp, B_u[gp]], writes=[B_pooled])

                def group_end(g):
                    gp = g % 2
                    t0 = g * 512
                    for oc in range(8):
                        grp, hf = oc // 2, oc % 2
                        for kc in range(2):
                            cx.op(PE, lambda: nt.matmul(pw_ps[:], lhsT=pw_sb[:, grp, kc, hf * 128:(hf + 1) * 128], rhs=pooledT[:, 2 * grp + kc, :],
                                                        start=(kc == 0), stop=(kc == 1)),
                                  reads=[B_pw, B_pooled], writes=[B_pwps])
                        cx.op(A, lambda: na.activation(out=mixst[gp][:, oc, :], in_=pw_ps[:], func=AF.Copy, scale=pscale[:, oc:oc + 1]),
                              reads=[B_pwps, B_c2], writes=[B_mixst[gp]])
                        cx.op(A, lambda: na.activation(out=sqp[:, oc, :], in_=pw_ps[:], func=AF.Square, scale=pscale[:, oc:oc + 1]),
                              reads=[B_pwps, B_c2], writes=[B_sqp])
                    for i in range(4):
                        for oc in range(8):
                            cx.op(PE, lambda: nt.matmul(ss_ps[:, 4 + i:5 + i], lhsT=sqp[:, oc, i * 128:(i + 1) * 128], rhs=ones_bf[:, 0:1],
                                                        start=(oc == 0), stop=(oc == 7)),
                                  reads=[B_sqp, B_c2], writes=[B_ss])
                    rstd_from_ssq(ss_ps[:, 0:4], rstd_pa[:, 32 + 4 * g:32 + 4 * g + 4], 1024, [B_ss], [B_rstd])
                    rstd_from_ssq(ss_ps[:, 4:8], rstd_pa[:, 4 * g:4 * g + 4], 1024, [B_ss], [B_rstd])
                    cx.op(SP, lambda: nsp.dma_start(out=mix_pm[:, :, t0:t0 + 512], in_=mixst[gp][:]),
                          reads=[B_mixst[gp]], writes=[B_mix], dma=d_ms[gp])

                units = [(g, i, j) for g in range(NG) for i in range(4) for j in range(2)]
                sq_of = {}
                pend = []

                def do_B(u, pts):
                    g, i, j = u
                    sb2 = stage_B(g, i, j, pts)
                    sq_of.setdefault((g, i), []).append(sb2)
                    for fn in pend:
                        fn()
                    del pend[:]
                    if j == 0:
                        pooling(g, i)
                    if j == 1:
                        pair = sq_of.pop((g, i))
                        pend.append(lambda: stage_C(i, pair))
                        if i == 3:
                            pend.append(lambda: group_end(g))

                prev = None
                for u in units:
                    g, i, j = u
                    if i == 0 and j == 0:
                        emit_loads(g)
                    pts = stage_A(g, i, j)
                    if prev is not None:
                        do_B(*prev)
                    prev = (u, pts)
                do_B(*prev)
                for fn in pend:
                    fn()
                del pend[:]
                if debug == 2:
                    d_dbg = cx.dsem("dbg2")
                    cx.op(SP, lambda: nsp.dma_start(out=dbg_rstd[:, :], in_=rstd_pa[:]), reads=[B_rstd], writes=[B_out], dma=d_dbg)
                cx.barrier()
        if debug == 2:
            cx.barrier()
            return nc

        s3 = es.enter_context(ExitStack())
        posT = sbuf(s3, "posT", [128, NT, NE], F32)
        gaT = sbuf(s3, "gaT", [128, NT, NE], F32)
        vals = sbuf(s3, "vals", [128, NT, NE, 5], BF16)
        iota512 = sbuf(s3, "iota512", [128, 512], F32)
        B_posT, B_gaT, B_vals, B_iota = Buf(), Buf(), Buf(), Buf()
        with ExitStack() as s2:
            affT = sbuf(s2, "affT", [NE, S], F32)
            B_aff = Buf("aff")
            with ExitStack() as ph:
                w_out_sb = sbuf(ph, "w_out_sb", [128, 16, D], BF16)
                gnfm = sbuf(ph, "gnfm", [128, 16], F32)
                wr_sb = sbuf(ph, "wr_sb", [128, 16, NE], F32)
                wr_hi = sbuf(ph, "wr_hi", [128, 16, NE], BF16)
                wr_lo = sbuf(ph, "wr_lo", [128, 16, NE], BF16)
                h2lo = [sbuf(ph, "h2lo%d" % i, [128, D], BF16) for i in range(2)]
                hiT = sbuf(ph, "hiT", [128, 16, 128], BF16)
                loT = sbuf(ph, "loT", [128, 16, 128], BF16)
                B_hiT, B_loT = Buf(), Buf()
                B_h2lo = [Buf() for _ in range(2)]
                g2bc = sbuf(ph, "g2bc", [128, D], F32)
                mixld = [sbuf(ph, "mixld%d" % i, [128, 16, 512], BF16) for i in range(2)]
                xt = [sbuf(ph, "xt%d" % i, [128, D], F32) for i in range(2)]
                x1t = [sbuf(ph, "x1t%d" % i, [128, D], F32) for i in range(2)]
                h2f = sbuf(ph, "h2f", [128, D], F32)
                h2b = [sbuf(ph, "h2b%d" % i, [128, D], BF16) for i in range(2)]
                ex = sbuf(ph, "ex", [NE, 512], F32)
                rc = sbuf(ph, "rc", [NE, 512], F32)
                ones16 = sbuf(ph, "ones16", [NE, NE], F32)
                ssq2 = sbuf(ph, "ssq2", [128, NT], F32)
                rs2 = sbuf(ph, "rs2", [128, NT], F32)
                P1 = [psum(ph, "P1_%d" % i, [128, 512], F32) for i in range(2)]
                P2 = [psum(ph, "P2_%d" % i, [128, 512], F32) for i in range(2)]
                tpf = [psum(ph, "tpf%d" % i, [128, 8, 128], BF16) for i in range(2)]
                lg_ps = [psum(ph, "lgps%d" % i, [NE, 512], F32) for i in range(2)]
                B_wo, B_ws, B_c3, B_g2 = Buf(), Buf(), Buf(), Buf()
                B_ml = [Buf() for _ in range(2)]
                B_xt = [Buf() for _ in range(2)]
                B_x1 = [Buf() for _ in range(2)]
                B_h2f = Buf()
                B_h2b = [Buf() for _ in range(2)]
                B_h2T, B_ex, B_rc, B_ssq2, B_rs2 = Buf(), Buf(), Buf(), Buf(), Buf()
                B_P1 = [Buf() for _ in range(2)]
                B_P2 = [Buf() for _ in range(2)]
                B_tpf = [Buf() for _ in range(2)]
                B_lg = [Buf() for _ in range(2)]
                B_sm = Buf()
                d_c3, d_ws = cx.dsem("c3"), cx.dsem("ws")
                d_ml, d_xt = [cx.dsem("ml0"), cx.dsem("ml1")], [cx.dsem("xt0"), cx.dsem("xt1")]
                d_x1, d_h2 = [cx.dsem("x1s0"), cx.dsem("x1s1")], [cx.dsem("h2s0"), cx.dsem("h2s1")]
                cx.op(SP, lambda: nsp.dma_start(out=gnfm[:], in_=gnfm_d[:, :]), writes=[B_c3], dma=d_c3)
                cx.op(SP, lambda: nsp.dma_start(out=g2bc[:], in_=g2bc_d[:, :]), writes=[B_g2], dma=d_c3)
                cx.op(SP, lambda: nsp.dma_start(out=wr_sb[:], in_=w_router_d.ap().rearrange("(kc p) e -> p kc e", p=128)),
                      writes=[B_c3], dma=d_c3)
                cx.op(V, lambda: nv.memset(ones16[:], 1.0), writes=[B_c3])
                cx.op(V, lambda: nv.memset(ssq2[:], 0.0), writes=[B_ssq2])
                cx.op(V, lambda: nv.tensor_copy(out=wr_hi[:], in_=wr_sb[:]), reads=[B_c3], writes=[B_c3])
                cx.op(V, lambda: nv.tensor_tensor(out=wr_sb[:], in0=wr_sb[:], in1=wr_hi[:], op=ALU.subtract), reads=[B_c3], writes=[B_c3])
                cx.op(V, lambda: nv.tensor_copy(out=wr_lo[:], in_=wr_sb[:]), reads=[B_c3], writes=[B_c3])
                for kc in range(16):
                    wst = h2f if kc % 2 == 0 else xt[0]
                    wB = B_h2f if kc % 2 == 0 else B_xt[0]
                    cx.op(SP, lambda: nsp.dma_start(out=wst[:], in_=w_out_d[kc * 128:(kc + 1) * 128, :]), writes=[wB], dma=d_ws)
                    cx.op(V, lambda: nv.tensor_scalar(out=w_out_sb[:, kc, :], in0=wst[:], scalar1=gnfm[:, kc:kc + 1], scalar2=None, op0=ALU.mult),
                          reads=[wB, B_c3], writes=[B_wo])
                pend_rt = []
                rt_done = {}
                tfc = [0]

                def flush_router():
                    for (g_, i_, tq_) in pend_rt:
                        for (src, srcB, dstT, dstB) in ((h2b[tq_], B_h2b[tq_], hiT, B_hiT), (h2lo[tq_], B_h2lo[tq_], loT, B_loT)):
                            for half in range(2):
                                tb = tfc[0] % 2
                                tfc[0] += 1
                                for k in range(8):
                                    kc = half * 8 + k
                                    cx.op(PE, lambda: nt.transpose(out=tpf[tb][:, k, :], in_=src[:, kc * 128:(kc + 1) * 128], identity=ident_bf[:]),
                                          reads=[srcB, B_const], writes=[B_tpf[tb]])
                                if half == 0:
                                    cx.op(A, lambda: na.copy(out=dstT[:, half * 8:half * 8 + 8, :], in_=tpf[tb][:]), reads=[B_tpf[tb]], writes=[dstB])
                                else:
                                    cx.op(V, lambda: nv.tensor_copy(out=dstT[:, half * 8:half * 8 + 8, :], in_=tpf[tb][:]), reads=[B_tpf[tb]], writes=[dstB])
                        n = 0
                        for (wsb, rT, rB) in ((wr_hi, hiT, B_hiT), (wr_hi, loT, B_loT), (wr_lo, hiT, B_hiT)):
                            for kc in range(16):
                                cx.op(PE, lambda: nt.matmul(lg_ps[g_ % 2][:, i_ * 128:(i_ + 1) * 128], lhsT=wsb[:, kc, :], rhs=rT[:, kc, :],
                                                            start=(n == 0), stop=(n == 47)),
                                      reads=[B_c3, rB], writes=[B_lg[g_ % 2]])
                                n += 1
                        rt_done[g_] = rt_done.get(g_, 0) + 1
                    del pend_rt[:]

                sm_done = set()

                def softmax_group(g_, final):
                    for gg in range(NG):
                        if gg in sm_done or rt_done.get(gg, 0) < 4:
                            continue
                        sm_done.add(gg)
                        tg = gg * 512
                        cx.op(A, lambda: na.activation(out=ex[:], in_=lg_ps[gg % 2][:], func=AF.Exp), reads=[B_lg[gg % 2]], writes=[B_ex])
                        cx.op(PE, lambda: nt.matmul(P1[0][0:NE, :], lhsT=ones16[:], rhs=ex[:], start=True, stop=True),
                              reads=[B_c3, B_ex], writes=[B_P1[0]])
                        cx.op(V, lambda: nv.reciprocal(out=rc[:], in_=P1[0][0:NE, :]), reads=[B_P1[0]], writes=[B_rc])
                        cx.op(V, lambda: nv.tensor_tensor(out=affT[:, tg:tg + 512], in0=ex[:], in1=rc[:], op=ALU.mult),
                              reads=[B_ex, B_rc], writes=[B_aff])

                pbi = 0
                tfi = 0
                for g in range(NG):
                    gp = g % 2
                    t0 = g * 512
                    cx.op(SP, lambda: nsp.dma_start(out=mixld[gp][:], in_=mix_pm[:, :, t0:t0 + 512]),
                          reads=[B_mix], writes=[B_ml[gp]], dma=d_ml[gp])
                    for i in range(4):
                        T = 4 * g + i
                        tq = T % 2
                        cx.op(SP, lambda: nsp.dma_start(out=xt[tq][:], in_=x_d[T * 128:(T + 1) * 128, :]), writes=[B_xt[tq]], dma=d_xt[tq])
                        for ds in range(4):
                            pb = pbi % 2
                            pbi += 1
                            dsl = slice(ds * 512, (ds + 1) * 512)
                            for kc in range(8):
                                cx.op(PE, lambda: nt.matmul(P1[pb][:], lhsT=mixld[gp][:, kc, i * 128:(i + 1) * 128], rhs=w_out_sb[:, kc, dsl],
                                                            start=(kc == 0), stop=(kc == 7)),
                                      reads=[B_ml[gp], B_wo], writes=[B_P1[pb]])
                            for kc in range(8, 16):
                                cx.op(PE, lambda: nt.matmul(P2[pb][:], lhsT=mixld[gp][:, kc, i * 128:(i + 1) * 128], rhs=w_out_sb[:, kc, dsl],
                                                            start=(kc == 8), stop=(kc == 15)),
                                      reads=[B_ml[gp], B_wo], writes=[B_P2[pb]])
                            cx.op(V, lambda: nv.scalar_tensor_tensor(out=x1t[tq][:, dsl], in0=P1[pb][:], scalar=rstd_pa[:, T:T + 1],
                                                                     in1=xt[tq][:, dsl], op0=ALU.mult, op1=ALU.add),
                                  reads=[B_P1[pb], B_rstd, B_xt[tq]], writes=[B_x1[tq]])
                            cx.op(V, lambda: nv.scalar_tensor_tensor(out=x1t[tq][:, dsl], in0=P2[pb][:], scalar=rstd_pa[:, 32 + T:33 + T],
                                                                     in1=x1t[tq][:, dsl], op0=ALU.mult, op1=ALU.add),
                                  reads=[B_P2[pb], B_rstd, B_x1[tq]], writes=[B_x1[tq]])
                        cx.op(SP, lambda: nsp.dma_start(out=acc_d[T * 128:(T + 1) * 128, :], in_=x1t[tq][:]),
                              reads=[B_x1[tq]], writes=[B_acc], dma=d_x1[tq])
                        cx.op(A, lambda: na.activation(out=h2f[:], in_=x1t[tq][:], func=AF.Square, accum_out=ssq2[:, T:T + 1]),
                              reads=[B_x1[tq]], writes=[B_h2f, B_ssq2])
                        rstd_from_ssq(ssq2[:, T:T + 1], rs2[:, T:T + 1], D, [B_ssq2], [B_rs2])
                        cx.op(V, lambda: nv.scalar_tensor_tensor(out=h2f[:], in0=x1t[tq][:], scalar=rs2[:, T:T + 1], in1=g2bc[:],
                                                                 op0=ALU.mult, op1=ALU.mult),
                              reads=[B_x1[tq], B_rs2, B_g2], writes=[B_h2f])
                        cx.op(A, lambda: na.copy(out=h2b[tq][:], in_=h2f[:]), reads=[B_h2f], writes=[B_h2b[tq]])
                        cx.op(SP, lambda: nsp.dma_start(out=h2_scr[T * 128:(T + 1) * 128, :], in_=h2b[tq][:]),
                              reads=[B_h2b[tq]], writes=[B_h2], dma=d_h2[tq])
                        cx.op(V, lambda: nv.tensor_tensor(out=h2lo[tq][:], in0=h2f[:], in1=h2b[tq][:], op=ALU.subtract),
                              reads=[B_h2f, B_h2b[tq]], writes=[B_h2lo[tq]])
                        flush_router()
                        pend_rt.append((g, i, tq))
                    softmax_group(g - 1 if g > 0 else None, final=False)
                flush_router()
                softmax_group(NG - 1, final=True)
                if debug in (3, 4):
                    d_dbg = cx.dsem("dbg3")
                    cx.op(SP, lambda: nsp.dma_start(out=dbg_aff[:, :], in_=affT[:]), reads=[B_aff], writes=[B_out], dma=d_dbg)
                cx.barrier()
            if debug == 3:
                cx.barrier()
                return nc

            with ExitStack() as ph:
                lo = sbuf(ph, "lo", [NE, 1], F32)
                hi = sbuf(ph, "hi", [NE, 1], F32)
                mid = sbuf(ph, "mid", [NE, 1], F32)
                cnt = sbuf(ph, "cnt", [NE, 1], F32)
                sel = sbuf(ph, "sel", [NE, 1], F32)
                nsel = sbuf(ph, "nsel", [NE, 1], F32)
                ta = sbuf(ph, "ta", [NE, 1], F32)
                tb_ = sbuf(ph, "tb", [NE, 1], F32)
                junk = sbuf(ph, "junk", [NE, S], F32)
                msk = sbuf(ph, "msk", [NE, S], F32)
                cs = sbuf(ph, "cs", [NE, S], F32)
                ones_s = sbuf(ph, "ones_s", [NE, S], F32)
                tokab = sbuf(ph, "tokab", [128, NT, 2], F32)
                r1 = sbuf(ph, "r1", [128, NT, NE], F32)
                pT_ps = psum(ph, "pTps", [128, NT, NE], F32)
                gT_ps = psum(ph, "gTps", [128, NT, NE], F32)
                B_b, B_junk, B_msk, B_cs, B_ones, B_tok, B_r1, B_pTps, B_gTps = [Buf() for _ in range(9)]
                d_c4 = cx.dsem("c4")
                cx.op(SP, lambda: nsp.dma_start(out=iota512[:], in_=iota512_d[:, :]), writes=[B_iota], dma=d_c4)
                cx.op(SP, lambda: nsp.dma_start(out=tokab[:], in_=tokab_d[:, :, :]), writes=[B_tok], dma=d_c4)
                cx.op(V, lambda: nv.memset(lo[:], 0.0), writes=[B_b])
                cx.op(V, lambda: nv.memset(hi[:], 1.0), writes=[B_b])
                cx.op(P, lambda: npl.memset(ones_s[:], 1.0), writes=[B_ones])
                for it in range(NBISECT):
                    cx.op(V, lambda: nv.tensor_scalar(out=mid[:], in0=lo[:], scalar1=hi[:, 0:1], scalar2=0.5, op0=ALU.add, op1=ALU.mult),
                          reads=[B_b], writes=[B_b])
                    cx.op(V, lambda: nv.memset(cnt[:], 0.0), reads=[B_b], writes=[B_b])
                    cx.op(V, lambda: nv.tensor_scalar(out=junk[:], in0=affT[:], scalar1=mid[:, 0:1], scalar2=0.0, op0=ALU.is_ge, op1=ALU.add,
                                                      accum_out=cnt[:]),
                          reads=[B_aff, B_b], writes=[B_junk, B_b])
                    cx.op(V, lambda: nv.tensor_scalar(out=sel[:], in0=cnt[:], scalar1=float(CAP), scalar2=None, op0=ALU.is_ge),
                          reads=[B_b], writes=[B_b])
                    cx.op(V, lambda: nv.tensor_scalar(out=nsel[:], in0=sel[:], scalar1=-1.0, scalar2=1.0, op0=ALU.mult, op1=ALU.add),
                          reads=[B_b], writes=[B_b])
                    cx.op(V, lambda: nv.tensor_scalar(out=ta[:], in0=mid[:], scalar1=nsel[:, 0:1], scalar2=None, op0=ALU.mult),
                          reads=[B_b], writes=[B_b])
                    cx.op(V, lambda: nv.scalar_tensor_tensor(out=hi[:], in0=hi[:], scalar=sel[:, 0:1], in1=ta[:], op0=ALU.mult, op1=ALU.add),
                          reads=[B_b], writes=[B_b])
                    cx.op(V, lambda: nv.tensor_scalar(out=tb_[:], in0=lo[:], scalar1=nsel[:, 0:1], scalar2=None, op0=ALU.mult),
                          reads=[B_b], writes=[B_b])
                    cx.op(V, lambda: nv.scalar_tensor_tensor(out=lo[:], in0=mid[:], scalar=sel[:, 0:1], in1=tb_[:], op0=ALU.mult, op1=ALU.add),
                          reads=[B_b], writes=[B_b])
                cx.op(V, lambda: nv.tensor_scalar(out=msk[:], in0=affT[:], scalar1=lo[:, 0:1], scalar2=None, op0=ALU.is_ge),
                      reads=[B_aff, B_b], writes=[B_msk])
                cx.op(V, lambda: nv.tensor_tensor_scan(out=cs[:], data0=ones_s[:], data1=msk[:], initial=0.0, op0=ALU.mult, op1=ALU.add),
                      reads=[B_ones, B_msk], writes=[B_cs])
                cx.op(V, lambda: nv.tensor_tensor(out=cs[:], in0=cs[:], in1=msk[:], op=ALU.mult), reads=[B_cs, B_msk], writes=[B_cs])
                cx.op(V, lambda: nv.tensor_scalar(out=cs[:], in0=cs[:], scalar1=-1.0, scalar2=None, op0=ALU.add), reads=[B_cs], writes=[B_cs])
                for jt in range(NT):
                    cx.op(PE, lambda: nt.transpose(out=pT_ps[:, jt, :], in_=cs[:, jt * 128:(jt + 1) * 128], identity=ident_f[0:NE, 0:NE]),
                          reads=[B_cs, B_const], writes=[B_pTps])
                for jt in range(NT):
                    cx.op(PE, lambda: nt.transpose(out=gT_ps[:, jt, :], in_=affT[:, jt * 128:(jt + 1) * 128], identity=ident_f[0:NE, 0:NE]),
                          reads=[B_aff, B_const], writes=[B_gTps])
                cx.op(V, lambda: nv.tensor_copy(out=posT[:], in_=pT_ps[:]), reads=[B_pTps], writes=[B_posT])
                cx.op(A, lambda: na.copy(out=gaT[:], in_=gT_ps[:]), reads=[B_gTps], writes=[B_gaT])
                cx.op(V, lambda: nv.tensor_copy(out=vals[:, :, :, 2], in_=gaT[:]), reads=[B_gaT], writes=[B_vals])
                cx.op(V, lambda: nv.tensor_tensor(out=r1[:], in0=gaT[:], in1=vals[:, :, :, 2], op=ALU.subtract), reads=[B_gaT, B_vals], writes=[B_r1])
                cx.op(V, lambda: nv.tensor_copy(out=vals[:, :, :, 3], in_=r1[:]), reads=[B_r1], writes=[B_vals])
                cx.op(V, lambda: nv.tensor_tensor(out=r1[:], in0=r1[:], in1=vals[:, :, :, 3], op=ALU.subtract), reads=[B_r1, B_vals], writes=[B_r1])
                cx.op(V, lambda: nv.tensor_copy(out=vals[:, :, :, 4], in_=r1[:]), reads=[B_r1], writes=[B_vals])
                for e in range(NE):
                    cx.op(P, lambda: npl.tensor_copy(out=vals[:, :, e, 0:2], in_=tokab[:]), reads=[B_tok], writes=[B_vals])
                if debug == 4:
                    d_dbg = cx.dsem("dbg4")
                    cx.op(SP, lambda: nsp.dma_start(out=dbg_thr[0, :, :], in_=lo[:]), reads=[B_b], writes=[B_out], dma=d_dbg)
                    cx.op(SP, lambda: nsp.dma_start(out=dbg_thr[1, :, :], in_=hi[:]), reads=[B_b], writes=[B_out], dma=d_dbg)
                cx.barrier()
        s4 = es.enter_context(ExitStack())
        oh = [sbuf(s4, "oh%d" % i, [128, 512], BF16) for i in range(2)]
        cmp_sb = sbuf(s4, "cmp_sb", [128, 4, 5], F32)
        idxf = [sbuf(s4, "idxf%d" % i, [128, 4], F32) for i in range(3)]
        idxi = [sbuf(s4, "idxi%d" % i, [128, 4], I32) for i in range(3)]
        gts = [sbuf(s4, "gts%d" % i, [128, 4], F32) for i in range(3)]
        idxqf = [sbuf(s4, "idxqf%d" % i, [128, 4, 4], F32) for i in range(3)]
        idxqi = [sbuf(s4, "idxqi%d" % i, [128, 4, 4], I32) for i in range(3)]
        cmp_ps = psum(s4, "cmpps", [128, 4, 5], F32)
        B_oh = [Buf() for _ in range(2)]
        B_cmps, B_cmpp = Buf(), Buf()
        B_idx = [Buf() for _ in range(3)]
        ohc = [0]

        def compact(e):
            ep = e % 3
            for jt in range(NT):
                ob = ohc[0] % 2
                ohc[0] += 1
                cx.op(V, lambda: nv.tensor_scalar(out=oh[ob][:], in0=iota512[:], scalar1=posT[:, jt, e:e + 1], scalar2=None, op0=ALU.is_equal),
                      reads=[B_iota, B_posT], writes=[B_oh[ob]])
                for cc in range(4):
                    cx.op(PE, lambda: nt.matmul(cmp_ps[:, cc, :], lhsT=oh[ob][:, cc * 128:(cc + 1) * 128], rhs=vals[:, jt, e, :],
                                                start=(jt == 0 and cc == 0), stop=(jt == NT - 1)),
                          reads=[B_oh[ob], B_vals], writes=[B_cmpp])
            cx.op(A, lambda: na.copy(out=cmp_sb[:], in_=cmp_ps[:]), reads=[B_cmpp], writes=[B_cmps])
            cx.op(V, lambda: nv.scalar_tensor_tensor(out=idxf[ep][:], in0=cmp_sb[:, :, 0], scalar=64.0, in1=cmp_sb[:, :, 1],
                                                     op0=ALU.mult, op1=ALU.add), reads=[B_cmps], writes=[B_idx[ep]])
            cx.op(V, lambda: nv.tensor_copy(out=idxi[ep][:], in_=idxf[ep][:]), reads=[B_idx[ep]], writes=[B_idx[ep]])
            for dsq in range(4):
                cx.op(V, lambda: nv.tensor_scalar(out=idxqf[ep][:, dsq, :], in0=idxf[ep][:], scalar1=4.0, scalar2=float(dsq), op0=ALU.mult, op1=ALU.add),
                      reads=[B_idx[ep]], writes=[B_idx[ep]])
            cx.op(V, lambda: nv.tensor_copy(out=idxqi[ep][:], in_=idxqf[ep][:]), reads=[B_idx[ep]], writes=[B_idx[ep]])
            cx.op(V, lambda: nv.tensor_tensor(out=gts[ep][:], in0=cmp_sb[:, :, 2], in1=cmp_sb[:, :, 3], op=ALU.add),
                  reads=[B_cmps], writes=[B_idx[ep]])
            cx.op(V, lambda: nv.tensor_tensor(out=gts[ep][:], in0=gts[ep][:], in1=cmp_sb[:, :, 4], op=ALU.add),
                  reads=[B_cmps, B_idx[ep]], writes=[B_idx[ep]])

        if debug == 4:
            d_dbg5 = cx.dsem("dbg5")
            for e in range(NE):
                compact(e)
                cx.op(SP, lambda: nsp.dma_start(out=dbg_idx[e, 0, :, :], in_=idxf[e % 3][:]), reads=[B_idx[e % 3]], writes=[B_out], dma=d_dbg5)
                cx.op(SP, lambda: nsp.dma_start(out=dbg_idx[e, 1, :, :], in_=gts[e % 3][:]), reads=[B_idx[e % 3]], writes=[B_out], dma=d_dbg5)
            cx.barrier()
            return nc

        with ExitStack() as ph:
            ring = [sbuf(ph, "ring%d" % i, [128, 16, 512], BF16) for i in range(NSLOT)]
            xs = sbuf(ph, "xs", [128, 4, D], BF16)
            xsT = sbuf(ph, "xsT", [128, 16, 512], BF16)
            gT = sbuf(ph, "gT", [128, 32, 512], BF16)
            sa = [sbuf(ph, "sa%d" % i, [128, 512], F32) for i in range(2)]
            ystage = [sbuf(ph, "ystage%d" % i, [128, 4, 512], F32) for i in range(2)]
            a_ps = [psum(ph, "aps%d" % i, [128, 512], F32) for i in range(2)]
            u_ps = [psum(ph, "ups%d" % i, [128, 512], F32) for i in range(2)]
            y_ps = [psum(ph, "yps%d" % i, [128, 512], F32) for i in range(2)]
            tp3 = psum(ph, "tp3", [128, 8, 128], BF16)
            B_ring = [Buf() for _ in range(NSLOT)]
            d_ring = [cx.dsem("rg%d" % i) for i in range(NSLOT)]
            B_xs, B_xsT, B_gT, B_tp3 = Buf(), Buf(), Buf(), Buf()
            B_ys = [Buf() for _ in range(2)]
            B_sa = [Buf() for _ in range(2)]
            B_aps = [Buf() for _ in range(2)]
            B_ups = [Buf() for _ in range(2)]
            B_yps = [Buf() for _ in range(2)]
            B_sc = [[Buf() for _ in range(16)] for _ in range(2)]
            d_xs = cx.dsem("xs")
            d_sc = [cx.dsem("sc%d" % i) for i in range(4)]
            wg_v = [w_gate_d[e].rearrange("(kc p) f -> p kc f", p=128) for e in range(NE)]
            wu_v = [w_up_d[e].rearrange("(kc p) f -> p kc f", p=128) for e in range(NE)]
            wd_v = [w_down_d[e].rearrange("(fc p) d -> p fc d", p=128) for e in range(NE)]
            acc_q = acc_d.ap().rearrange("s (a c) -> (s a) c", c=512)
            loads = []
            for e in range(NE):
                for fg in range(8):
                    loads.append(wg_v[e][:, :, fg * 512:(fg + 1) * 512])
                    loads.append(wu_v[e][:, :, fg * 512:(fg + 1) * 512])
                for ds in range(4):
                    for hf in range(2):
                        loads.append(wd_v[e][:, hf * 16:(hf + 1) * 16, ds * 512:(ds + 1) * 512])
            nxt = [0]

            def ensure_loads(upto):
                while nxt[0] <= upto and nxt[0] < len(loads):
                    k = nxt[0]
                    sl = k % NSLOT
                    src = loads[k]
                    cx.op(P, lambda: npl.dma_start(out=ring[sl][:], in_=src), writes=[B_ring[sl]], dma=d_ring[sl])
                    nxt[0] += 1

            def gather(e):
                ep = e % 3
                for cc in range(4):
                    cx.op(P, lambda: npl.indirect_dma_start(out=xs[:, cc, :], out_offset=None, in_=h2_scr[:, :],
                                                            in_offset=bass.IndirectOffsetOnAxis(ap=idxi[ep][:, cc:cc + 1], axis=0)),
                          reads=[B_idx[ep], B_h2], writes=[B_xs], dma=d_xs)

            def transposes(e):
                for cc in range(4):
                    for half in range(2):
                        for k in range(8):
                            kc = half * 8 + k
                            cx.op(PE, lambda: nt.transpose(out=tp3[:, k, :], in_=xs[:, cc, kc * 128:(kc + 1) * 128], identity=ident_bf[:]),
                                  reads=[B_xs, B_const], writes=[B_tp3])
                        dst = xsT[:, half * 8:half * 8 + 8, cc * 128:(cc + 1) * 128]
                        if half == 0:
                            cx.op(A, lambda: na.copy(out=dst, in_=tp3[:]), reads=[B_tp3], writes=[B_xsT])
                        else:
                            cx.op(V, lambda: nv.tensor_copy(out=dst, in_=tp3[:]), reads=[B_tp3], writes=[B_xsT])

            pend_sc = []

            def flush_sc():
                for (e_, ds_) in pend_sc:
                    ep_ = e_ % 3
                    for tt in range(4):
                        cx.op(P, lambda: npl.indirect_dma_start(out=acc_q,
                                                                out_offset=bass.IndirectOffsetOnAxis(ap=idxqi[ep_][:, ds_, tt:tt + 1], axis=0),
                                                                in_=ystage[ds_ % 2][:, tt, :], in_offset=None, compute_op=ALU.add),
                              reads=[B_ys[ds_ % 2], B_idx[ep_], B_acc] + B_sc[(e_ + 1) % 2], writes=[B_sc[e_ % 2][ds_ * 4 + tt]], dma=d_sc[tt])
                del pend_sc[:]

            ensure_loads(NSLOT - 1)
            compact(0)
            gather(0)
            transposes(0)
            compact(1)
            gather(1)
            abi = 0
            ybi = 0
            for e in range(NE):
                ep = e % 3
                sp_ = e % 2
                base = e * 24
                for fg in range(8):
                    kg = base + 2 * fg
                    ku = kg + 1
                    ensure_loads(ku + NSLOT - 2)
                    sg, su = kg % NSLOT, ku % NSLOT
                    if fg == 1:
                        flush_sc()
                    for fl in range(4):
                        fc = fg * 4 + fl
                        ab = abi % 2
                        abi += 1
                        for kc in range(16):
                            cx.op(PE, lambda: nt.matmul(a_ps[ab][:], lhsT=ring[sg][:, kc, fl * 128:(fl + 1) * 128], rhs=xsT[:, kc, :],
                                                        start=(kc == 0), stop=(kc == 15)),
                                  reads=[B_ring[sg], B_xsT], writes=[B_aps[ab]])
                        for kc in range(16):
                            cx.op(PE, lambda: nt.matmul(u_ps[ab][:], lhsT=ring[su][:, kc, fl * 128:(fl + 1) * 128], rhs=xsT[:, kc, :],
                                                        start=(kc == 0), stop=(kc == 15)),
                                  reads=[B_ring[su], B_xsT], writes=[B_ups[ab]])
                        cx.op(A, lambda: na.activation(out=sa[ab][:], in_=a_ps[ab][:], func=AF.Silu), reads=[B_aps[ab]], writes=[B_sa[ab]])
                        cx.op(V, lambda: nv.tensor_tensor(out=gT[:, fc, :], in0=sa[ab][:], in1=u_ps[ab][:], op=ALU.mult),
                              reads=[B_sa[ab], B_ups[ab]], writes=[B_gT])
                for ds in range(4):
                    k0 = base + 16 + 2 * ds
                    k1 = k0 + 1
                    ensure_loads(k1 + NSLOT - 2)
                    s0, s1_ = k0 % NSLOT, k1 % NSLOT
                    for tt in range(4):
                        yb = ybi % 2
                        ybi += 1
                        for fc in range(32):
                            sl = s0 if fc < 16 else s1_
                            cx.op(PE, lambda: nt.matmul(y_ps[yb][:], lhsT=gT[:, fc, tt * 128:(tt + 1) * 128], rhs=ring[sl][:, fc % 16, :],
                                                        start=(fc == 0), stop=(fc == 31)),
                                  reads=[B_gT, B_ring[sl]], writes=[B_yps[yb]])
                        cx.op(A, lambda: na.activation(out=ystage[ds % 2][:, tt, :], in_=y_ps[yb][:], func=AF.Copy,
                                                       scale=gts[ep][:, tt:tt + 1]),
                              reads=[B_yps[yb], B_idx[ep]], writes=[B_ys[ds % 2]])
                    flush_sc()
                    pend_sc.append((e, ds))
                    if ds == 0 and e + 1 < NE:
                        transposes(e + 1)
                        if e + 2 < NE:
                            compact(e + 2)
                if e + 2 < NE:
                    gather(e + 2)
            flush_sc()
            cx.barrier()

        with ExitStack() as ph:
            gfbc = sbuf(ph, "gfbc", [128, D], F32)
            a4 = [sbuf(ph, "a4_%d" % i, [128, D], F32) for i in range(2)]
            o4 = [sbuf(ph, "o4_%d" % i, [128, D], F32) for i in range(2)]
            ssq4 = sbuf(ph, "ssq4", [128, NT], F32)
            rs4 = sbuf(ph, "rs4", [128, NT], F32)
            B_gf, B_ssq4, B_rs4 = Buf(), Buf(), Buf()
            B_a4 = [Buf() for _ in range(2)]
            B_o4 = [Buf() for _ in range(2)]
            d_a4 = [cx.dsem("a40"), cx.dsem("a41")]
            cx.op(SP, lambda: nsp.dma_start(out=gfbc[:], in_=gfbc_d[:, :]), writes=[B_gf], dma=d_const)
            cx.op(V, lambda: nv.memset(ssq4[:], 0.0), writes=[B_ssq4])
            for T in range(NT):
                tq = T % 2
                cx.op(SP, lambda: nsp.dma_start(out=a4[tq][:], in_=acc_d[T * 128:(T + 1) * 128, :]),
                      reads=[B_acc] + B_sc[0] + B_sc[1], writes=[B_a4[tq]], dma=d_a4[tq])
                cx.op(A, lambda: na.activation(out=o4[tq][:], in_=a4[tq][:], func=AF.Square, accum_out=ssq4[:, T:T + 1]),
                      reads=[B_a4[tq]], writes=[B_o4[tq], B_ssq4])
                rstd_from_ssq(ssq4[:, T:T + 1], rs4[:, T:T + 1], D, [B_ssq4], [B_rs4])
                cx.op(V, lambda: nv.scalar_tensor_tensor(out=o4[tq][:], in0=a4[tq][:], scalar=rs4[:, T:T + 1], in1=gfbc[:],
                                                         op0=ALU.mult, op1=ALU.mult),
                      reads=[B_a4[tq], B_rs4, B_gf], writes=[B_o4[tq]])
                cx.op(SP, lambda: nsp.dma_start(out=out_d[T * 128:(T + 1) * 128, :], in_=o4[tq][:]),
                      reads=[B_o4[tq]], writes=[B_out], dma=d_out)
        cx.barrier()

    return nc


_CONST = None
_NC_CACHE = {}


def _shared_maps(inp):
    global _CONST
    if _CONST is None:
        _CONST = _host_constants()
    c = _CONST
    f32 = lambda a: np.ascontiguousarray(np.asarray(a, dtype=np.float32))
    bc = lambda v: np.ascontiguousarray(np.broadcast_to(f32(v).reshape(1, -1), (128, f32(v).size)))
    m = {}
    m["g1bc"] = bc(inp["norm1_g"][0])
    m["g2bc"] = bc(inp["norm2_g"][0])
    m["gfbc"] = bc(inp["final_g"])
    m["w_in"] = f32(inp["w_in"][0])
    m["pool_w"] = f32(inp["pool_w"][0])
    m["w_out"] = f32(inp["w_out"][0])
    m["w_router"] = f32(inp["w_router"][0])
    m["w_gate"] = f32(inp["w_gate"][0])
    m["w_up"] = f32(inp["w_up"][0])
    m["w_down"] = f32(inp["w_down"][0])
    m["pscale"] = np.ascontiguousarray(f32(inp["pool_scale"][0]).reshape(8, 128).T)
    gn = np.concatenate([f32(inp["gn_pool"][0]), f32(inp["gn_attn"][0])])
    m["gnfm"] = np.ascontiguousarray(gn.reshape(16, 128).T)
    sink = f32(inp["sink"][0])
    m["sinkbc"] = np.ascontiguousarray(np.broadcast_to(np.repeat(sink, 128)[None, :], (128, 1024)))
    rb = f32(inp["rel_bias"])
    bidx = c["_bidx"]
    bt = np.zeros((2, 3, 128, 4, 128), np.float32)
    for j in range(2):
        for gq in range(4):
            bt[j, :, :, gq, :] = rb[:, 4 * j + gq][bidx]
    m["biastab"] = np.ascontiguousarray(bt.reshape(6, 128, 512))
    for k in ("mask01", "ident_bf", "ident_f", "iota512", "tokab", "edgef"):
        m[k] = c[k]
    return m


def _get_nc(debug=0):
    if debug not in _NC_CACHE:
        _NC_CACHE[debug] = build_nc(debug)
    return _NC_CACHE[debug]


def kernel(**inputs):
    inp = {k: np.asarray(v) for k, v in inputs.items()}
    shared = _shared_maps(inp)
    x = np.asarray(inp["x"], dtype=np.float32)
    in_maps = []
    for c in range(NCORES):
        m = dict(shared)
        m["x"] = np.ascontiguousarray(x[c])
        in_maps.append(m)
    nc = _get_nc(0)
    res = run_bass_kernel_spmd(nc, in_maps, core_ids=list(range(NCORES)))
    out = np.stack([np.asarray(res.results[b]["out"], dtype=np.float32) for b in range(4)], axis=0)
    return out
```

```python
import os
import math
import numpy as np
import ml_dtypes
from contextlib import ExitStack
import concourse.bass as bass
import concourse.mybir as mybir
from concourse.bass_utils import run_bass_kernel_spmd

F32 = mybir.dt.float32
BF16 = mybir.dt.bfloat16
I32 = mybir.dt.int32
ALU = mybir.AluOpType
AF = mybir.ActivationFunctionType

S = 4096
D = 2048
NT = S // 128
NG = S // 512
INW = 2560
DFF = 4096
NE = 16
CAP = 512
EPS = 1e-6
NCORES = 4
NSLOT = 6
NBISECT = 31
SEM_EPOCH = 30000


class Buf:
    __slots__ = ("name", "w", "rs")

    def __init__(self, name=""):
        self.name = name
        self.w = None
        self.rs = {}


class SemC:
    __slots__ = ("sem", "count", "is_dma", "unit")

    def __init__(self, sem, is_dma, unit):
        self.sem = sem
        self.count = 0
        self.is_dma = is_dma
        self.unit = unit


class Ctx:
    def __init__(self, nc, es):
        self.nc = nc
        self.es = es
        self.engs = {"pe": nc.tensor, "act": nc.scalar, "dve": nc.vector, "pool": nc.gpsimd, "sp": nc.sync}
        self.esem = {}
        self.all_sems = []
        self.nsem = 0
        for k in self.engs:
            self._new_esem(k)
        self.waited = {k: {} for k in self.engs}
        self.nwaits = 0
        self.nops = 0

    def _new_esem(self, k):
        s = SemC(self.es.enter_context(self.nc.semaphore("e%s%d" % (k, self.nsem))), False, 1)
        self.nsem += 1
        self.esem[k] = s
        self.all_sems.append(s)

    def dsem(self, name):
        s = SemC(self.es.enter_context(self.nc.semaphore("d" + name)), True, 16)
        self.nsem += 1
        self.all_sems.append(s)
        return s

    def _wait(self, eng, semc, val):
        if semc.is_dma:
            val = semc.count * semc.unit
        if val <= 0:
            return
        w = self.waited[eng]
        if w.get(id(semc), 0) >= val:
            return
        self.engs[eng].wait_ge(semc.sem, val)
        w[id(semc)] = val
        self.nwaits += 1

    def op(self, eng, fn, reads=(), writes=(), dma=None):
        for b in reads:
            if b.w is not None:
                self._wait(eng, b.w[0], b.w[1])
        for b in writes:
            for t in b.rs.values():
                if dma is None and t[2] == eng:
                    continue
                self._wait(eng, t[0], t[1])
            t = b.w
            if t is not None and not (dma is None and t[2] == eng):
                self._wait(eng, t[0], t[1])
        inst = fn()
        self.nops += 1
        if dma is not None:
            dma.count += 1
            inst.then_inc(dma.sem, dma.unit)
            tok = (dma, dma.count * dma.unit, None)
        else:
            s = self.esem[eng]
            if s.count >= SEM_EPOCH:
                self._new_esem(eng)
                s = self.esem[eng]
            s.count += 1
            inst.then_inc(s.sem, 1)
            tok = (s, s.count, eng)
        for b in writes:
            b.w = tok
            b.rs = {}
        for b in reads:
            b.rs[id(tok[0])] = tok
        return tok

    def barrier(self):
        for e in self.engs:
            for s in self.all_sems:
                if s is self.esem.get(e):
                    continue
                self._wait(e, s, s.count * s.unit)


def _t5_bucket_np(rel):
    half, max_exact = 16, 8
    ret = np.where(rel > 0, half, 0)
    n = np.abs(rel)
    nf = np.maximum(n, 1).astype(np.float64)
    large = max_exact + np.floor(2.0 * np.log2(nf / max_exact) + 1e-6).astype(np.int64)
    large = np.minimum(large, half - 1)
    return ret + np.where(n < max_exact, n, large)


def _host_constants():
    c = {}
    c["ident_bf"] = np.eye(128, dtype=np.float32).astype(ml_dtypes.bfloat16)
    c["ident_f"] = np.eye(128, dtype=np.float32)
    c["iota512"] = np.broadcast_to(np.arange(512, dtype=np.float32), (128, 512)).copy()
    tok = (np.arange(128)[:, None] + 128 * np.arange(32)[None, :])
    tokab = np.stack([tok // 64, tok % 64], axis=-1).astype(np.float32)
    c["tokab"] = tokab.copy()
    kk = np.arange(128)[:, None]
    qq = np.arange(128)[None, :]
    mask = np.zeros((3, 128, 512), np.float32)
    bidx = np.zeros((3, 128, 128), np.int64)
    for kb in range(3):
        rel = (kb - 1) * 128 + kk - qq
        m = np.where(np.abs(rel) <= 128, 0.0, -1.0e5).astype(np.float32)
        mask[kb] = np.tile(m, (1, 4))
        bidx[kb] = _t5_bucket_np(rel)
    c["mask01"] = mask
    c["_bidx"] = bidx
    edge = np.zeros((4, 16), np.float32)
    for wi, w in enumerate((2, 4, 8, 16)):
        for i in range(16):
            t = i if i < 8 else S - 16 + i
            lo = max(t - w // 2, 0)
            hi = min(t + w // 2, S)
            edge[wi, i] = 1.0 / float(hi - lo)
    c["edgef"] = np.broadcast_to(edge[None], (128, 4, 16)).copy()
    return c


def build_nc(debug=0):
    nc = bass.Bass("TRN2", target_bir_lowering=False)
    dt_in = lambda name, shape, dt=F32: nc.dram_tensor(name, shape, dt, kind="ExternalInput")
    x_d = dt_in("x", [S, D])
    g1bc_d = dt_in("g1bc", [128, D])
    g2bc_d = dt_in("g2bc", [128, D])
    gfbc_d = dt_in("gfbc", [128, D])
    w_in_d = dt_in("w_in", [D, INW])
    pool_w_d = dt_in("pool_w", [4, 256, 256])
    w_out_d = dt_in("w_out", [D, D])
    w_router_d = dt_in("w_router", [D, NE])
    if debug == 0 or debug >= 5:
        w_gate_d = dt_in("w_gate", [NE, D, DFF])
        w_up_d = dt_in("w_up", [NE, D, DFF])
        w_down_d = dt_in("w_down", [NE, DFF, D])
    pscale_d = dt_in("pscale", [128, 8])
    gnfm_d = dt_in("gnfm", [128, 16])
    sinkbc_d = dt_in("sinkbc", [128, 1024])
    biastab_d = dt_in("biastab", [6, 128, 512])
    mask01_d = dt_in("mask01", [3, 128, 512])
    ident_bf_d = dt_in("ident_bf", [128, 128], BF16)
    ident_f_d = dt_in("ident_f", [128, 128])
    iota512_d = dt_in("iota512", [128, 512])
    tokab_d = dt_in("tokab", [128, 32, 2])
    edgef_d = dt_in("edgef", [128, 4, 16])
    out_d = nc.dram_tensor("out", [S, D], F32, kind="ExternalOutput")
    dk = lambda lv: "ExternalOutput" if debug == lv else "Internal"
    qu_scr = nc.dram_tensor("qu_scr", [16, 128, S], BF16, kind=dk(1))
    mix_scr = nc.dram_tensor("mix_scr", [16, 128, S], BF16, kind=dk(2))
    h2_scr = nc.dram_tensor("h2_scr", [S, D], BF16, kind=dk(3))
    acc_d = nc.dram_tensor("acc", [S, D], F32, kind=dk(3))
    if debug:
        dbg_aff = nc.dram_tensor("dbg_aff", [NE, S], F32, kind="ExternalOutput")
        dbg_kv = nc.dram_tensor("dbg_kv", [128, 2 * S + NT * 256], BF16, kind="ExternalOutput")
        dbg_rstd = nc.dram_tensor("dbg_rstd", [128, 64], F32, kind="ExternalOutput")
        dbg_idx = nc.dram_tensor("dbg_idx", [NE, 2, 128, 4], F32, kind="ExternalOutput")
        dbg_thr = nc.dram_tensor("dbg_thr", [2, NE, 1], F32, kind="ExternalOutput")

    qu_pm = qu_scr.ap().rearrange("c p t -> p c t")
    mix_pm = mix_scr.ap().rearrange("c p t -> p c t")

    with ExitStack() as es:
        cx = Ctx(nc, es)
        sbuf = lambda st, name, shape, dt: st.enter_context(nc.sbuf_tensor("s_" + name, shape, dt))
        psum = lambda st, name, shape, dt=F32: st.enter_context(nc.psum_tensor("p_" + name, shape, dt))
        V, A, P, PE, SP = "dve", "act", "pool", "pe", "sp"
        nv, na, npl, nt, nsp = nc.vector, nc.scalar, nc.gpsimd, nc.tensor, nc.sync

        B_qu, B_mix, B_h2, B_acc, B_out = Buf("qu"), Buf("mix"), Buf("h2"), Buf("acc"), Buf("out")
        d_out = cx.dsem("out")

        ident_bf = sbuf(es, "ident_bf", [128, 128], BF16)
        ident_f = sbuf(es, "ident_f", [128, 128], F32)
        rstd_pa = sbuf(es, "rstd_pa", [128, 64], F32)
        eps_t = sbuf(es, "eps_t", [128, 1], F32)
        B_const, B_rstd = Buf("const"), Buf("rstd")
        d_const = cx.dsem("const")
        cx.op(SP, lambda: nsp.dma_start(out=ident_bf[:], in_=ident_bf_d[:, :]), writes=[B_const], dma=d_const)
        cx.op(SP, lambda: nsp.dma_start(out=ident_f[:], in_=ident_f_d[:, :]), writes=[B_const], dma=d_const)
        cx.op(V, lambda: nv.memset(eps_t[:], EPS), writes=[B_const])

        def rstd_from_ssq(ssq_ap, out_ap, n, rb, wb):
            cx.op(A, lambda: na.activation(out=out_ap, in_=ssq_ap, func=AF.Sqrt, scale=1.0 / float(n), bias=eps_t[:, 0:1]),
                  reads=list(rb) + [B_const], writes=wb)
            cx.op(V, lambda: nv.reciprocal(out=out_ap, in_=out_ap), reads=wb, writes=wb)

        with ExitStack() as s1:
            kT_all = sbuf(s1, "kT_all", [128, 2, S], BF16)
            V_all = sbuf(s1, "V_all", [128, NT, 256], BF16)
            B_kT = [Buf("kT%d" % g) for g in range(NG)]
            B_V = [Buf("V%d" % g) for g in range(NG)]

            with ExitStack() as ph:
                w_in_sb = sbuf(ph, "w_in_sb", [128, 16, INW], BF16)
                g1bc = sbuf(ph, "g1bc", [128, D], F32)
                xbuf = [sbuf(ph, "xb%d" % i, [128, D], F32) for i in range(2)]
                hb = [sbuf(ph, "hb%d" % i, [128, D], BF16) for i in range(2)]
                hT = [sbuf(ph, "hT%d" % i, [128, 16, 512], BF16) for i in range(2)]
                qu_st = sbuf(ph, "qu_st", [128, 16, 512], BF16)
                ssq = sbuf(ph, "ssq", [128, NT], F32)
                rs1 = sbuf(ph, "rs1", [128, NT], F32)
                tp = [psum(ph, "tp%d" % i, [128, 8, 128], BF16) for i in range(3)]
                pp = [psum(ph, "pp%d" % i, [128, 512], F32) for i in range(5)]
                B_w, B_g1 = Buf("w_in"), Buf("g1")
                B_x = [Buf() for _ in range(2)]
                B_hb = [Buf() for _ in range(2)]
                B_hT = [Buf() for _ in range(2)]
                B_ssq = [Buf() for _ in range(2)]
                B_rs = [Buf() for _ in range(2)]
                B_tp = [Buf() for _ in range(3)]
                B_pp = [Buf() for _ in range(5)]
                B_qs = Buf("qu_st")
                d_w, d_x, d_qs = cx.dsem("w1"), [cx.dsem("x0"), cx.dsem("x1")], cx.dsem("qs")
                w_in_v = w_in_d.ap().rearrange("(kc p) n -> p kc n", p=128)
                for q4 in range(4):
                    cx.op(P, lambda: npl.dma_start(out=w_in_sb[:, 4 * q4:4 * q4 + 4, :], in_=w_in_v[:, 4 * q4:4 * q4 + 4, :]),
                          writes=[B_w], dma=d_w)
                cx.op(SP, lambda: nsp.dma_start(out=g1bc[:], in_=g1bc_d[:, :]), writes=[B_g1], dma=d_const)
                cx.op(V, lambda: nv.memset(ssq[:], 0.0), writes=B_ssq)
                c1 = {"tp": 0, "pp": 0, "ev": 0}

                def norm_tile(g, i):
                    T = 4 * g + i
                    tpar = T % 2
                    cx.op(SP, lambda: nsp.dma_start(out=xbuf[tpar][:], in_=x_d[T * 128:(T + 1) * 128, :]),
                          writes=[B_x[tpar]], dma=d_x[tpar])
                    cx.op(A, lambda: na.activation(out=hb[tpar][:], in_=xbuf[tpar][:], func=AF.Square,
                                                   accum_out=ssq[:, T:T + 1]),
                          reads=[B_x[tpar]], writes=[B_hb[tpar], B_ssq[tpar]])
                    rstd_from_ssq(ssq[:, T:T + 1], rs1[:, T:T + 1], D, [B_ssq[tpar]], [B_rs[tpar]])
                    cx.op(V, lambda: nv.scalar_tensor_tensor(out=hb[tpar][:], in0=xbuf[tpar][:], scalar=rs1[:, T:T + 1],
                                                             in1=g1bc[:], op0=ALU.mult, op1=ALU.mult),
                          reads=[B_x[tpar], B_rs[tpar], B_g1], writes=[B_hb[tpar]])

                def tr_tile(g, i):
                    T = 4 * g + i
                    tpar = T % 2
                    gp = g % 2
                    for half in range(2):
                        tb = c1["tp"] % 3
                        c1["tp"] += 1
                        for k in range(8):
                            kc = half * 8 + k
                            cx.op(PE, lambda: nt.transpose(out=tp[tb][:, k, :], in_=hb[tpar][:, kc * 128:(kc + 1) * 128],
                                                           identity=ident_bf[:]),
                                  reads=[B_hb[tpar], B_const], writes=[B_tp[tb]])
                        dst = hT[gp][:, half * 8:half * 8 + 8, i * 128:(i + 1) * 128]
                        if half == 0:
                            cx.op(A, lambda: na.copy(out=dst, in_=tp[tb][:]), reads=[B_tp[tb]], writes=[B_hT[gp]])
                        else:
                            cx.op(V, lambda: nv.tensor_copy(out=dst, in_=tp[tb][:]), reads=[B_tp[tb]], writes=[B_hT[gp]])

                def proj_chain(g, oc):
                    gp = g % 2
                    pb = c1["pp"] % 5
                    c1["pp"] += 1
                    c1["ev"] += 1
                    if oc < 18:
                        c0 = oc * 128
                        for kc in range(16):
                            cx.op(PE, lambda: nt.matmul(pp[pb][:], lhsT=w_in_sb[:, kc, c0:c0 + 128], rhs=hT[gp][:, kc, :],
                                                        start=(kc == 0), stop=(kc == 15)),
                                  reads=[B_w, B_hT[gp]], writes=[B_pp[pb]])
                        if oc < 16:
                            dst, wb = qu_st[:, oc, :], B_qs
                        else:
                            dst, wb = kT_all[:, oc - 16, g * 512:(g + 1) * 512], B_kT[g]
                        src = pp[pb][:]
                    else:
                        i = oc - 18
                        for kc in range(16):
                            cx.op(PE, lambda: nt.matmul(pp[pb][:, 0:256], lhsT=hT[gp][:, kc, i * 128:(i + 1) * 128],
                                                        rhs=w_in_sb[:, kc, 2304:2560], start=(kc == 0), stop=(kc == 15)),
                                  reads=[B_w, B_hT[gp]], writes=[B_pp[pb]])
                        dst, wb, src = V_all[:, 4 * g + i, :], B_V[g], pp[pb][:, 0:256]
                    if c1["ev"] % 2 == 0:
                        cx.op(A, lambda: na.copy(out=dst, in_=src), reads=[B_pp[pb]], writes=[wb])
                    else:
                        cx.op(V, lambda: nv.tensor_copy(out=dst, in_=src), reads=[B_pp[pb]], writes=[wb])
                    if oc == 15:
                        cx.op(SP, lambda: nsp.dma_start(out=qu_pm[:, :, g * 512:(g + 1) * 512], in_=qu_st[:]),
                              reads=[B_qs], writes=[B_qu], dma=d_qs)

                for i in range(4):
                    norm_tile(0, i)
                    tr_tile(0, i)
                chunks = [list(range(0, 6)), list(range(6, 12)), list(range(12, 17)), list(range(17, 22))]
                for g in range(NG):
                    for c in range(4):
                        if g + 1 < NG:
                            norm_tile(g + 1, c)
                        for oc in chunks[c]:
                            proj_chain(g, oc)
                        if g + 1 < NG:
                            tr_tile(g + 1, c)
                if debug:
                    d_dbg = cx.dsem("dbg")
                    cx.op(SP, lambda: nsp.dma_start(out=dbg_kv[:, 0:2 * S], in_=kT_all[:]), reads=B_kT, writes=[B_out], dma=d_dbg)
                    cx.op(SP, lambda: nsp.dma_start(out=dbg_kv[:, 2 * S:], in_=V_all[:]), reads=B_V, writes=[B_out], dma=d_dbg)
                cx.barrier()
            if debug == 1:
                cx.barrier()
                return nc

            with ExitStack() as ph:
                eb = sbuf(ph, "eb", [128, 6, 512], F32)
                Bs = sbuf(ph, "Bs", [128, 6, 512], BF16)
                B_Bs = Buf()
                mk = sbuf(ph, "mk", [128, 3, 512], F32)
                sinkexp = sbuf(ph, "sinkexp", [128, 1024], F32)
                ones_bf = sbuf(ph, "ones_bf", [128, 128], BF16)
                pw_sb = sbuf(ph, "pw_sb", [128, 4, 2, 256], BF16)
                pscale = sbuf(ph, "pscale", [128, 8], F32)
                edgef = sbuf(ph, "edgef", [128, 4, 16], F32)
                qT = [sbuf(ph, "qT%d" % i, [128, 8, 512], BF16) for i in range(2)]
                uT = [sbuf(ph, "uT%d" % i, [128, 8, 528], BF16) for i in range(2)]
                tmp = [sbuf(ph, "ptmp%d" % i, [128, 2, 528], F32) for i in range(4)]
                etmp = sbuf(ph, "etmp", [128, 2, 8], F32)
                tmp4 = sbuf(ph, "ptmp4", [128, 2, 512], F32)
                pooledT = sbuf(ph, "pooledT", [128, 8, 512], BF16)
                sqp = sbuf(ph, "sqp", [128, 8, 512], BF16)
                et = [sbuf(ph, "et%d" % i, [128, 512], F32) for i in range(2)]
                pt = [sbuf(ph, "pt%d" % i, [128, 512], BF16) for i in range(9)]
                den = [sbuf(ph, "den%d" % i, [128, 512], F32) for i in range(2)]
                at = [sbuf(ph, "at%d" % i, [128, 4, 128], F32) for i in range(2)]
                sqa = [sbuf(ph, "sqa%d" % i, [128, 4, 128], BF16) for i in range(4)]
                mixst = [sbuf(ph, "mixst%d" % i, [128, 16, 512], BF16) for i in range(2)]
                sps = [psum(ph, "sps%d" % i, [128, 512], F32) for i in range(2)]
                o_ps = [psum(ph, "ops%d" % i, [128, 4, 128], F32) for i in range(2)]
                d_ps = [psum(ph, "dps%d" % i, [128, 512], F32) for i in range(2)]
                pw_ps = psum(ph, "pwps", [128, 512], F32)
                ss_ps = psum(ph, "ssps", [128, 512], F32)
                B_eb, B_mk, B_sk, B_pw, B_c2 = Buf(), Buf(), Buf(), Buf(), Buf()
                B_q = [Buf() for _ in range(2)]
                B_u = [Buf() for _ in range(2)]
                B_tmp, B_etmp, B_pooled, B_sqp = Buf(), Buf(), Buf(), Buf()
                B_et = [Buf() for _ in range(2)]
                B_pt = [Buf() for _ in range(9)]
                B_den = [Buf() for _ in range(2)]
                B_at = [Buf() for _ in range(2)]
                B_sqa = [Buf() for _ in range(4)]
                B_mixst = [Buf() for _ in range(2)]
                B_sps = [Buf() for _ in range(2)]
                B_ops = [Buf() for _ in range(2)]
                B_dps = [Buf() for _ in range(2)]
                B_pwps, B_ss = Buf(), Buf()
                d_c2, d_q, d_u, d_ms = cx.dsem("c2"), [cx.dsem("q0"), cx.dsem("q1")], [cx.dsem("u0"), cx.dsem("u1")], [cx.dsem("ms0"), cx.dsem("ms1")]
                d_pw = cx.dsem("pw")
                cx.op(SP, lambda: nsp.dma_start(out=eb[:], in_=biastab_d.ap().rearrange("j p c -> p j c")), writes=[B_eb], dma=d_c2)
                cx.op(SP, lambda: nsp.dma_start(out=mk[:], in_=mask01_d.ap().rearrange("j p c -> p j c")), writes=[B_mk], dma=d_c2)
                cx.op(SP, lambda: nsp.dma_start(out=sinkexp[:], in_=sinkbc_d[:, :]), writes=[B_sk], dma=d_c2)
                cx.op(SP, lambda: nsp.dma_start(out=pscale[:], in_=pscale_d[:, :]), writes=[B_c2], dma=d_c2)
                cx.op(SP, lambda: nsp.dma_start(out=edgef[:], in_=edgef_d[:, :, :]), writes=[B_c2], dma=d_c2)
                cx.op(P, lambda: npl.dma_start(out=pw_sb[:], in_=pool_w_d.ap().rearrange("g (kc p) d -> p g kc d", p=128)),
                      writes=[B_pw], dma=d_pw)
                cx.op(V, lambda: nv.memset(ones_bf[:], 1.0), writes=[B_c2])
                cx.op(A, lambda: na.activation(out=sinkexp[:], in_=sinkexp[:], func=AF.Exp), reads=[B_sk], writes=[B_sk])
                for jk in range(6):
                    cx.op(V, lambda: nv.scalar_tensor_tensor(out=Bs[:, jk, :], in0=eb[:, jk, :], scalar=1.0 / (128.0 ** -0.5), in1=mk[:, jk % 3, :],
                                                             op0=ALU.mult, op1=ALU.add),
                          reads=[B_eb, B_mk], writes=[B_Bs])
                cx.op(V, lambda: nv.memset(uT[0][:, :, 0:8], 0.0), writes=[B_u[0]])
                SCALE = 128.0 ** -0.5
                ctr = {"sp": 0, "pt": 0, "bk": 0, "sq": 0}
                NPT = len(pt)

                def emit_loads(g):
                    gp = g % 2
                    t0 = g * 512
                    cx.op(SP, lambda: nsp.dma_start(out=qT[gp][:], in_=qu_pm[:, 8:16, t0:t0 + 512]),
                          reads=[B_qu], writes=[B_q[gp]], dma=d_q[gp])
                    if g == 0:
                        cx.op(SP, lambda: nsp.dma_start(out=uT[gp][:, :, 8:528], in_=qu_pm[:, 0:8, 0:520]),
                              reads=[B_qu], writes=[B_u[gp]], dma=d_u[gp])
                    elif g == NG - 1:
                        cx.op(V, lambda: nv.memset(uT[gp][:, :, 520:528], 0.0), writes=[B_u[gp]])
                        cx.op(SP, lambda: nsp.dma_start(out=uT[gp][:, :, 0:520], in_=qu_pm[:, 0:8, t0 - 8:S]),
                              reads=[B_qu], writes=[B_u[gp]], dma=d_u[gp])
                    else:
                        cx.op(SP, lambda: nsp.dma_start(out=uT[gp][:], in_=qu_pm[:, 0:8, t0 - 8:t0 + 520]),
                              reads=[B_qu], writes=[B_u[gp]], dma=d_u[gp])

                def stage_A(g, i, j):
                    gp = g % 2
                    Bk = 4 * g + i
                    pts = []
                    for kb in range(3):
                        KB = Bk + kb - 1
                        if KB < 0 or KB >= NT:
                            continue
                        sb_ = ctr["sp"] % 2
                        ctr["sp"] += 1
                        cx.op(PE, lambda: nt.matmul(sps[sb_][:], lhsT=kT_all[:, j, KB * 128:(KB + 1) * 128],
                                                    rhs=qT[gp][:, 4 * j:4 * j + 4, i * 128:(i + 1) * 128], start=True, stop=False),
                              reads=[B_kT[KB // 4], B_q[gp]], writes=[B_sps[sb_]])
                        cx.op(PE, lambda: nt.matmul(sps[sb_][:], lhsT=ident_bf[:], rhs=Bs[:, j * 3 + kb, :], start=False, stop=True),
                              reads=[B_const, B_Bs], writes=[B_sps[sb_]])
                        pb_ = ctr["pt"] % NPT
                        ctr["pt"] += 1
                        cx.op(A, lambda: na.activation(out=pt[pb_][:], in_=sps[sb_][:], func=AF.Exp, scale=SCALE),
                              reads=[B_sps[sb_]], writes=[B_pt[pb_]])
                        pts.append((pb_, KB))
                    return pts

                def stage_B(g, i, j, pts):
                    gp = g % 2
                    ob = ctr["bk"] % 2
                    ctr["bk"] += 1
                    for n, (pb_, KB) in enumerate(pts):
                        cx.op(PE, lambda: nt.matmul(o_ps[ob][:], lhsT=V_all[:, KB, j * 128:(j + 1) * 128], rhs=pt[pb_][:],
                                                    start=(n == 0), stop=(n == len(pts) - 1)),
                              reads=[B_V[KB // 4], B_pt[pb_]], writes=[B_ops[ob]])
                    for n, (pb_, KB) in enumerate(pts):
                        cx.op(PE, lambda: nt.matmul(d_ps[ob][:], lhsT=ones_bf[:], rhs=pt[pb_][:],
                                                    start=(n == 0), stop=(n == len(pts) - 1)),
                              reads=[B_c2, B_pt[pb_]], writes=[B_dps[ob]])
                    cx.op(V, lambda: nv.tensor_tensor(out=den[ob][:], in0=d_ps[ob][:], in1=sinkexp[:, j * 512:(j + 1) * 512], op=ALU.add),
                          reads=[B_dps[ob], B_sk], writes=[B_den[ob]])
                    cx.op(V, lambda: nv.reciprocal(out=den[ob][:], in_=den[ob][:]), reads=[B_den[ob]], writes=[B_den[ob]])
                    cx.op(V, lambda: nv.tensor_tensor(out=at[ob][:], in0=o_ps[ob][:],
                                                      in1=den[ob][:].rearrange("p (a b) -> p a b", b=128), op=ALU.mult),
                          reads=[B_ops[ob], B_den[ob]], writes=[B_at[ob]])
                    sb2 = ctr["sq"] % 4
                    ctr["sq"] += 1
                    cx.op(A, lambda: na.activation(out=sqa[sb2][:], in_=at[ob][:], func=AF.Square),
                          reads=[B_at[ob]], writes=[B_sqa[sb2]])
                    cx.op(V, lambda: nv.tensor_copy(out=mixst[gp][:, 8 + 4 * j:8 + 4 * j + 4, i * 128:(i + 1) * 128], in_=at[ob][:]),
                          reads=[B_at[ob]], writes=[B_mixst[gp]])
                    return sb2

                def stage_C(i, sq_pair):
                    n = 0
                    for sb2 in sq_pair:
                        for gq in range(4):
                            cx.op(PE, lambda: nt.matmul(ss_ps[:, i:i + 1], lhsT=sqa[sb2][:, gq, :], rhs=ones_bf[:, 0:1],
                                                        start=(n == 0), stop=(n == 7)),
                                  reads=[B_sqa[sb2], B_c2], writes=[B_ss])
                            n += 1

                def pooling(g, grp):
                    gp = g % 2
                    w = (2, 4, 8, 16)[grp]
                    U = uT[gp][:, 2 * grp:2 * grp + 2, :]
                    cx.op(V, lambda: nv.tensor_tensor(out=tmp[0][:, :, 1:528], in0=U[:, :, 0:527], in1=U[:, :, 1:528], op=ALU.add),
                          reads=[B_u[gp]], writes=[B_tmp])
                    if grp >= 1:
                        cx.op(V, lambda: nv.tensor_tensor(out=tmp[1][:, :, 2:527], in0=tmp[0][:, :, 1:526], in1=tmp[0][:, :, 3:528], op=ALU.add),
                              reads=[B_tmp], writes=[B_tmp])
                    if grp >= 2:
                        cx.op(V, lambda: nv.tensor_tensor(out=tmp[2][:, :, 4:525], in0=tmp[1][:, :, 2:523], in1=tmp[1][:, :, 6:527], op=ALU.add),
                              reads=[B_tmp], writes=[B_tmp])
                    if grp >= 3:
                        cx.op(V, lambda: nv.tensor_tensor(out=tmp[3][:, :, 8:521], in0=tmp[2][:, :, 4:517], in1=tmp[2][:, :, 12:525], op=ALU.add),
                              reads=[B_tmp], writes=[B_tmp])
                    sw = tmp[grp]
                    cx.op(V, lambda: nv.scalar_tensor_tensor(out=pooledT[:, 2 * grp:2 * grp + 2, :], in0=sw[:, :, 8:520], scalar=1.0 / w,
                                                             in1=U[:, :, 8:520], op0=ALU.mult, op1=ALU.subtract),
                          reads=[B_tmp, B_u[gp]], writes=[B_pooled])
                    edges = []
                    if g == 0:
                        edges.append((8, 0, 0))
                    if g == NG - 1:
                        edges.append((512, 504, 8))
                    for (uc, pc, ec) in edges:
                        for ch in range(2):
                            cx.op(V, lambda: nv.tensor_tensor(out=etmp[:, ch, :], in0=sw[:, ch, uc:uc + 8], in1=edgef[:, grp, ec:ec + 8], op=ALU.mult),
                                  reads=[B_tmp, B_c2], writes=[B_etmp])
                            cx.op(V, lambda: nv.tensor_tensor(out=pooledT[:, 2 * grp + ch, pc:pc + 8], in0=etmp[:, ch, :],
                                                              in1=uT[gp][:, 2 * grp + ch, uc:uc + 8], op=ALU.subtract),
                                  reads=[B_etmp, B_u[gp]], writes=[B_pooled])

                def group_end(g):
                    gp = g % 2
                    t0 = g * 512
                    for oc in range(8):
                        grp, hf = oc // 2, oc % 2
                        for kc in range(2):
                            cx.op(PE, lambda: nt.matmul(pw_ps[:], lhsT=pw_sb[:, grp, kc, hf * 128:(hf + 1) * 128], rhs=pooledT[:, 2 * grp + kc, :],
                                                        start=(kc == 0), stop=(kc == 1)),
                                  reads=[B_pw, B_pooled], writes=[B_pwps])
                        cx.op(A, lambda: na.activation(out=mixst[gp][:, oc, :], in_=pw_ps[:], func=AF.Copy, scale=pscale[:, oc:oc + 1]),
                              reads=[B_pwps, B_c2], writes=[B_mixst[gp]])
                        cx.op(A, lambda: na.activation(out=sqp[:, oc, :], in_=pw_ps[:], func=AF.Square, scale=pscale[:, oc:oc + 1]),
                              reads=[B_pwps, B_c2], writes=[B_sqp])
                    for i in range(4):
                        for oc in range(8):
                            cx.op(PE, lambda: nt.matmul(ss_ps[:, 4 + i:5 + i], lhsT=sqp[:, oc, i * 128:(i + 1) * 128], rhs=ones_bf[:, 0:1],
                                                        start=(oc == 0), stop=(oc == 7)),
                                  reads=[B_sqp, B_c2], writes=[B_ss])
                    rstd_from_ssq(ss_ps[:, 0:4], rstd_pa[:, 32 + 4 * g:32 + 4 * g + 4], 1024, [B_ss], [B_rstd])
                    rstd_from_ssq(ss_ps[:, 4:8], rstd_pa[:, 4 * g:4 * g + 4], 1024, [B_ss], [B_rstd])
                    cx.op(SP, lambda: nsp.dma_start(out=mix_pm[:, :, t0:t0 + 512], in_=mixst[gp][:]),
                          reads=[B_mixst[gp]], writes=[B_mix], dma=d_ms[gp])

                units = [(g, i, j) for g in range(NG) for i in range(4) for j in range(2)]
                sq_of = {}
                pend = []

                def do_B(u, pts):
                    g, i, j = u
                    sb2 = stage_B(g, i, j, pts)
                    sq_of.setdefault((g, i), []).append(sb2)
                    for fn in pend:
                        fn()
                    del pend[:]
                    if j == 0:
                        pooling(g, i)
                    if j == 1:
                        pair = sq_of.pop((g, i))
                        pend.append(lambda: stage_C(i, pair))
                        if i == 3:
                            pend.append(lambda: group_end(g))

                prev = None
                for u in units:
                    g, i, j = u
                    if i == 0 and j == 0:
                        emit_loads(g)
                    pts = stage_A(g, i, j)
                    if prev is not None:
                        do_B(*prev)
                    prev = (u, pts)
                do_B(*prev)
                for fn in pend:
                    fn()
                del pend[:]
                if debug == 2:
                    d_dbg = cx.dsem("dbg2")
                    cx.op(SP, lambda: nsp.dma_start(out=dbg_rstd[:, :], in_=rstd_pa[:]), reads=[B_rstd], writes=[B_out], dma=d_dbg)
                cx.barrier()
        if debug == 2:
            cx.barrier()
            return nc

        s3 = es.enter_context(ExitStack())
        posT = sbuf(s3, "posT", [128, NT, NE], F32)
        gaT = sbuf(s3, "gaT", [128, NT, NE], F32)
        vals = sbuf(s3, "vals", [128, NT, NE, 5], BF16)
        iota512 = sbuf(s3, "iota512", [128, 512], F32)
        B_posT, B_gaT, B_vals, B_iota = Buf(), Buf(), Buf(), Buf()
        with ExitStack() as s2:
            affT = sbuf(s2, "affT", [NE, S], F32)
            B_aff = Buf("aff")
            with ExitStack() as ph:
                w_out_sb = sbuf(ph, "w_out_sb", [128, 16, D], BF16)
                gnfm = sbuf(ph, "gnfm", [128, 16], F32)
                wr_sb = sbuf(ph, "wr_sb", [128, 16, NE], F32)
                wr_hi = sbuf(ph, "wr_hi", [128, 16, NE], BF16)
                wr_lo = sbuf(ph, "wr_lo", [128, 16, NE], BF16)
                h2lo = [sbuf(ph, "h2lo%d" % i, [128, D], BF16) for i in range(2)]
                hiT = sbuf(ph, "hiT", [128, 16, 128], BF16)
                loT = sbuf(ph, "loT", [128, 16, 128], BF16)
                B_hiT, B_loT = Buf(), Buf()
                B_h2lo = [Buf() for _ in range(2)]
                g2bc = sbuf(ph, "g2bc", [128, D], F32)
                mixld = [sbuf(ph, "mixld%d" % i, [128, 16, 512], BF16) for i in range(2)]
                xt = [sbuf(ph, "xt%d" % i, [128, D], F32) for i in range(2)]
                x1t = [sbuf(ph, "x1t%d" % i, [128, D], F32) for i in range(2)]
                h2f = sbuf(ph, "h2f", [128, D], F32)
                h2b = [sbuf(ph, "h2b%d" % i, [128, D], BF16) for i in range(2)]
                ex = sbuf(ph, "ex", [NE, 512], F32)
                rc = sbuf(ph, "rc", [NE, 512], F32)
                ones16 = sbuf(ph, "ones16", [NE, NE], F32)
                ssq2 = sbuf(ph, "ssq2", [128, NT], F32)
                rs2 = sbuf(ph, "rs2", [128, NT], F32)
                P1 = [psum(ph, "P1_%d" % i, [128, 512], F32) for i in range(2)]
                P2 = [psum(ph, "P2_%d" % i, [128, 512], F32) for i in range(2)]
                tpf = [psum(ph, "tpf%d" % i, [128, 8, 128], BF16) for i in range(2)]
                lg_ps = [psum(ph, "lgps%d" % i, [NE, 512], F32) for i in range(2)]
                B_wo, B_ws, B_c3, B_g2 = Buf(), Buf(), Buf(), Buf()
                B_ml = [Buf() for _ in range(2)]
                B_xt = [Buf() for _ in range(2)]
                B_x1 = [Buf() for _ in range(2)]
                B_h2f = Buf()
                B_h2b = [Buf() for _ in range(2)]
                B_h2T, B_ex, B_rc, B_ssq2, B_rs2 = Buf(), Buf(), Buf(), Buf(), Buf()
                B_P1 = [Buf() for _ in range(2)]
                B_P2 = [Buf() for _ in range(2)]
                B_tpf = [Buf() for _ in range(2)]
                B_lg = [Buf() for _ in range(2)]
                B_sm = Buf()
                d_c3, d_ws = cx.dsem("c3"), cx.dsem("ws")
                d_ml, d_xt = [cx.dsem("ml0"), cx.dsem("ml1")], [cx.dsem("xt0"), cx.dsem("xt1")]
                d_x1, d_h2 = [cx.dsem("x1s0"), cx.dsem("x1s1")], [cx.dsem("h2s0"), cx.dsem("h2s1")]
                cx.op(SP, lambda: nsp.dma_start(out=gnfm[:], in_=gnfm_d[:, :]), writes=[B_c3], dma=d_c3)
                cx.op(SP, lambda: nsp.dma_start(out=g2bc[:], in_=g2bc_d[:, :]), writes=[B_g2], dma=d_c3)
                cx.op(SP, lambda: nsp.dma_start(out=wr_sb[:], in_=w_router_d.ap().rearrange("(kc p) e -> p kc e", p=128)),
                      writes=[B_c3], dma=d_c3)
                cx.op(V, lambda: nv.memset(ones16[:], 1.0), writes=[B_c3])
                cx.op(V, lambda: nv.memset(ssq2[:], 0.0), writes=[B_ssq2])
                cx.op(V, lambda: nv.tensor_copy(out=wr_hi[:], in_=wr_sb[:]), reads=[B_c3], writes=[B_c3])
                cx.op(V, lambda: nv.tensor_tensor(out=wr_sb[:], in0=wr_sb[:], in1=wr_hi[:], op=ALU.subtract), reads=[B_c3], writes=[B_c3])
                cx.op(V, lambda: nv.tensor_copy(out=wr_lo[:], in_=wr_sb[:]), reads=[B_c3], writes=[B_c3])
                for kc in range(16):
                    wst = h2f if kc % 2 == 0 else xt[0]
                    wB = B_h2f if kc % 2 == 0 else B_xt[0]
                    cx.op(SP, lambda: nsp.dma_start(out=wst[:], in_=w_out_d[kc * 128:(kc + 1) * 128, :]), writes=[wB], dma=d_ws)
                    cx.op(V, lambda: nv.tensor_scalar(out=w_out_sb[:, kc, :], in0=wst[:], scalar1=gnfm[:, kc:kc + 1], scalar2=None, op0=ALU.mult),
                          reads=[wB, B_c3], writes=[B_wo])
                pend_rt = []
                rt_done = {}
                tfc = [0]

                def flush_router():
                    for (g_, i_, tq_) in pend_rt:
                        for (src, srcB, dstT, dstB) in ((h2b[tq_], B_h2b[tq_], hiT, B_hiT), (h2lo[tq_], B_h2lo[tq_], loT, B_loT)):
                            for half in range(2):
                                tb = tfc[0] % 2
                                tfc[0] += 1
                                for k in range(8):
                                    kc = half * 8 + k
                                    cx.op(PE, lambda: nt.transpose(out=tpf[tb][:, k, :], in_=src[:, kc * 128:(kc + 1) * 128], identity=ident_bf[:]),
                                          reads=[srcB, B_const], writes=[B_tpf[tb]])
                                if half == 0:
                                    cx.op(A, lambda: na.copy(out=dstT[:, half * 8:half * 8 + 8, :], in_=tpf[tb][:]), reads=[B_tpf[tb]], writes=[dstB])
                                else:
                                    cx.op(V, lambda: nv.tensor_copy(out=dstT[:, half * 8:half * 8 + 8, :], in_=tpf[tb][:]), reads=[B_tpf[tb]], writes=[dstB])
                        n = 0
                        for (wsb, rT, rB) in ((wr_hi, hiT, B_hiT), (wr_hi, loT, B_loT), (wr_lo, hiT, B_hiT)):
                            for kc in range(16):
                                cx.op(PE, lambda: nt.matmul(lg_ps[g_ % 2][:, i_ * 128:(i_ + 1) * 128], lhsT=wsb[:, kc, :], rhs=rT[:, kc, :],
                                                            start=(n == 0), stop=(n == 47)),
                                      reads=[B_c3, rB], writes=[B_lg[g_ % 2]])
                                n += 1
                        rt_done[g_] = rt_done.get(g_, 0) + 1
                    del pend_rt[:]

                sm_done = set()

                def softmax_group(g_, final):
                    for gg in range(NG):
                        if gg in sm_done or rt_done.get(gg, 0) < 4:
                            continue
                        sm_done.add(gg)
                        tg = gg * 512
                        cx.op(A, lambda: na.activation(out=ex[:], in_=lg_ps[gg % 2][:], func=AF.Exp), reads=[B_lg[gg % 2]], writes=[B_ex])
                        cx.op(PE, lambda: nt.matmul(P1[0][0:NE, :], lhsT=ones16[:], rhs=ex[:], start=True, stop=True),
                              reads=[B_c3, B_ex], writes=[B_P1[0]])
                        cx.op(V, lambda: nv.reciprocal(out=rc[:], in_=P1[0][0:NE, :]), reads=[B_P1[0]], writes=[B_rc])
                        cx.op(V, lambda: nv.tensor_tensor(out=affT[:, tg:tg + 512], in0=ex[:], in1=rc[:], op=ALU.mult),
                              reads=[B_ex, B_rc], writes=[B_aff])

                pbi = 0
                tfi = 0
                for g in range(NG):
                    gp = g % 2
                    t0 = g * 512
                    cx.op(SP, lambda: nsp.dma_start(out=mixld[gp][:], in_=mix_pm[:, :, t0:t0 + 512]),
                          reads=[B_mix], writes=[B_ml[gp]], dma=d_ml[gp])
                    for i in range(4):
                        T = 4 * g + i
                        tq = T % 2
                        cx.op(SP, lambda: nsp.dma_start(out=xt[tq][:], in_=x_d[T * 128:(T + 1) * 128, :]), writes=[B_xt[tq]], dma=d_xt[tq])
                        for ds in range(4):
                            pb = pbi % 2
                            pbi += 1
                            dsl = slice(ds * 512, (ds + 1) * 512)
                            for kc in range(8):
                                cx.op(PE, lambda: nt.matmul(P1[pb][:], lhsT=mixld[gp][:, kc, i * 128:(i + 1) * 128], rhs=w_out_sb[:, kc, dsl],
                                                            start=(kc == 0), stop=(kc == 7)),
                                      reads=[B_ml[gp], B_wo], writes=[B_P1[pb]])
                            for kc in range(8, 16):
                                cx.op(PE, lambda: nt.matmul(P2[pb][:], lhsT=mixld[gp][:, kc, i * 128:(i + 1) * 128], rhs=w_out_sb[:, kc, dsl],
                                                            start=(kc == 8), stop=(kc == 15)),
                                      reads=[B_ml[gp], B_wo], writes=[B_P2[pb]])
                            cx.op(V, lambda: nv.scalar_tensor_tensor(out=x1t[tq][:, dsl], in0=P1[pb][:], scalar=rstd_pa[:, T:T + 1],
                                                                     in1=xt[tq][:, dsl], op0=ALU.mult, op1=ALU.add),
                                  reads=[B_P1[pb], B_rstd, B_xt[tq]], writes=[B_x1[tq]])
                            cx.op(V, lambda: nv.scalar_tensor_tensor(out=x1t[tq][:, dsl], in0=P2[pb][:], scalar=rstd_pa[:, 32 + T:33 + T],
                                                                     in1=x1t[tq][:, dsl], op0=ALU.mult, op1=ALU.add),
                                  reads=[B_P2[pb], B_rstd, B_x1[tq]], writes=[B_x1[tq]])
                        cx.op(SP, lambda: nsp.dma_start(out=acc_d[T * 128:(T + 1) * 128, :], in_=x1t[tq][:]),
                              reads=[B_x1[tq]], writes=[B_acc], dma=d_x1[tq])
                        cx.op(A, lambda: na.activation(out=h2f[:], in_=x1t[tq][:], func=AF.Square, accum_out=ssq2[:, T:T + 1]),
                              reads=[B_x1[tq]], writes=[B_h2f, B_ssq2])
                        rstd_from_ssq(ssq2[:, T:T + 1], rs2[:, T:T + 1], D, [B_ssq2], [B_rs2])
                        cx.op(V, lambda: nv.scalar_tensor_tensor(out=h2f[:], in0=x1t[tq][:], scalar=rs2[:, T:T + 1], in1=g2bc[:],
                                                                 op0=ALU.mult, op1=ALU.mult),
                              reads=[B_x1[tq], B_rs2, B_g2], writes=[B_h2f])
                        cx.op(A, lambda: na.copy(out=h2b[tq][:], in_=h2f[:]), reads=[B_h2f], writes=[B_h2b[tq]])
                        cx.op(SP, lambda: nsp.dma_start(out=h2_scr[T * 128:(T + 1) * 128, :], in_=h2b[tq][:]),
                              reads=[B_h2b[tq]], writes=[B_h2], dma=d_h2[tq])
                        cx.op(V, lambda: nv.tensor_tensor(out=h2lo[tq][:], in0=h2f[:], in1=h2b[tq][:], op=ALU.subtract),
                              reads=[B_h2f, B_h2b[tq]], writes=[B_h2lo[tq]])
                        flush_router()
                        pend_rt.append((g, i, tq))
                    softmax_group(g - 1 if g > 0 else None, final=False)
                flush_router()
                softmax_group(NG - 1, final=True)
                if debug in (3, 4):
                    d_dbg = cx.dsem("dbg3")
                    cx.op(SP, lambda: nsp.dma_start(out=dbg_aff[:, :], in_=affT[:]), reads=[B_aff], writes=[B_out], dma=d_dbg)
                cx.barrier()
            if debug == 3:
                cx.barrier()
                return nc

            with ExitStack() as ph:
                lo = sbuf(ph, "lo", [NE, 1], F32)
                hi = sbuf(ph, "hi", [NE, 1], F32)
                mid = sbuf(ph, "mid", [NE, 1], F32)
                cntall = sbuf(ph, "cntall", [NE, NBISECT], F32)
                sel = sbuf(ph, "sel", [NE, 1], F32)
                nsel = sbuf(ph, "nsel", [NE, 1], F32)
                ta = sbuf(ph, "ta", [NE, 1], F32)
                tb_ = sbuf(ph, "tb", [NE, 1], F32)
                junk = sbuf(ph, "junk", [NE, S], F32)
                msk = sbuf(ph, "msk", [NE, S], F32)
                cs = sbuf(ph, "cs", [NE, S], F32)
                ones_s = sbuf(ph, "ones_s", [NE, S], F32)
                tokab = sbuf(ph, "tokab", [128, NT, 2], F32)
                r1 = sbuf(ph, "r1", [128, NT, NE], F32)
                pT_ps = psum(ph, "pTps", [128, NT, NE], F32)
                gT_ps = psum(ph, "gTps", [128, NT, NE], F32)
                B_b, B_junk, B_msk, B_cs, B_ones, B_tok, B_r1, B_pTps, B_gTps = [Buf() for _ in range(9)]
                d_c4 = cx.dsem("c4")
                cx.op(SP, lambda: nsp.dma_start(out=iota512[:], in_=iota512_d[:, :]), writes=[B_iota], dma=d_c4)
                cx.op(SP, lambda: nsp.dma_start(out=tokab[:], in_=tokab_d[:, :, :]), writes=[B_tok], dma=d_c4)
                cx.op(V, lambda: nv.memset(lo[:], 0.0), writes=[B_b])
                cx.op(V, lambda: nv.memset(hi[:], 1.0), writes=[B_b])
                cx.op(P, lambda: npl.memset(ones_s[:], 1.0), writes=[B_ones])
                cx.op(V, lambda: nv.memset(cntall[:], 0.0), writes=[B_b])
                for it in range(NBISECT):
                    cx.op(V, lambda: nv.tensor_scalar(out=mid[:], in0=lo[:], scalar1=hi[:, 0:1], scalar2=0.5, op0=ALU.add, op1=ALU.mult),
                          reads=[B_b], writes=[B_b])
                    cx.op(V, lambda: nv.tensor_scalar(out=junk[:], in0=affT[:], scalar1=mid[:, 0:1], scalar2=0.0, op0=ALU.is_ge, op1=ALU.add,
                                                      accum_out=cntall[:, it:it + 1]),
                          reads=[B_aff, B_b], writes=[B_junk, B_b])
                    cx.op(V, lambda: nv.tensor_scalar(out=sel[:], in0=cntall[:, it:it + 1], scalar1=float(CAP), scalar2=None, op0=ALU.is_ge),
                          reads=[B_b], writes=[B_b])
                    cx.op(V, lambda: nv.scalar_tensor_tensor(out=lo[:], in0=mid[:], scalar=sel[:, 0:1], in1=lo[:], op0=ALU.mult, op1=ALU.max),
                          reads=[B_b], writes=[B_b])
                    cx.op(V, lambda: nv.scalar_tensor_tensor(out=ta[:], in0=sel[:], scalar=2.0, in1=mid[:], op0=ALU.mult, op1=ALU.add),
                          reads=[B_b], writes=[B_b])
                    cx.op(V, lambda: nv.tensor_tensor(out=hi[:], in0=hi[:], in1=ta[:], op=ALU.min), reads=[B_b], writes=[B_b])
                cx.op(V, lambda: nv.tensor_scalar(out=msk[:], in0=affT[:], scalar1=lo[:, 0:1], scalar2=None, op0=ALU.is_ge),
                      reads=[B_aff, B_b], writes=[B_msk])
                cx.op(V, lambda: nv.tensor_tensor_scan(out=cs[:], data0=ones_s[:], data1=msk[:], initial=0.0, op0=ALU.mult, op1=ALU.add),
                      reads=[B_ones, B_msk], writes=[B_cs])
                cx.op(V, lambda: nv.tensor_tensor(out=cs[:], in0=cs[:], in1=msk[:], op=ALU.mult), reads=[B_cs, B_msk], writes=[B_cs])
                cx.op(V, lambda: nv.tensor_scalar(out=cs[:], in0=cs[:], scalar1=-1.0, scalar2=None, op0=ALU.add), reads=[B_cs], writes=[B_cs])
                for jt in range(NT):
                    cx.op(PE, lambda: nt.transpose(out=pT_ps[:, jt, :], in_=cs[:, jt * 128:(jt + 1) * 128], identity=ident_f[0:NE, 0:NE]),
                          reads=[B_cs, B_const], writes=[B_pTps])
                for jt in range(NT):
                    cx.op(PE, lambda: nt.transpose(out=gT_ps[:, jt, :], in_=affT[:, jt * 128:(jt + 1) * 128], identity=ident_f[0:NE, 0:NE]),
                          reads=[B_aff, B_const], writes=[B_gTps])
                cx.op(V, lambda: nv.tensor_copy(out=posT[:], in_=pT_ps[:]), reads=[B_pTps], writes=[B_posT])
                cx.op(A, lambda: na.copy(out=gaT[:], in_=gT_ps[:]), reads=[B_gTps], writes=[B_gaT])
                cx.op(V, lambda: nv.tensor_copy(out=vals[:, :, :, 2], in_=gaT[:]), reads=[B_gaT], writes=[B_vals])
                cx.op(V, lambda: nv.tensor_tensor(out=r1[:], in0=gaT[:], in1=vals[:, :, :, 2], op=ALU.subtract), reads=[B_gaT, B_vals], writes=[B_r1])
                cx.op(V, lambda: nv.tensor_copy(out=vals[:, :, :, 3], in_=r1[:]), reads=[B_r1], writes=[B_vals])
                cx.op(V, lambda: nv.tensor_tensor(out=r1[:], in0=r1[:], in1=vals[:, :, :, 3], op=ALU.subtract), reads=[B_r1, B_vals], writes=[B_r1])
                cx.op(V, lambda: nv.tensor_copy(out=vals[:, :, :, 4], in_=r1[:]), reads=[B_r1], writes=[B_vals])
                for e in range(NE):
                    cx.op(P, lambda: npl.tensor_copy(out=vals[:, :, e, 0:2], in_=tokab[:]), reads=[B_tok], writes=[B_vals])
                if debug == 4:
                    d_dbg = cx.dsem("dbg4")
                    cx.op(SP, lambda: nsp.dma_start(out=dbg_thr[0, :, :], in_=lo[:]), reads=[B_b], writes=[B_out], dma=d_dbg)
                    cx.op(SP, lambda: nsp.dma_start(out=dbg_thr[1, :, :], in_=hi[:]), reads=[B_b], writes=[B_out], dma=d_dbg)
                cx.barrier()
        s4 = es.enter_context(ExitStack())
        oh = [sbuf(s4, "oh%d" % i, [128, 512], BF16) for i in range(2)]
        cmp_sb = sbuf(s4, "cmp_sb", [128, 4, 5], F32)
        idxf = [sbuf(s4, "idxf%d" % i, [128, 4], F32) for i in range(3)]
        idxi = [sbuf(s4, "idxi%d" % i, [128, 4], I32) for i in range(3)]
        gts = [sbuf(s4, "gts%d" % i, [128, 4], F32) for i in range(3)]
        idxqf = [sbuf(s4, "idxqf%d" % i, [128, 4, 4], F32) for i in range(3)]
        idxqi = [sbuf(s4, "idxqi%d" % i, [128, 4, 4], I32) for i in range(3)]
        cmp_ps = psum(s4, "cmpps", [128, 4, 5], F32)
        B_oh = [Buf() for _ in range(2)]
        B_cmps, B_cmpp = Buf(), Buf()
        B_idx = [Buf() for _ in range(3)]
        ohc = [0]

        def compact(e):
            ep = e % 3
            for jt in range(NT):
                ob = ohc[0] % 2
                ohc[0] += 1
                cx.op(V, lambda: nv.tensor_scalar(out=oh[ob][:], in0=iota512[:], scalar1=posT[:, jt, e:e + 1], scalar2=None, op0=ALU.is_equal),
                      reads=[B_iota, B_posT], writes=[B_oh[ob]])
                for cc in range(4):
                    cx.op(PE, lambda: nt.matmul(cmp_ps[:, cc, :], lhsT=oh[ob][:, cc * 128:(cc + 1) * 128], rhs=vals[:, jt, e, :],
                                                start=(jt == 0 and cc == 0), stop=(jt == NT - 1)),
                          reads=[B_oh[ob], B_vals], writes=[B_cmpp])
            cx.op(A, lambda: na.copy(out=cmp_sb[:], in_=cmp_ps[:]), reads=[B_cmpp], writes=[B_cmps])
            cx.op(V, lambda: nv.scalar_tensor_tensor(out=idxf[ep][:], in0=cmp_sb[:, :, 0], scalar=64.0, in1=cmp_sb[:, :, 1],
                                                     op0=ALU.mult, op1=ALU.add), reads=[B_cmps], writes=[B_idx[ep]])
            cx.op(V, lambda: nv.tensor_copy(out=idxi[ep][:], in_=idxf[ep][:]), reads=[B_idx[ep]], writes=[B_idx[ep]])
            for dsq in range(4):
                cx.op(V, lambda: nv.tensor_scalar(out=idxqf[ep][:, dsq, :], in0=idxf[ep][:], scalar1=4.0, scalar2=float(dsq), op0=ALU.mult, op1=ALU.add),
                      reads=[B_idx[ep]], writes=[B_idx[ep]])
            cx.op(V, lambda: nv.tensor_copy(out=idxqi[ep][:], in_=idxqf[ep][:]), reads=[B_idx[ep]], writes=[B_idx[ep]])
            cx.op(V, lambda: nv.tensor_tensor(out=gts[ep][:], in0=cmp_sb[:, :, 2], in1=cmp_sb[:, :, 3], op=ALU.add),
                  reads=[B_cmps], writes=[B_idx[ep]])
            cx.op(V, lambda: nv.tensor_tensor(out=gts[ep][:], in0=gts[ep][:], in1=cmp_sb[:, :, 4], op=ALU.add),
                  reads=[B_cmps, B_idx[ep]], writes=[B_idx[ep]])

        if debug == 4:
            d_dbg5 = cx.dsem("dbg5")
            for e in range(NE):
                compact(e)
                cx.op(SP, lambda: nsp.dma_start(out=dbg_idx[e, 0, :, :], in_=idxf[e % 3][:]), reads=[B_idx[e % 3]], writes=[B_out], dma=d_dbg5)
                cx.op(SP, lambda: nsp.dma_start(out=dbg_idx[e, 1, :, :], in_=gts[e % 3][:]), reads=[B_idx[e % 3]], writes=[B_out], dma=d_dbg5)
            cx.barrier()
            return nc

        with ExitStack() as ph:
            ring = [sbuf(ph, "ring%d" % i, [128, 16, 512], BF16) for i in range(NSLOT)]
            xs = sbuf(ph, "xs", [128, 4, D], BF16)
            xsT = sbuf(ph, "xsT", [128, 16, 512], BF16)
            gT = sbuf(ph, "gT", [128, 32, 512], BF16)
            sa = [sbuf(ph, "sa%d" % i, [128, 512], F32) for i in range(2)]
            ystage = [sbuf(ph, "ystage%d" % i, [128, 4, 512], F32) for i in range(2)]
            a_ps = [psum(ph, "aps%d" % i, [128, 512], F32) for i in range(2)]
            u_ps = [psum(ph, "ups%d" % i, [128, 512], F32) for i in range(2)]
            y_ps = [psum(ph, "yps%d" % i, [128, 512], F32) for i in range(2)]
            tp3 = psum(ph, "tp3", [128, 8, 128], BF16)
            B_ring = [Buf() for _ in range(NSLOT)]
            d_ring = [cx.dsem("rg%d" % i) for i in range(NSLOT)]
            B_xs, B_xsT, B_gT, B_tp3 = Buf(), Buf(), Buf(), Buf()
            B_ys = [Buf() for _ in range(2)]
            B_sa = [Buf() for _ in range(2)]
            B_aps = [Buf() for _ in range(2)]
            B_ups = [Buf() for _ in range(2)]
            B_yps = [Buf() for _ in range(2)]
            B_sc = [[Buf() for _ in range(16)] for _ in range(2)]
            d_xs = cx.dsem("xs")
            d_sc = [cx.dsem("sc%d" % i) for i in range(4)]
            wg_v = [w_gate_d[e].rearrange("(kc p) f -> p kc f", p=128) for e in range(NE)]
            wu_v = [w_up_d[e].rearrange("(kc p) f -> p kc f", p=128) for e in range(NE)]
            wd_v = [w_down_d[e].rearrange("(fc p) d -> p fc d", p=128) for e in range(NE)]
            acc_q = acc_d.ap().rearrange("s (a c) -> (s a) c", c=512)
            loads = []
            for e in range(NE):
                for fg in range(8):
                    loads.append(wg_v[e][:, :, fg * 512:(fg + 1) * 512])
                    loads.append(wu_v[e][:, :, fg * 512:(fg + 1) * 512])
                for ds in range(4):
                    for hf in range(2):
                        loads.append(wd_v[e][:, hf * 16:(hf + 1) * 16, ds * 512:(ds + 1) * 512])
            nxt = [0]

            def ensure_loads(upto):
                while nxt[0] <= upto and nxt[0] < len(loads):
                    k = nxt[0]
                    sl = k % NSLOT
                    src = loads[k]
                    cx.op(P, lambda: npl.dma_start(out=ring[sl][:], in_=src), writes=[B_ring[sl]], dma=d_ring[sl])
                    nxt[0] += 1

            def gather(e):
                ep = e % 3
                for cc in range(4):
                    cx.op(P, lambda: npl.indirect_dma_start(out=xs[:, cc, :], out_offset=None, in_=h2_scr[:, :],
                                                            in_offset=bass.IndirectOffsetOnAxis(ap=idxi[ep][:, cc:cc + 1], axis=0)),
                          reads=[B_idx[ep], B_h2], writes=[B_xs], dma=d_xs)

            def transposes(e):
                for cc in range(4):
                    for half in range(2):
                        for k in range(8):
                            kc = half * 8 + k
                            cx.op(PE, lambda: nt.transpose(out=tp3[:, k, :], in_=xs[:, cc, kc * 128:(kc + 1) * 128], identity=ident_bf[:]),
                                  reads=[B_xs, B_const], writes=[B_tp3])
                        dst = xsT[:, half * 8:half * 8 + 8, cc * 128:(cc + 1) * 128]
                        if half == 0:
                            cx.op(A, lambda: na.copy(out=dst, in_=tp3[:]), reads=[B_tp3], writes=[B_xsT])
                        else:
                            cx.op(V, lambda: nv.tensor_copy(out=dst, in_=tp3[:]), reads=[B_tp3], writes=[B_xsT])

            pend_sc = []

            def flush_sc():
                for (e_, ds_) in pend_sc:
                    ep_ = e_ % 3
                    for tt in range(4):
                        cx.op(P, lambda: npl.indirect_dma_start(out=acc_q,
                                                                out_offset=bass.IndirectOffsetOnAxis(ap=idxqi[ep_][:, ds_, tt:tt + 1], axis=0),
                                                                in_=ystage[ds_ % 2][:, tt, :], in_offset=None, compute_op=ALU.add),
                              reads=[B_ys[ds_ % 2], B_idx[ep_], B_acc] + B_sc[(e_ + 1) % 2], writes=[B_sc[e_ % 2][ds_ * 4 + tt]], dma=d_sc[tt])
                del pend_sc[:]

            ensure_loads(NSLOT - 1)
            compact(0)
            gather(0)
            transposes(0)
            compact(1)
            gather(1)
            abi = 0
            ybi = 0
            for e in range(NE):
                ep = e % 3
                sp_ = e % 2
                base = e * 24
                for fg in range(8):
                    kg = base + 2 * fg
                    ku = kg + 1
                    ensure_loads(ku + NSLOT - 2)
                    sg, su = kg % NSLOT, ku % NSLOT
                    if fg == 1:
                        flush_sc()
                    for fl in range(4):
                        fc = fg * 4 + fl
                        ab = abi % 2
                        abi += 1
                        for kc in range(16):
                            cx.op(PE, lambda: nt.matmul(a_ps[ab][:], lhsT=ring[sg][:, kc, fl * 128:(fl + 1) * 128], rhs=xsT[:, kc, :],
                                                        start=(kc == 0), stop=(kc == 15)),
                                  reads=[B_ring[sg], B_xsT], writes=[B_aps[ab]])
                        for kc in range(16):
                            cx.op(PE, lambda: nt.matmul(u_ps[ab][:], lhsT=ring[su][:, kc, fl * 128:(fl + 1) * 128], rhs=xsT[:, kc, :],
                                                        start=(kc == 0), stop=(kc == 15)),
                                  reads=[B_ring[su], B_xsT], writes=[B_ups[ab]])
                        cx.op(A, lambda: na.activation(out=sa[ab][:], in_=a_ps[ab][:], func=AF.Silu), reads=[B_aps[ab]], writes=[B_sa[ab]])
                        cx.op(V, lambda: nv.tensor_tensor(out=gT[:, fc, :], in0=sa[ab][:], in1=u_ps[ab][:], op=ALU.mult),
                              reads=[B_sa[ab], B_ups[ab]], writes=[B_gT])
                for ds in range(4):
                    k0 = base + 16 + 2 * ds
                    k1 = k0 + 1
                    ensure_loads(k1 + NSLOT - 2)
                    s0, s1_ = k0 % NSLOT, k1 % NSLOT
                    for tt in range(4):
                        yb = ybi % 2
                        ybi += 1
                        for fc in range(32):
                            sl = s0 if fc < 16 else s1_
                            cx.op(PE, lambda: nt.matmul(y_ps[yb][:], lhsT=gT[:, fc, tt * 128:(tt + 1) * 128], rhs=ring[sl][:, fc % 16, :],
                                                        start=(fc == 0), stop=(fc == 31)),
                                  reads=[B_gT, B_ring[sl]], writes=[B_yps[yb]])
                        cx.op(A, lambda: na.activation(out=ystage[ds % 2][:, tt, :], in_=y_ps[yb][:], func=AF.Copy,
                                                       scale=gts[ep][:, tt:tt + 1]),
                              reads=[B_yps[yb], B_idx[ep]], writes=[B_ys[ds % 2]])
                    flush_sc()
                    pend_sc.append((e, ds))
                    if ds == 0 and e + 1 < NE:
                        transposes(e + 1)
                        if e + 2 < NE:
                            compact(e + 2)
                if e + 2 < NE:
                    gather(e + 2)
            flush_sc()
            cx.barrier()

        with ExitStack() as ph:
            gfbc = sbuf(ph, "gfbc", [128, D], F32)
            a4 = [sbuf(ph, "a4_%d" % i, [128, D], F32) for i in range(2)]
            o4 = [sbuf(ph, "o4_%d" % i, [128, D], F32) for i in range(2)]
            ssq4 = sbuf(ph, "ssq4", [128, NT], F32)
            rs4 = sbuf(ph, "rs4", [128, NT], F32)
            B_gf, B_ssq4, B_rs4 = Buf(), Buf(), Buf()
            B_a4 = [Buf() for _ in range(2)]
            B_o4 = [Buf() for _ in range(2)]
            d_a4 = [cx.dsem("a40"), cx.dsem("a41")]
            cx.op(SP, lambda: nsp.dma_start(out=gfbc[:], in_=gfbc_d[:, :]), writes=[B_gf], dma=d_const)
            cx.op(V, lambda: nv.memset(ssq4[:], 0.0), writes=[B_ssq4])
            for T in range(NT):
                tq = T % 2
                cx.op(SP, lambda: nsp.dma_start(out=a4[tq][:], in_=acc_d[T * 128:(T + 1) * 128, :]),
                      reads=[B_acc] + B_sc[0] + B_sc[1], writes=[B_a4[tq]], dma=d_a4[tq])
                cx.op(A, lambda: na.activation(out=o4[tq][:], in_=a4[tq][:], func=AF.Square, accum_out=ssq4[:, T:T + 1]),
                      reads=[B_a4[tq]], writes=[B_o4[tq], B_ssq4])
                rstd_from_ssq(ssq4[:, T:T + 1], rs4[:, T:T + 1], D, [B_ssq4], [B_rs4])
                cx.op(V, lambda: nv.scalar_tensor_tensor(out=o4[tq][:], in0=a4[tq][:], scalar=rs4[:, T:T + 1], in1=gfbc[:],
                                                         op0=ALU.mult, op1=ALU.mult),
                      reads=[B_a4[tq], B_rs4, B_gf], writes=[B_o4[tq]])
                cx.op(SP, lambda: nsp.dma_start(out=out_d[T * 128:(T + 1) * 128, :], in_=o4[tq][:]),
                      reads=[B_o4[tq]], writes=[B_out], dma=d_out)
        cx.barrier()

    return nc


_CONST = None
_NC_CACHE = {}


def _shared_maps(inp):
    global _CONST
    if _CONST is None:
        _CONST = _host_constants()
    c = _CONST
    f32 = lambda a: np.ascontiguousarray(np.asarray(a, dtype=np.float32))
    bc = lambda v: np.ascontiguousarray(np.broadcast_to(f32(v).reshape(1, -1), (128, f32(v).size)))
    m = {}
    m["g1bc"] = bc(inp["norm1_g"][0])
    m["g2bc"] = bc(inp["norm2_g"][0])
    m["gfbc"] = bc(inp["final_g"])
    m["w_in"] = f32(inp["w_in"][0])
    m["pool_w"] = f32(inp["pool_w"][0])
    m["w_out"] = f32(inp["w_out"][0])
    m["w_router"] = f32(inp["w_router"][0])
    m["w_gate"] = f32(inp["w_gate"][0])
    m["w_up"] = f32(inp["w_up"][0])
    m["w_down"] = f32(inp["w_down"][0])
    m["pscale"] = np.ascontiguousarray(f32(inp["pool_scale"][0]).reshape(8, 128).T)
    gn = np.concatenate([f32(inp["gn_pool"][0]), f32(inp["gn_attn"][0])])
    m["gnfm"] = np.ascontiguousarray(gn.reshape(16, 128).T)
    sink = f32(inp["sink"][0])
    m["sinkbc"] = np.ascontiguousarray(np.broadcast_to(np.repeat(sink, 128)[None, :], (128, 1024)))
    rb = f32(inp["rel_bias"])
    bidx = c["_bidx"]
    bt = np.zeros((2, 3, 128, 4, 128), np.float32)
    for j in range(2):
        for gq in range(4):
            bt[j, :, :, gq, :] = rb[:, 4 * j + gq][bidx]
    m["biastab"] = np.ascontiguousarray(bt.reshape(6, 128, 512))
    for k in ("mask01", "ident_bf", "ident_f", "iota512", "tokab", "edgef"):
        m[k] = c[k]
    return m


def _get_nc(debug=0):
    if debug not in _NC_CACHE:
        _NC_CACHE[debug] = build_nc(debug)
    return _NC_CACHE[debug]


def kernel(**inputs):
    inp = {k: np.asarray(v) for k, v in inputs.items()}
    shared = _shared_maps(inp)
    x = np.asarray(inp["x"], dtype=np.float32)
    in_maps = []
    for c in range(NCORES):
        m = dict(shared)
        m["x"] = np.ascontiguousarray(x[c])
        in_maps.append(m)
    nc = _get_nc(0)
    res = run_bass_kernel_spmd(nc, in_maps, core_ids=list(range(NCORES)))
    out = np.stack([np.asarray(res.results[b]["out"], dtype=np.float32) for b in range(4)], axis=0)
    return out
```
